# Optimizing a Trainium2 kernel written in Bass

```python
import math
import jax, jax.numpy as jnp
from jax import lax
import numpy as np

D_MODEL = 1024
BATCH = 8
SEQ = 4096
DEPTH = 2

N_MIXERS = 2
BLOCK_Q = 128
DA_HEADS = 8
DA_HEAD_DIM = 64
SB_HEADS = 16
SB_HEAD_DIM = 64
REL_BUCKETS = 32
REL_MAX_DIST = 128
MOE_GROUPS = 4
MOE_EXPERTS_PER_GROUP = 8
MOE_TOP_K = 2
MOE_HIDDEN = 512
N_EXPERTS = MOE_GROUPS * MOE_EXPERTS_PER_GROUP
DEEPNORM_ALPHA = (2 * DEPTH) ** 0.25
DEEPNORM_BETA = (8 * DEPTH) ** -0.25
LN_EPS = 1e-5
RMS_EPS = 1e-6
NEG_INF = -1e30
N_DA_LAYERS = (DEPTH + 1) // 2
N_SB_LAYERS = DEPTH // 2

kernel_name = "hybrid_diffattn_stickbreak_hmoe_deepnorm"


def layer_norm(x, g, b):
    xf = x.astype(jnp.float32)
    mu = jnp.mean(xf, axis=-1, keepdims=True)
    xc = xf - mu
    var = jnp.mean(xc * xc, axis=-1, keepdims=True)
    return (xc * lax.rsqrt(var + LN_EPS) * g.astype(jnp.float32) + b.astype(jnp.float32)).astype(x.dtype)


def rel_bucket(q_pos, k_pos):
    n = jnp.maximum(q_pos[:, None] - k_pos[None, :], 0)
    max_exact = REL_BUCKETS // 2
    nf = jnp.maximum(n, 1).astype(jnp.float32)
    large = max_exact + (jnp.log(nf / max_exact) / math.log(REL_MAX_DIST / max_exact)
                         * (REL_BUCKETS - max_exact)).astype(jnp.int32)
    large = jnp.minimum(large, REL_BUCKETS - 1)
    return jnp.where(n < max_exact, n, large)


def diff_attention(x, wq, wk, wv, wo, lq1, lk1, lq2, lk2, subln_g, rel_table, layer_idx):
    b, s, _ = x.shape
    q = (x @ wq).reshape(b, s, DA_HEADS, 2, DA_HEAD_DIM)
    k = (x @ wk).reshape(b, s, DA_HEADS, 2, DA_HEAD_DIM)
    v = (x @ wv).reshape(b, s, DA_HEADS, 2 * DA_HEAD_DIM)
    lam_init = 0.8 - 0.6 * math.exp(-0.3 * layer_idx)
    f32 = jnp.float32
    lam = (jnp.exp(jnp.sum(lq1.astype(f32) * lk1.astype(f32)))
           - jnp.exp(jnp.sum(lq2.astype(f32) * lk2.astype(f32))) + lam_init)
    k_pos = jnp.arange(s)
    scale = DA_HEAD_DIM ** -0.5
    g = subln_g.astype(f32)

    def block(i):
        start = i * BLOCK_Q
        qb = lax.dynamic_slice_in_dim(q, start, BLOCK_Q, axis=1)
        q_pos = start + jnp.arange(BLOCK_Q)
        bias = jnp.transpose(rel_table[rel_bucket(q_pos, k_pos)], (2, 0, 1)).astype(f32)
        logits = jnp.einsum('bqhcd,bkhcd->bhcqk', qb, k).astype(f32) * scale + bias[None, :, None]
        causal = k_pos[None, :] <= q_pos[:, None]
        logits = jnp.where(causal, logits, NEG_INF)
        p = jax.nn.softmax(logits, axis=-1)
        attn = p[:, :, 0] - lam * p[:, :, 1]
        o = jnp.einsum('bhqk,bkhe->bqhe', attn.astype(x.dtype), v).astype(f32)
        o = o * lax.rsqrt(jnp.mean(o * o, axis=-1, keepdims=True) + RMS_EPS) * g
        return (o * (1.0 - lam_init)).astype(x.dtype)

    blocks = lax.map(block, jnp.arange(s // BLOCK_Q))
    o = jnp.moveaxis(blocks, 0, 1).reshape(b, s, DA_HEADS * 2 * DA_HEAD_DIM)
    return o @ wo


def stick_breaking(x, wq, wk, wv, wo):
    b, s, _ = x.shape
    q = (x @ wq).reshape(b, s, SB_HEADS, SB_HEAD_DIM)
    k = (x @ wk).reshape(b, s, SB_HEADS, SB_HEAD_DIM)
    v = (x @ wv).reshape(b, s, SB_HEADS, SB_HEAD_DIM)
    k_pos = jnp.arange(s)
    scale = SB_HEAD_DIM ** -0.5

    def block(i):
        start = i * BLOCK_Q
        qb = lax.dynamic_slice_in_dim(q, start, BLOCK_Q, axis=1)
        q_pos = start + jnp.arange(BLOCK_Q)
        z = jnp.einsum('bqhd,bkhd->bhqk', qb, k).astype(jnp.float32) * scale
        strict = k_pos[None, :] < q_pos[:, None]
        log_fail = jnp.where(strict, jax.nn.log_sigmoid(-z), 0.0)
        later = lax.cumsum(log_fail, axis=3, reverse=True) - log_fail
        w = jnp.where(strict, jnp.exp(jax.nn.log_sigmoid(z) + later), 0.0)
        return jnp.einsum('bhqk,bkhd->bqhd', w.astype(x.dtype), v)

    blocks = lax.map(block, jnp.arange(s // BLOCK_Q))
    o = jnp.moveaxis(blocks, 0, 1).reshape(b, s, SB_HEADS * SB_HEAD_DIM)
    return o @ wo


def hier_moe(x, w_group, b_group, w_expert, b_expert, w1, w3, w2):
    b, s, d = x.shape
    t = b * s
    f32 = jnp.float32
    xt = x.reshape(t, d)
    g_prob = jax.nn.softmax((xt @ w_group + b_group).astype(f32), axis=-1)
    g_gate, g_idx = lax.top_k(g_prob, 1)
    e_logits = (xt @ w_expert + b_expert).astype(f32).reshape(t, MOE_GROUPS, MOE_EXPERTS_PER_GROUP)
    e_logits = jnp.take_along_axis(e_logits, g_idx[:, :, None], axis=1)[:, 0]
    e_prob = jax.nn.softmax(e_logits, axis=-1)
    e_gate, e_idx = lax.top_k(e_prob, MOE_TOP_K)
    e_gate = e_gate / jnp.sum(e_gate, axis=-1, keepdims=True)
    weight = g_gate * e_gate
    expert_id = g_idx * MOE_EXPERTS_PER_GROUP + e_idx
    gates = jnp.sum(jax.nn.one_hot(expert_id, N_EXPERTS, dtype=f32) * weight[..., None], axis=1)

    def step(acc, ew):
        w1e, w3e, w2e, ge = ew
        h = jax.nn.silu(xt @ w1e) * (xt @ w3e)
        return acc + ge[:, None] * (h @ w2e).astype(f32), None

    y, _ = lax.scan(step, jnp.zeros((t, d), f32), (w1, w3, w2, gates.T))
    return y.astype(x.dtype).reshape(b, s, d)


def setup_inputs(seed: int = 0) -> dict:
    key = jax.random.key(seed)
    ks = jax.random.split(key, 26)
    nrm = jax.random.normal
    D = D_MODEL
    da_qk = DA_HEADS * 2 * DA_HEAD_DIM
    da_v = DA_HEADS * 2 * DA_HEAD_DIM
    sb_w = SB_HEADS * SB_HEAD_DIM
    inv = D ** -0.5
    return {
        "x": nrm(ks[0], (BATCH, SEQ, D), jnp.float32),
        "rel_table": 0.5 * nrm(ks[1], (REL_BUCKETS, DA_HEADS), jnp.float32),
        "da_wq": inv * nrm(ks[2], (N_DA_LAYERS, D, da_qk), jnp.float32),
        "da_wk": inv * nrm(ks[3], (N_DA_LAYERS, D, da_qk), jnp.float32),
        "da_wv": DEEPNORM_BETA * inv * nrm(ks[4], (N_DA_LAYERS, D, da_v), jnp.float32),
        "da_wo": DEEPNORM_BETA * da_v ** -0.5 * nrm(ks[5], (N_DA_LAYERS, da_v, D), jnp.float32),
        "da_lq1": 0.1 * nrm(ks[6], (N_DA_LAYERS, DA_HEAD_DIM), jnp.float32),
        "da_lk1": 0.1 * nrm(ks[7], (N_DA_LAYERS, DA_HEAD_DIM), jnp.float32),
        "da_lq2": 0.1 * nrm(ks[8], (N_DA_LAYERS, DA_HEAD_DIM), jnp.float32),
        "da_lk2": 0.1 * nrm(ks[9], (N_DA_LAYERS, DA_HEAD_DIM), jnp.float32),
        "da_subln_g": 1.0 + 0.02 * nrm(ks[10], (N_DA_LAYERS, 2 * DA_HEAD_DIM), jnp.float32),
        "sb_wq": inv * nrm(ks[11], (N_SB_LAYERS, D, sb_w), jnp.float32),
        "sb_wk": inv * nrm(ks[12], (N_SB_LAYERS, D, sb_w), jnp.float32),
        "sb_wv": DEEPNORM_BETA * inv * nrm(ks[13], (N_SB_LAYERS, D, sb_w), jnp.float32),
        "sb_wo": DEEPNORM_BETA * sb_w ** -0.5 * nrm(ks[14], (N_SB_LAYERS, sb_w, D), jnp.float32),
        "ln_mix_g": 1.0 + 0.02 * nrm(ks[15], (DEPTH, D), jnp.float32),
        "ln_mix_b": 0.02 * nrm(ks[16], (DEPTH, D), jnp.float32),
        "ln_ffn_g": 1.0 + 0.02 * nrm(ks[17], (DEPTH, D), jnp.float32),
        "ln_ffn_b": 0.02 * nrm(ks[18], (DEPTH, D), jnp.float32),
        "moe_w_group": inv * nrm(ks[19], (DEPTH, D, MOE_GROUPS), jnp.float32),
        "moe_b_group": 0.01 * nrm(ks[20], (DEPTH, MOE_GROUPS), jnp.float32),
        "moe_w_expert": inv * nrm(ks[21], (DEPTH, D, N_EXPERTS), jnp.float32),
        "moe_b_expert": 0.01 * nrm(ks[22], (DEPTH, N_EXPERTS), jnp.float32),
        "moe_w1": inv * nrm(ks[23], (DEPTH, N_EXPERTS, D, MOE_HIDDEN), jnp.float32),
        "moe_w3": inv * nrm(ks[24], (DEPTH, N_EXPERTS, D, MOE_HIDDEN), jnp.float32),
        "moe_w2": DEEPNORM_BETA * MOE_HIDDEN ** -0.5 * nrm(ks[25], (DEPTH, N_EXPERTS, MOE_HIDDEN, D), jnp.float32),
    }


def reference(x, rel_table, da_wq, da_wk, da_wv, da_wo, da_lq1, da_lk1, da_lq2, da_lk2, da_subln_g,
              sb_wq, sb_wk, sb_wv, sb_wo, ln_mix_g, ln_mix_b, ln_ffn_g, ln_ffn_b,
              moe_w_group, moe_b_group, moe_w_expert, moe_b_expert, moe_w1, moe_w3, moe_w2):
    for layer in range(DEPTH):
        j = layer // N_MIXERS
        if layer % N_MIXERS == 0:
            mix = diff_attention(x, da_wq[j], da_wk[j], da_wv[j], da_wo[j], da_lq1[j], da_lk1[j],
                                 da_lq2[j], da_lk2[j], da_subln_g[j], rel_table, layer)
        else:
            mix = stick_breaking(x, sb_wq[j], sb_wk[j], sb_wv[j], sb_wo[j])
        x = layer_norm(DEEPNORM_ALPHA * x + mix, ln_mix_g[layer], ln_mix_b[layer])
        ffn = hier_moe(x, moe_w_group[layer], moe_b_group[layer], moe_w_expert[layer],
                       moe_b_expert[layer], moe_w1[layer], moe_w3[layer], moe_w2[layer])
        x = layer_norm(DEEPNORM_ALPHA * x + ffn, ln_ffn_g[layer], ln_ffn_b[layer])
    return x
```

```python
import math
from contextlib import ExitStack
import numpy as np
import ml_dtypes
import concourse.bass as bass
import concourse.mybir as mybir
from concourse.bass_utils import run_bass_kernel_spmd

F32 = mybir.dt.float32
BF16 = mybir.dt.bfloat16
AF = mybir.ActivationFunctionType
ALU = mybir.AluOpType
AX = mybir.AxisListType

S = 4096
D = 1024
NT = S // 128
DEPTH = 2
NE = 32
HID = 512
ALPHA = (2 * DEPTH) ** 0.25
LN_EPS = 1e-5
RMS_EPS = 1e-6
GW = 383
SL = 256
NSLOT = 63
NPOS = NSLOT * SL
KMAX = (S + SL - 1) // SL
I32 = mybir.dt.int32
SPARSE = True


class Sched:
    LIMIT = 30000
    NDMA = 24

    def __init__(self, nc):
        self.nc = nc
        self.eng = {"pe": nc.tensor, "act": nc.scalar, "dve": nc.vector, "pool": nc.gpsimd, "sp": nc.sync}
        self.esem = {}
        self.ecnt = {}
        self.nsem = 0
        for e in self.eng:
            self._newsem(e)
        self.seen = {e: {} for e in self.eng}
        self.lastw = {}
        self.readers = {}
        self.dpools = {}

    def _newsem(self, e):
        self.esem[e] = self.nc.alloc_semaphore(f"es_{e}_{self.nsem}")
        self.nsem += 1
        self.ecnt[e] = 0

    def _wait(self, e, dep):
        sem, val, weng = dep
        if weng == e and e == "pe":
            return
        k = sem.num
        if self.seen[e].get(k, 0) >= val:
            return
        self.eng[e].wait_ge(sem, val)
        self.seen[e][k] = val

    def _deps(self, e, R, W):
        for r in R:
            w = self.lastw.get(r)
            if w:
                for tok in w.values():
                    self._wait(e, tok)
        for w_ in W:
            lw = self.lastw.get(w_)
            if lw:
                for tok in lw.values():
                    self._wait(e, tok)
            rd = self.readers.get(w_)
            if rd:
                for tok in rd.values():
                    self._wait(e, tok)

    def _book(self, tok, R, W):
        for r in R:
            self.readers.setdefault(r, {})[tok[0].num] = tok
        for w_ in W:
            self.lastw.setdefault(w_, {})[tok[0].num] = tok
            self.readers[w_] = {}

    def op(self, e, fn, R=(), W=()):
        self._deps(e, R, W)
        if self.ecnt[e] >= self.LIMIT:
            self._newsem(e)
        inst = fn(self.eng[e])
        self.ecnt[e] += 1
        inst.then_inc(self.esem[e], 1)
        self._book((self.esem[e], self.ecnt[e], e), R, W)

    def dma(self, q, fn, R=(), W=(), pool="misc", n=6):
        if pool not in self.dpools:
            self.dpools[pool] = dict(sems=[self.nc.alloc_semaphore(f"dq_{pool}_{i}") for i in range(n)], val=[0] * n, nxt=0)
        P = self.dpools[pool]
        i = P["nxt"]
        P["nxt"] = (i + 1) % len(P["sems"])
        sem = P["sems"][i]
        if P["val"][i] > 0:
            self._wait(q, (sem, P["val"][i], "dma"))
        self._deps(q, R, W)
        inst = fn(self.eng[q])
        P["val"][i] += 16
        inst.then_inc(sem, 16)
        self._book((sem, P["val"][i], "dma"), R, W)

    def _dma_toks(self):
        toks = []
        for P in self.dpools.values():
            for sem, v in zip(P["sems"], P["val"]):
                if v > 0:
                    toks.append((sem, v, "dma"))
        return toks

    def barrier(self):
        toks = [(self.esem[e], self.ecnt[e], "x") for e in self.eng if self.ecnt[e] > 0]
        toks += self._dma_toks()
        for e in self.eng:
            for t in toks:
                self._wait(e, t)
        self.lastw = {}
        self.readers = {}

    def final_wait(self, e="sp"):
        for t in self._dma_toks():
            self._wait(e, t)
        for o in self.eng:
            if o != e and self.ecnt[o] > 0:
                self._wait(e, (self.esem[o], self.ecnt[o], o))


def _consts_host():
    ident = np.eye(128, dtype=np.float32)
    kk = np.arange(128)[:, None]
    qq = np.arange(128)[None, :]
    tri_incl = (kk >= np.arange(128)[None, :]).astype(np.float32)
    tri_low = (kk < np.arange(128)[None, :]).astype(np.float32)
    mask_le = (kk <= qq).astype(np.float32)
    mask_lt = (kk < qq).astype(np.float32)
    n = np.maximum(np.arange(GW) - 127, 0)
    nf = np.maximum(n, 1).astype(np.float32)
    large = 16 + (np.log(nf / np.float32(16)) / np.float32(math.log(128 / 16)) * np.float32(16)).astype(np.int32)
    large = np.minimum(large, 31)
    bucket = np.where(n < 16, n, large)
    oh = (bucket[None, :] == np.arange(32)[:, None]).astype(np.float32)
    bf = ml_dtypes.bfloat16
    return {
        "c_ident": ident.astype(bf), "c_tri_incl": tri_incl.astype(bf), "c_tri_low": tri_low.astype(bf),
        "c_mask_le": mask_le, "c_mask_lt": mask_lt.astype(bf), "c_oh": oh,
        "c_pidx": np.arange(128, dtype=np.float32).reshape(128, 1),
        "c_sidx": np.tile(np.arange(NSLOT, dtype=np.float32)[None, :], (128, 1)),
    }


def build_program(stop_after=None, debug=False):
    nc = bass.Bass("TRN2", target_bir_lowering=False)
    sc = Sched(nc)

    def din(name, shape, dt=F32):
        return nc.dram_tensor(name, list(shape), dt, kind="ExternalInput").ap()

    x_in = din("x", [S, D])
    rel_table = din("rel_table", [32, 8])
    attw = [
        dict(wq=din("da_wq", [D, D]), wk=din("da_wk", [D, D]), wv=din("da_wv", [D, D]), wo=din("da_wo", [D, D])),
        dict(wq=din("sb_wq", [D, D]), wk=din("sb_wk", [D, D]), wv=din("sb_wv", [D, D]), wo=din("sb_wo", [D, D])),
    ]
    da_l = din("da_l", [4, 64])
    da_g = din("da_subln_g", [1, 128])
    ln_mix_g = din("ln_mix_g", [DEPTH, D]); ln_mix_b = din("ln_mix_b", [DEPTH, D])
    ln_ffn_g = din("ln_ffn_g", [DEPTH, D]); ln_ffn_b = din("ln_ffn_b", [DEPTH, D])
    w_group = din("moe_w_group", [DEPTH, D, 4]); b_group = din("moe_b_group", [DEPTH, 4])
    w_expert = din("moe_w_expert", [DEPTH, D, NE]); b_expert = din("moe_b_expert", [DEPTH, NE])
    w1 = din("moe_w1", [DEPTH, NE, D, HID]); w3 = din("moe_w3", [DEPTH, NE, D, HID]); w2 = din("moe_w2", [DEPTH, NE, HID, D])
    c_ident = din("c_ident", [128, 128], BF16); c_tri_incl = din("c_tri_incl", [128, 128], BF16)
    c_tri_low = din("c_tri_low", [128, 128], BF16); c_mask_le = din("c_mask_le", [128, 128])
    c_mask_lt = din("c_mask_lt", [128, 128], BF16); c_oh = din("c_oh", [32, GW])
    c_pidx = din("c_pidx", [128, 1]); c_sidx = din("c_sidx", [128, NSLOT])
    Xs = nc.dram_tensor("Xs", [NPOS, D], BF16, kind="Internal").ap()
    Ys = nc.dram_tensor("Ys", [NPOS, D], F32, kind="Internal").ap()
    Wc = [nc.dram_tensor(f"Wc{i}", [NE * 128, 4096], BF16, kind="Internal").ap() for i in range(3)]

    out = nc.dram_tensor("out", [S, D], F32, kind="ExternalOutput").ap()
    skind = "ExternalOutput" if debug else "Internal"
    xr = [x_in] + [nc.dram_tensor(f"xr{i}", [S, D], F32, kind=skind).ap() for i in (1, 2, 3)] + [out]
    gd = nc.dram_tensor("gd", [8, 128, GW], F32, kind="Internal")

    banks = [nc.alloc_psum_tensor(f"bank{i}", [128, 512], F32) for i in range(8)]

    _uid = [0]

    def sbt(name, shape, dt):
        _uid[0] += 1
        return nc.sbuf_tensor(f"{name}_u{_uid[0]}", shape, dt)

    def bk(i):
        return banks[i].ap()

    def bkh(i):
        return banks[i].bitcast(BF16).ap()

    xT = nc.alloc_sbuf_tensor("xT", [128, 8, S], BF16)
    ident = nc.alloc_sbuf_tensor("ident", [128, 128], BF16)
    tri_incl = nc.alloc_sbuf_tensor("tri_incl", [128, 128], BF16)
    tri_low = nc.alloc_sbuf_tensor("tri_low", [128, 128], BF16)
    mask_le = nc.alloc_sbuf_tensor("mask_le", [128, 128], F32)
    mask_lt = nc.alloc_sbuf_tensor("mask_lt", [128, 128], BF16)
    cst = nc.alloc_sbuf_tensor("cst", [128, 8], F32)
    sc.dma("sp", lambda e: e.dma_start(out=ident.ap(), in_=c_ident), W=["ident"])
    sc.dma("sp", lambda e: e.dma_start(out=tri_incl.ap(), in_=c_tri_incl), W=["tri_incl"])
    sc.dma("sp", lambda e: e.dma_start(out=tri_low.ap(), in_=c_tri_low), W=["tri_low"])
    sc.dma("sp", lambda e: e.dma_start(out=mask_le.ap(), in_=c_mask_le), W=["mask_le"])
    sc.dma("sp", lambda e: e.dma_start(out=mask_lt.ap(), in_=c_mask_lt), W=["mask_lt"])
    sc.op("dve", lambda e: e.memset(cst.ap()[:, 0:1], LN_EPS), W=["cst"])
    sc.op("dve", lambda e: e.memset(cst.ap()[:, 1:2], RMS_EPS), W=["cst"])
    sc.op("dve", lambda e: e.memset(cst.ap()[:, 2:3], 1.0), W=["cst"])

    def make_xT_tile(src_bf, src_res, t, bank):
        for c in range(8):
            sc.op("pe", lambda e, c=c: e.transpose(out=bkh(bank)[:, c * 128:(c + 1) * 128], in_=src_bf[:, c * 128:(c + 1) * 128],
                                                    identity=ident.ap()),
                  R=[src_res, "ident"], W=[f"bank{bank}"])
        sc.op("act", lambda e: e.activation(out=xT.ap()[:, :, t * 128:(t + 1) * 128],
                                            in_=bkh(bank)[:, :].rearrange("p (c f) -> p c f", c=8), func=AF.Copy),
              R=[f"bank{bank}"], W=[("xT", t)])

    def ln_tile(y, yres, gb, stat, t, dst, xbf, lnbank):
        sres = ("stat", stat.name)
        st6 = stat.ap()[:, 0:12].rearrange("p (a b) -> p a b", a=2)
        for hh in range(2):
            sc.op("dve", lambda e, hh=hh: e.bn_stats(out=st6[:, hh, :], in_=y[:, hh * 512:(hh + 1) * 512]), R=[yres], W=[sres])
        sc.op("dve", lambda e: e.bn_aggr(out=stat.ap()[:, 12:14], in_=stat.ap()[:, 0:12]), R=[sres], W=[sres])
        sc.op("act", lambda e: e.activation(out=stat.ap()[:, 14:15], in_=stat.ap()[:, 13:14], func=AF.Ln, bias=float(LN_EPS), scale=1.0),
              R=[sres], W=[sres])
        sc.op("act", lambda e: e.activation(out=stat.ap()[:, 15:16], in_=stat.ap()[:, 14:15], func=AF.Exp, scale=-0.5), R=[sres], W=[sres])
        sc.op("dve", lambda e: e.tensor_scalar(out=stat.ap()[:, 16:17], in0=stat.ap()[:, 12:13], scalar1=stat.ap()[:, 15:16], scalar2=-1.0,
                                               op0=ALU.mult, op1=ALU.mult), R=[sres], W=[sres])
        sc.op("act", lambda e: e.activation(out=y, in_=y, func=AF.Identity, bias=stat.ap()[:, 16:17], scale=stat.ap()[:, 15:16]), R=[yres, sres], W=[yres])
        sc.op("dve", lambda e: e.tensor_tensor(out=y, in0=y, in1=gb.ap()[:, 0, :], op=ALU.mult), R=[yres, "gb0"], W=[yres])
        sc.op("dve", lambda e: e.tensor_tensor(out=y, in0=y, in1=gb.ap()[:, 1, :], op=ALU.add), R=[yres, "gb1"], W=[yres])
        sc.dma("sp", lambda e: e.dma_start(out=dst[t * 128:(t + 1) * 128, :], in_=y), R=[yres], W=[], pool="st", n=4)
        if xbf is not None:
            xb, xbres = xbf
            sc.op("act", lambda e: e.activation(out=xb, in_=y, func=AF.Copy), R=[yres], W=[xbres])
            make_xT_tile(xb, xbres, t, lnbank)

    def ln_s1(y, yres, stat):
        sres = ("stat", stat.name)
        st6 = stat.ap()[:, 0:12].rearrange("p (a b) -> p a b", a=2)
        for hh in range(2):
            sc.op("dve", lambda e, hh=hh: e.bn_stats(out=st6[:, hh, :], in_=y[:, hh * 512:(hh + 1) * 512]), R=[yres], W=[sres])
        sc.op("dve", lambda e: e.bn_aggr(out=stat.ap()[:, 12:14], in_=stat.ap()[:, 0:12]), R=[sres], W=[sres])

    def ln_s2(y, yres, stat):
        sres = ("stat", stat.name)
        sc.op("act", lambda e: e.activation(out=stat.ap()[:, 14:15], in_=stat.ap()[:, 13:14], func=AF.Ln, bias=float(LN_EPS), scale=1.0),
              R=[sres], W=[sres])
        sc.op("act", lambda e: e.activation(out=stat.ap()[:, 15:16], in_=stat.ap()[:, 14:15], func=AF.Exp, scale=-0.5), R=[sres], W=[sres])
        sc.op("dve", lambda e: e.tensor_scalar(out=stat.ap()[:, 16:17], in0=stat.ap()[:, 12:13], scalar1=stat.ap()[:, 15:16], scalar2=-1.0,
                                               op0=ALU.mult, op1=ALU.mult), R=[sres], W=[sres])
        sc.op("act", lambda e: e.activation(out=y, in_=y, func=AF.Identity, bias=stat.ap()[:, 16:17], scale=stat.ap()[:, 15:16]), R=[yres, sres], W=[yres])

    def ln_s3(y, yres, gb, t, dst, xbf):
        sc.op("dve", lambda e: e.tensor_tensor(out=y, in0=y, in1=gb.ap()[:, 0, :], op=ALU.mult), R=[yres, "gb0"], W=[yres])
        sc.op("dve", lambda e: e.tensor_tensor(out=y, in0=y, in1=gb.ap()[:, 1, :], op=ALU.add), R=[yres, "gb1"], W=[yres])
        sc.dma("sp", lambda e: e.dma_start(out=dst[t * 128:(t + 1) * 128, :], in_=y), R=[yres], W=[], pool="st", n=4)
        if xbf is not None:
            xb, xbres = xbf
            sc.op("act", lambda e: e.activation(out=xb, in_=y, func=AF.Copy), R=[yres], W=[xbres])

    def ln_pipeline(pre, produce_a, produce, yb, ybres, stat, gb, dst, xbf):
        NB = len(yb)
        for i in range(-5, NT + 2):
            if pre is not None and 0 <= i + 5 < NT:
                pre(i + 5)
            if produce_a is not None and 0 <= i + 3 < NT:
                produce_a(i + 3)
            if 0 <= i + 1 < NT:
                t = i + 1
                ln_s2(yb[t % NB].ap(), ybres(t % NB), stat[t % NB])
            if 0 <= i + 2 < NT:
                t = i + 2
                produce(t)
                ln_s1(yb[t % NB].ap(), ybres(t % NB), stat[t % NB])
            if 0 <= i < NT:
                t = i
                ln_s3(yb[t % NB].ap(), ybres(t % NB), gb, t, dst, None if xbf is None else (xbf[t % NB].ap(), f"xbf{t % NB}"))
            if xbf is not None and 0 <= i - 1 < NT:
                t = i - 1
                make_xT_tile(xbf[t % NB].ap(), f"xbf{t % NB}", t, 6 + (t % 2))

    def load_gb(gb, g_ap, b_ap, layer):
        sc.dma("sp", lambda e: e.dma_start(out=gb.ap()[:, 0, :], in_=g_ap[layer:layer + 1, :].partition_broadcast(128)), W=["gb0"])
        sc.dma("sp", lambda e: e.dma_start(out=gb.ap()[:, 1, :], in_=b_ap[layer:layer + 1, :].partition_broadcast(128)), W=["gb1"])

    with ExitStack() as es:
        xld = [es.enter_context(sbt(f"xld{i}", [128, D], BF16)) for i in range(2)]
        for t in range(NT):
            b = t % 2
            sc.dma("pool", lambda e, b=b, t=t: e.dma_start(out=xld[b].ap(), in_=x_in[t * 128:(t + 1) * 128, :]), W=[f"xld{b}"])
            make_xT_tile(xld[b].ap(), f"xld{b}", t, 6 + b)
        sc.barrier()

    def attention_layer(layer):
        kind = "diff" if layer % 2 == 0 else "sb"
        W_ = attw[layer % 2]
        VW = 130 if kind == "diff" else 128
        with ExitStack() as es:
            oT = es.enter_context(sbt("oT", [128, 8, S], BF16))
            with ExitStack() as es2:
                ent = es2.enter_context
                qT = ent(sbt("qT", [128, 2, S], BF16))
                kT = ent(sbt("kT", [128, S], BF16))
                V = ent(sbt("V", [128, NT, VW], BF16))
                wqkv = [ent(sbt(f"wqkv{i}", [128, 3, 8, 128], BF16)) for i in range(2)]
                NB = 4
                if kind == "diff":
                    Pt = [ent(sbt(f"Pt{i}", [128, 512], BF16)) for i in range(NB)]
                    Ep = ent(sbt("Ep", [128, 8, 2, 128], F32))
                    gtmp = ent(sbt("gtmp", [128, GW], F32))
                    rb = ent(sbt("rb", [32, 128], F32))
                    rtab = ent(sbt("rtab", [32, 8], F32))
                    oh = ent(sbt("oh", [32, GW], F32))
                    ones32 = ent(sbt("ones32", [32, 128], F32))
                    cfar = ent(sbt("cfar", [128, 16], F32))
                    lam = ent(sbt("lam", [128, 8], F32))
                    lvec = ent(sbt("lvec", [128, 4, 64], F32))
                    gsub = ent(sbt("gsub", [128, 128], F32))
                    R1 = ent(sbt("R1", [128, 4, 128], F32))
                    ot = [ent(sbt(f"ot{i}", [128, 128], F32)) for i in range(4)]
                    sm = [ent(sbt(f"sm{i}", [128, 8], F32)) for i in range(4)]
                    junk = ent(sbt("junk", [128, 128], F32))
                else:
                    eb = [ent(sbt(f"eb{i}", [128, 512], BF16)) for i in range(NB)]
                    spb = [ent(sbt(f"spb{i}", [128, 512], BF16)) for i in range(NB)]
                    gb_ = [ent(sbt(f"gb_{i}", [128, 512], BF16)) for i in range(NB)]
                    wb = [ent(sbt(f"wb{i}", [128, 512], BF16)) for i in range(NB)]
                onb = [ent(sbt(f"onb{i}", [128, 128], BF16)) for i in range(4)]

                sc.op("pool", lambda e: e.memset(qT.ap()[:, 0, :], 0.0), W=["qT"])
                sc.op("dve", lambda e: e.memset(qT.ap()[:, 1, :], 0.0), W=["qT"])
                lam_init = 0.8 - 0.6 * math.exp(-0.3 * layer)
                if kind == "diff":
                    sc.dma("sp", lambda e: e.dma_start(out=lvec.ap(), in_=da_l.partition_broadcast(128)), W=["lvec"])
                    sc.dma("sp", lambda e: e.dma_start(out=gsub.ap(), in_=da_g[0:1, :].partition_broadcast(128)), W=["gsub"])
                    sc.op("dve", lambda e: e.tensor_scalar(out=gsub.ap(), in0=gsub.ap(), scalar1=float(1.0 - lam_init), scalar2=None, op0=ALU.mult),
                          R=["gsub"], W=["gsub"])
                    sc.op("dve", lambda e: e.tensor_tensor(out=lvec.ap()[:, 0, :], in0=lvec.ap()[:, 0, :], in1=lvec.ap()[:, 1, :], op=ALU.mult), R=["lvec"], W=["lvec"])
                    sc.op("dve", lambda e: e.tensor_tensor(out=lvec.ap()[:, 2, :], in0=lvec.ap()[:, 2, :], in1=lvec.ap()[:, 3, :], op=ALU.mult), R=["lvec"], W=["lvec"])
                    sc.op("dve", lambda e: e.tensor_reduce(out=lam.ap()[:, 0:1], in_=lvec.ap()[:, 0, :], axis=AX.X, op=ALU.add), R=["lvec"], W=["lam"])
                    sc.op("dve", lambda e: e.tensor_reduce(out=lam.ap()[:, 1:2], in_=lvec.ap()[:, 2, :], axis=AX.X, op=ALU.add), R=["lvec"], W=["lam"])
                    sc.op("act", lambda e: e.activation(out=lam.ap()[:, 2:4], in_=lam.ap()[:, 0:2], func=AF.Exp), R=["lam"], W=["lam"])
                    sc.op("dve", lambda e: e.scalar_tensor_tensor(out=lam.ap()[:, 4:5], in0=lam.ap()[:, 3:4], scalar=float(-lam_init), in1=lam.ap()[:, 2:3],
                                                                  op0=ALU.add, op1=ALU.subtract), R=["lam"], W=["lam"])
                    sc.dma("sp", lambda e: e.dma_start(out=rtab.ap(), in_=rel_table), W=["rtab"])
                    sc.dma("sp", lambda e: e.dma_start(out=oh.ap(), in_=c_oh), W=["oh"])
                    sc.dma("sp", lambda e: e.dma_start(out=cfar.ap()[:, 0:8], in_=rel_table[31:32, :].partition_broadcast(128)), W=["cfar"])
                    sc.op("dve", lambda e: e.tensor_scalar(out=cfar.ap()[:, 8:16], in0=cfar.ap()[:, 0:8], scalar1=-1.0, scalar2=None, op0=ALU.mult), R=["cfar"], W=["cfar"])
                    sc.op("dve", lambda e: e.memset(ones32.ap(), 1.0), W=["ones32"])
                    for h in range(8):
                        sc.op("dve", lambda e, h=h: e.tensor_scalar(out=rb.ap(), in0=ones32.ap(), scalar1=rtab.ap()[:, h:h + 1], scalar2=None, op0=ALU.mult),
                              R=["ones32", "rtab"], W=["rb"])
                        sc.op("pe", lambda e: e.matmul(bk(6)[:, 0:GW], lhsT=rb.ap(), rhs=oh.ap(), start=True, stop=True), R=["rb", "oh"], W=["bank6"])
                        sc.op("act", lambda e: e.activation(out=gtmp.ap(), in_=bk(6)[:, 0:GW], func=AF.Copy), R=["bank6"], W=["gtmp"])
                        sc.dma("sp", lambda e, h=h: e.dma_start(out=gd.ap()[h], in_=gtmp.ap()), R=["gtmp"], W=["gd"])
                        for Dd in range(2):
                            src = bass.AP(tensor=gd.ap().tensor, offset=h * 128 * GW + Dd * 128 + 127, ap=[[GW - 1, 128], [1, 128]])
                            sc.dma("sp", lambda e, h=h, Dd=Dd, src=src: e.dma_start(out=Ep.ap()[:, h, Dd, :], in_=src), R=["gd"], W=["Ep"])
                            sc.op("act", lambda e, h=h, Dd=Dd: e.activation(out=Ep.ap()[:, h, Dd, :], in_=Ep.ap()[:, h, Dd, :], func=AF.Exp,
                                                                             bias=cfar.ap()[:, 8 + h:9 + h], scale=1.0), R=["Ep", "cfar"], W=["Ep"])
                        sc.op("dve", lambda e, h=h: e.tensor_tensor(out=Ep.ap()[:, h, 0, :], in0=Ep.ap()[:, h, 0, :], in1=mask_le.ap(), op=ALU.mult),
                              R=["Ep", "mask_le"], W=["Ep"])
                    sc.op("dve", lambda e: e.memset(V.ap()[:, :, 128:130], 1.0), W=["Vones"])

                def load_w(j):
                    b = j % 2
                    for i, nm in enumerate(("wq", "wk", "wv")):
                        src = W_[nm].rearrange("(c p) f -> p c f", p=128)[:, :, j * 128:(j + 1) * 128]
                        sc.dma("pool", lambda e, i=i, src=src, b=b: e.dma_start(out=wqkv[b].ap()[:, i, :, :], in_=src), W=[(f"wqkv{b}", i)], pool="wqkv", n=6)

                def project(j):
                    b = j % 2
                    wres = f"wqkv{b}"
                    for which, dstT, scale in ((0, qT, 0.125), (1, kT, None)):
                        for tc in range(8):
                            pb = 6 + (tc % 2)
                            for c in range(8):
                                sc.op("pe", lambda e, c=c, tc=tc, pb=pb, which=which: e.matmul(
                                    bk(pb)[:, :], lhsT=wqkv[b].ap()[:, which, c, :], rhs=xT.ap()[:, c, tc * 512:(tc + 1) * 512],
                                    start=(c == 0), stop=(c == 7)),
                                    R=[(wres, which)] + [("xT", t) for t in range(4 * tc, 4 * tc + 4)], W=[f"bank{pb}"])
                            if scale is not None:
                                sc.op("act", lambda e, tc=tc, pb=pb: e.activation(out=qT.ap()[0:64, 0, tc * 512:(tc + 1) * 512], in_=bk(pb)[0:64, :], func=AF.Copy, scale=scale),
                                      R=[f"bank{pb}"], W=["qT"])
                                sc.op("dve", lambda e, tc=tc, pb=pb: e.tensor_scalar(out=qT.ap()[64:128, 1, tc * 512:(tc + 1) * 512], in0=bk(pb)[64:128, :], scalar1=float(scale), scalar2=None,
                                                                                  op0=ALU.mult), R=[f"bank{pb}"], W=["qT"])
                            else:
                                sc.op("dve", lambda e, tc=tc, pb=pb: e.tensor_copy(out=dstT.ap()[:, tc * 512:(tc + 1) * 512], in_=bk(pb)[:, :]),
                                      R=[f"bank{pb}"], W=["kT"])
                    for tg in range(8):
                        pb = 6 + (tg % 2)
                        for tt in range(4):
                            t = 4 * tg + tt
                            for c in range(8):
                                sc.op("pe", lambda e, c=c, t=t, tt=tt, pb=pb: e.matmul(
                                    bk(pb)[:, tt * 128:(tt + 1) * 128], lhsT=xT.ap()[:, c, t * 128:(t + 1) * 128], rhs=wqkv[b].ap()[:, 2, c, :],
                                    start=(c == 0), stop=(c == 7), skip_group_check=True),
                                    R=[(wres, 2), ("xT", t)], W=[f"bank{pb}"])
                        sc.op("act" if tg % 2 == 0 else "dve",
                              lambda e, tg=tg, pb=pb: (e.activation(out=V.ap()[:, 4 * tg:4 * tg + 4, 0:128], in_=bk(pb)[:, :].rearrange("p (a f) -> p a f", a=4), func=AF.Copy)
                                                       if tg % 2 == 0 else
                                                       e.tensor_copy(out=V.ap()[:, 4 * tg:4 * tg + 4, 0:128], in_=bk(pb)[:, :].rearrange("p (a f) -> p a f", a=4))),
                              R=[f"bank{pb}"], W=["V"])

                def finish_tile(j, t, src_bf, src_res):
                    sc.op("pe", lambda e: e.transpose(out=bkh(7)[:, 0:128], in_=src_bf, identity=ident.ap()), R=[src_res, "ident"], W=["bank7"])
                    sc.op("dve", lambda e: e.tensor_copy(out=oT.ap()[:, j, t * 128:(t + 1) * 128], in_=bkh(7)[:, 0:128]), R=["bank7"], W=[("oT", t)])

                def attn_diff(j):
                    units = []
                    for qc in range(8):
                        for c in range(2):
                            for kt in range(4 * qc + 4):
                                units.append((qc, c, kt))
                    n = len(units)
                    gidx = {}
                    for (qc, c, kt) in units:
                        gidx.setdefault((qc, c), len(gidx))

                    def stage_A(u):
                        qc, c, kt = units[u]
                        qlo = max(0, kt - 4 * qc)
                        zb = (0, 1, 6)[u % 3]
                        sc.op("pe", lambda e: e.matmul(bk(zb)[:, qlo * 128:512], lhsT=kT.ap()[:, kt * 128:(kt + 1) * 128],
                                                       rhs=qT.ap()[:, c, qc * 512 + qlo * 128:(qc + 1) * 512], start=True, stop=True),
                              R=["qT", "kT"], W=[f"bank{zb}"])

                    def stage_B(u):
                        qc, c, kt = units[u]
                        qlo = max(0, kt - 4 * qc)
                        zb = (0, 1, 6)[u % 3]
                        pb = u % NB
                        sc.op("act", lambda e: e.activation(out=Pt[pb].ap()[:, qlo * 128:512], in_=bk(zb)[:, qlo * 128:512], func=AF.Exp),
                              R=[f"bank{zb}"], W=[f"Pt{pb}"])
                        for Dd in range(2):
                            ql = kt + Dd - 4 * qc
                            if 0 <= ql <= 3:
                                sc.op("dve", lambda e, ql=ql, Dd=Dd: e.tensor_tensor(out=Pt[pb].ap()[:, ql * 128:(ql + 1) * 128], in0=Pt[pb].ap()[:, ql * 128:(ql + 1) * 128],
                                                                                    in1=Ep.ap()[:, j, Dd, :], op=ALU.mult),
                                      R=[f"Pt{pb}", "Ep"], W=[f"Pt{pb}"])

                    def stage_H(u):
                        qc, c, kt = units[u]
                        qlo = max(0, kt - 4 * qc)
                        pb = u % NB
                        g = gidx[(qc, c)]
                        ob = 2 + 2 * (g % 2)
                        for ql in range(qlo, 4):
                            bank = ob + ql // 2
                            col = (ql % 2) * 256
                            sc.op("pe", lambda e, ql=ql, bank=bank, col=col: e.matmul(
                                bk(bank)[:, col:col + 129], lhsT=Pt[pb].ap()[:, ql * 128:(ql + 1) * 128], rhs=V.ap()[:, kt, 0:129],
                                start=(kt == 0 and ql % 2 == 0), stop=(kt == 4 * qc + ql), skip_group_check=True),
                                R=[f"Pt{pb}", "V", "Vones"], W=[f"bank{bank}"])
                        if kt == 4 * qc + 3:
                            for ql in range(4):
                                pending.append((u + 1 + ql, (lambda ql=ql, c=c, qc=qc, ob=ob: evac_chain(ql, c, qc, ob))))

                    def evac_chain(ql, c, qc, ob):
                        bank = ob + ql // 2
                        col = (ql % 2) * 256
                        s_ = sm[ql]
                        sres = f"sm{ql}"
                        sc.op("dve", lambda e: e.reciprocal(out=s_.ap()[:, 0:1], in_=bk(bank)[:, col + 128:col + 129]), R=[f"bank{bank}"], W=[sres])
                        if c == 0:
                            sc.op("dve", lambda e: e.tensor_scalar(out=R1.ap()[:, ql, :], in0=bk(bank)[:, col:col + 128], scalar1=s_.ap()[:, 0:1], scalar2=None, op0=ALU.mult),
                                  R=[f"bank{bank}", sres], W=[("R1", ql)])
                            return
                        o_ = ot[ql]
                        ores = f"ot{ql}"
                        nb_ = onb[ql]
                        nres = f"onb{ql}"
                        sc.op("dve", lambda e: e.tensor_tensor(out=s_.ap()[:, 1:2], in0=s_.ap()[:, 0:1], in1=lam.ap()[:, 4:5], op=ALU.mult), R=[sres, "lam"], W=[sres])
                        sc.op("dve", lambda e: e.scalar_tensor_tensor(out=o_.ap(), in0=bk(bank)[:, col:col + 128], scalar=s_.ap()[:, 1:2], in1=R1.ap()[:, ql, :],
                                                                      op0=ALU.mult, op1=ALU.add), R=[f"bank{bank}", sres, ("R1", ql)], W=[ores])
                        sc.op("act", lambda e: e.activation(out=junk.ap(), in_=o_.ap(), func=AF.Square, accum_out=s_.ap()[:, 2:3]), R=[ores], W=[sres, "junk"])
                        sc.op("act", lambda e: e.activation(out=s_.ap()[:, 3:4], in_=s_.ap()[:, 2:3], func=AF.Ln, bias=float(RMS_EPS), scale=1.0 / 128.0),
                              R=[sres, "cst"], W=[sres])
                        sc.op("act", lambda e: e.activation(out=s_.ap()[:, 4:5], in_=s_.ap()[:, 3:4], func=AF.Exp, scale=-0.5), R=[sres], W=[sres])
                        def part2():
                            sc.op("dve", lambda e: e.scalar_tensor_tensor(out=nb_.ap(), in0=o_.ap(), scalar=s_.ap()[:, 4:5], in1=gsub.ap(), op0=ALU.mult, op1=ALU.mult),
                                  R=[ores, sres, "gsub"], W=[nres])
                            pending.append((cur[0] + 2, (lambda: finish_tile(j, 4 * qc + ql, nb_.ap(), nres))))
                        pending.append((cur[0] + 2, part2))

                    pending = []
                    cur = [0]

                    def run_pending(i, flush=False):
                        k = 0
                        while k < len(pending):
                            if flush or pending[k][0] <= i:
                                fn = pending.pop(k)[1]
                                fn()
                            else:
                                k += 1

                    for i in range(-3, n):
                        cur[0] = i
                        if 0 <= i + 3 < n:
                            stage_A(i + 3)
                        if 0 <= i + 2 < n:
                            stage_B(i + 2)
                        if 0 <= i < n:
                            stage_H(i)
                        run_pending(i)
                    cur[0] = n
                    while pending:
                        run_pending(n, flush=True)

                def attn_sb(j):
                    units = []
                    for qc in range(8):
                        for kt in range(4 * qc + 3, -1, -1):
                            for c in range(2):
                                units.append((qc, c, kt))
                    n = len(units)

                    def geo(u):
                        qc, c, kt = units[u]
                        return qc, c, kt, max(0, kt - 4 * qc)

                    def stage_A(u):
                        qc, c, kt, qlo = geo(u)
                        zb = (0, 1, 6)[u % 3]
                        sc.op("pe", lambda e: e.matmul(bk(zb)[:, qlo * 128:512], lhsT=kT.ap()[:, kt * 128:(kt + 1) * 128],
                                                       rhs=qT.ap()[:, c, qc * 512 + qlo * 128:(qc + 1) * 512], start=True, stop=True),
                              R=["qT", "kT"], W=[f"bank{zb}"])

                    def stage_B(u):
                        qc, c, kt, qlo = geo(u)
                        zb = (0, 1, 6)[u % 3]
                        b = u % NB
                        sc.op("act", lambda e: e.activation(out=eb[b].ap()[:, qlo * 128:512], in_=bk(zb)[:, qlo * 128:512], func=AF.Exp),
                              R=[f"bank{zb}"], W=[f"eb{b}"])
                        if kt >= 4 * qc:
                            sc.op("dve", lambda e: e.tensor_tensor(out=eb[b].ap()[:, qlo * 128:(qlo + 1) * 128], in0=eb[b].ap()[:, qlo * 128:(qlo + 1) * 128],
                                                                  in1=mask_lt.ap(), op=ALU.mult), R=[f"eb{b}", "mask_lt"], W=[f"eb{b}"])

                    def stage_C(u):
                        qc, c, kt, qlo = geo(u)
                        b = u % NB
                        sc.op("act", lambda e: e.activation(out=spb[b].ap()[:, qlo * 128:512], in_=eb[b].ap()[:, qlo * 128:512], func=AF.Ln, bias=1.0, scale=1.0),
                              R=[f"eb{b}"], W=[f"spb{b}"])

                    def stage_D(u):
                        qc, c, kt, qlo = geo(u)
                        b = u % NB
                        cb = 2 + c
                        sc.op("pe", lambda e: e.matmul(bk(cb)[:, qlo * 128:512], lhsT=tri_incl.ap(), rhs=spb[b].ap()[:, qlo * 128:512],
                                                       start=(kt == 4 * qc + 3), stop=False, skip_group_check=True),
                              R=[f"spb{b}", "tri_incl"], W=[f"bank{cb}"])

                    def stage_E(u):
                        qc, c, kt, qlo = geo(u)
                        b = u % NB
                        cb = 2 + c
                        sc.op("act", lambda e: e.activation(out=gb_[b].ap()[:, qlo * 128:512], in_=bk(cb)[:, qlo * 128:512], func=AF.Exp, scale=-1.0),
                              R=[f"bank{cb}"], W=[f"gb_{b}"])

                    def stage_F(u):
                        qc, c, kt, qlo = geo(u)
                        b = u % NB
                        cb = 2 + c
                        if kt == 0:
                            return
                        sc.op("pe", lambda e: e.matmul(bk(cb)[:, qlo * 128:512], lhsT=tri_low.ap(), rhs=spb[b].ap()[:, qlo * 128:512],
                                                       start=False, stop=False, skip_group_check=True),
                              R=[f"spb{b}", "tri_low"], W=[f"bank{cb}"])

                    def stage_G(u):
                        qc, c, kt, qlo = geo(u)
                        b = u % NB
                        sc.op("dve", lambda e: e.tensor_tensor(out=wb[b].ap()[:, qlo * 128:512], in0=eb[b].ap()[:, qlo * 128:512], in1=gb_[b].ap()[:, qlo * 128:512], op=ALU.mult),
                              R=[f"eb{b}", f"gb_{b}"], W=[f"wb{b}"])

                    def stage_H(u):
                        qc, c, kt, qlo = geo(u)
                        b = u % NB
                        ob = 4 + (qc % 2)
                        for ql in range(qlo, 4):
                            col = (c * 4 + ql) * 64
                            sc.op("pe", lambda e, ql=ql, col=col: e.matmul(
                                bk(ob)[:, col:col + 64], lhsT=wb[b].ap()[:, ql * 128:(ql + 1) * 128], rhs=V.ap()[:, kt, c * 64:(c + 1) * 64],
                                start=(kt == 4 * qc + 3 and c == 0), stop=(kt == 0), skip_group_check=True),
                                R=[f"wb{b}", "V"], W=[f"bank{ob}"])
                        if kt == 0 and c == 1:
                            for ql in range(4):
                                pending.append((cur[0] + 1 + ql, (lambda ql=ql, qc=qc, ob=ob: evac_sb(ql, qc, ob))))

                    def evac_sb(ql, qc, ob):
                        nb_ = onb[ql]
                        nres = f"onb{ql}"
                        src = bass.AP(tensor=bk(ob).tensor, offset=bk(ob).offset + ql * 64, ap=[list(bk(ob).ap[0]), [256, 2], [1, 64]])
                        sc.op("dve", lambda e: e.tensor_copy(out=nb_.ap().rearrange("p (a f) -> p a f", a=2), in_=src), R=[f"bank{ob}"], W=[nres])
                        pending.append((cur[0] + 2, (lambda: finish_tile(j, 4 * qc + ql, nb_.ap(), nres))))

                    pending = []
                    cur = [0]

                    def run_pending(i, flush=False):
                        k = 0
                        while k < len(pending):
                            if flush or pending[k][0] <= i:
                                fn = pending.pop(k)[1]
                                fn()
                            else:
                                k += 1

                    for i in range(-4, n + 1):
                        cur[0] = i
                        if 0 <= i + 3 < n:
                            stage_A(i + 3)
                        if 0 <= i + 2 < n:
                            stage_B(i + 2)
                        if 0 <= i + 1 < n:
                            stage_D(i + 1)
                            stage_E(i + 1)
                        if 0 <= i + 2 < n:
                            stage_C(i + 2)
                        if 0 <= i < n:
                            stage_F(i)
                            stage_G(i)
                        if 0 <= i - 1 < n:
                            stage_H(i - 1)
                        run_pending(i)
                    cur[0] = n + 1
                    while pending:
                        run_pending(n + 1, flush=True)

                def convert_experts(j):
                    for ei in range(4 * j, 4 * j + 4):
                        for wi, (wsrc, nch) in enumerate(((w1, 8), (w3, 8), (w2, 4))):
                            dstv = Wc[wi][ei * 128:(ei + 1) * 128, :].rearrange("p (c h) -> p c h", c=nch)
                            srcv = wsrc[layer, ei].rearrange("(c p) h -> p c h", p=128)
                            sc.dma("pool", lambda e, dstv=dstv, srcv=srcv: e.dma_start(out=dstv, in_=srcv), R=[("oT", NT - 1)], W=[("Wc", wi, ei)], pool="cv", n=8)

                load_w(0)
                for j in range(8):
                    if j + 1 < 8:
                        load_w(j + 1)
                    project(j)
                    if kind == "diff":
                        attn_diff(j)
                    else:
                        attn_sb(j)
                    if j < 7:
                        convert_experts(j)
                    if j == 6:
                        convert_experts(7)
                sc.barrier()

            with ExitStack() as es3:
                ent = es3.enter_context
                wo = ent(sbt("wo", [128, 8, D], BF16))
                gb = ent(sbt("gb", [128, 2, D], F32))
                NBW = 4
                stat = [ent(sbt(f"stat{i}", [128, 20], F32)) for i in range(NBW)]
                xres = [ent(sbt(f"xres{i}", [128, D], F32)) for i in range(NBW)]
                yb = [ent(sbt(f"yb{i}", [128, D], F32)) for i in range(NBW)]
                xbf = [ent(sbt(f"xbf{i}", [128, D], BF16)) for i in range(NBW)]
                for c in range(8):
                    sc.dma("pool", lambda e, c=c: e.dma_start(out=wo.ap()[:, c, :], in_=W_["wo"][c * 128:(c + 1) * 128, :]), W=[("wo", c)], pool="wo", n=8)
                load_gb(gb, ln_mix_g, ln_mix_b, layer)
                src = xr[2 * layer]
                dst = xr[2 * layer + 1]
                def wo_produce_a(t):
                    b = t % NBW
                    sc.dma("sp", lambda e: e.dma_start(out=xres[b].ap(), in_=src[t * 128:(t + 1) * 128, :]), W=[f"xres{b}"], pool="xr", n=4)
                    for hh in range(2):
                        pb = 2 * (t % 2) + hh
                        for jj in range(8):
                            sc.op("pe", lambda e, jj=jj, hh=hh, pb=pb: e.matmul(
                                bk(pb)[:, :], lhsT=oT.ap()[:, jj, t * 128:(t + 1) * 128], rhs=wo.ap()[:, jj, hh * 512:(hh + 1) * 512],
                                start=(jj == 0), stop=(jj == 7)), R=[("oT", t), ("wo", jj)], W=[f"bank{pb}"])

                def wo_produce(t):
                    b = t % NBW
                    for hh in range(2):
                        pb = 2 * (t % 2) + hh
                        sc.op("dve", lambda e, hh=hh, pb=pb: e.scalar_tensor_tensor(
                            out=yb[b].ap()[:, hh * 512:(hh + 1) * 512], in0=xres[b].ap()[:, hh * 512:(hh + 1) * 512], scalar=float(ALPHA),
                            in1=bk(pb)[:, :], op0=ALU.mult, op1=ALU.add), R=[f"xres{b}", f"bank{pb}"], W=[f"yb{b}"])

                ln_pipeline(None, wo_produce_a, wo_produce, yb, (lambda b: f"yb{b}"), stat, gb, dst, xbf)
                sc.barrier()

    def moe_layer(layer):
        with ExitStack() as es:
            ent = es.enter_context
            TG = 8
            acc = ent(sbt("acc", [128, TG, D], F32))
            wA = [ent(sbt(f"wA{i}", [128, 2, 8, HID], BF16)) for i in range(2)]
            wB = [ent(sbt(f"wB{i}", [128, 4, D], BF16)) for i in range(2)]
            sl = [ent(sbt(f"sl{i}", [128, 4, 512], BF16)) for i in range(2)]
            gg = [ent(sbt(f"gg{i}", [128, 4, 512], BF16)) for i in range(2)]
            wr = ent(sbt("wr", [128, 8, 36], BF16))
            br = ent(sbt("br", [128, 36], F32))
            lg = ent(sbt("lg", [128, NT, 36], F32))
            gates = ent(sbt("gates", [128, NT, NE], F32))
            rt = ent(sbt("rt", [128, 8, 64], F32))
            gb = ent(sbt("gb", [128, 2, D], F32))
            stat = ent(sbt("stat", [128, 20], F32))
            xres = [ent(sbt(f"xres{i}", [128, D], F32)) for i in range(2)]
            xbf = [ent(sbt(f"xbf{i}", [128, D], BF16)) for i in range(2)]

            sc.dma("pool", lambda e: e.dma_start(out=wr.ap()[:, :, 0:4], in_=w_group[layer].rearrange("(c p) g -> p c g", p=128)), W=["wr"])
            sc.dma("pool", lambda e: e.dma_start(out=wr.ap()[:, :, 4:36], in_=w_expert[layer].rearrange("(c p) g -> p c g", p=128)), W=["wr"])
            sc.dma("sp", lambda e: e.dma_start(out=br.ap()[:, 0:4], in_=b_group[layer:layer + 1, :].partition_broadcast(128)), W=["br"])
            sc.dma("sp", lambda e: e.dma_start(out=br.ap()[:, 4:36], in_=b_expert[layer:layer + 1, :].partition_broadcast(128)), W=["br"])
            load_gb(gb, ln_ffn_g, ln_ffn_b, layer)
            for t in range(NT):
                pb = 6 + (t % 2)
                for c in range(8):
                    sc.op("pe", lambda e, c=c, t=t, pb=pb: e.matmul(bk(pb)[:, 0:36], lhsT=xT.ap()[:, c, t * 128:(t + 1) * 128], rhs=wr.ap()[:, c, :],
                                                                    start=(c == 0), stop=(c == 7)), R=[("xT", t), "wr"], W=[f"bank{pb}"])
                sc.op("dve", lambda e, t=t, pb=pb: e.tensor_tensor(out=lg.ap()[:, t, :], in0=bk(pb)[:, 0:36], in1=br.ap(), op=ALU.add),
                      R=[f"bank{pb}", "br"], W=["lg"])
            for t in range(NT):
                r_ = rt.ap()
                L = lg.ap()[:, t, :]
                ops = []
                sc.op("dve", lambda e, L=L: e.tensor_reduce(out=r_[:, 0, 0:1], in_=L[:, 0:4], axis=AX.X, op=ALU.max), R=["lg"], W=["rt"])
                sc.op("dve", lambda e, L=L: e.tensor_scalar(out=r_[:, 0, 4:8], in0=L[:, 0:4], scalar1=r_[:, 0, 0:1], scalar2=None, op0=ALU.is_equal), R=["lg", "rt"], W=["rt"])
                sc.op("dve", lambda e, L=L: e.tensor_scalar(out=r_[:, 0, 8:12], in0=L[:, 0:4], scalar1=r_[:, 0, 0:1], scalar2=None, op0=ALU.subtract), R=["lg", "rt"], W=["rt"])
                sc.op("act", lambda e: e.activation(out=r_[:, 0, 8:12], in_=r_[:, 0, 8:12], func=AF.Exp, accum_out=r_[:, 0, 1:2]), R=["rt"], W=["rt"])
                sc.op("dve", lambda e: e.reciprocal(out=r_[:, 0, 2:3], in_=r_[:, 0, 1:2]), R=["rt"], W=["rt"])
                sc.op("dve", lambda e: e.tensor_scalar(out=r_[:, 0, 12:16], in0=r_[:, 0, 4:8], scalar1=-1.0, scalar2=1e30, op0=ALU.add, op1=ALU.mult), R=["rt"], W=["rt"])
                sc.op("dve", lambda e, L=L: e.tensor_tensor(out=r_[:, 1, 0:32].rearrange("p (g e) -> p g e", g=4), in0=L[:, 4:36].rearrange("p (g e) -> p g e", g=4),
                                                            in1=r_[:, 0, 12:16].unsqueeze(2).to_broadcast([128, 4, 8]), op=ALU.add), R=["lg", "rt"], W=["rt"])
                sc.op("dve", lambda e: e.max(out=r_[:, 2, 0:8], in_=r_[:, 1, 0:32]), R=["rt"], W=["rt"])
                sc.op("dve", lambda e: e.tensor_scalar(out=r_[:, 3, 0:32], in0=r_[:, 1, 0:32], scalar1=r_[:, 2, 0:1], scalar2=None, op0=ALU.is_equal), R=["rt"], W=["rt"])
                sc.op("dve", lambda e: e.tensor_scalar(out=r_[:, 4, 0:32], in0=r_[:, 1, 0:32], scalar1=r_[:, 2, 1:2], scalar2=None, op0=ALU.is_equal), R=["rt"], W=["rt"])
                sc.op("dve", lambda e: e.tensor_tensor(out=r_[:, 2, 8:9], in0=r_[:, 2, 1:2], in1=r_[:, 2, 0:1], op=ALU.subtract), R=["rt"], W=["rt"])
                sc.op("act", lambda e: e.activation(out=r_[:, 2, 9:10], in_=r_[:, 2, 8:9], func=AF.Exp), R=["rt"], W=["rt"])
                sc.op("dve", lambda e: e.tensor_scalar(out=r_[:, 2, 10:11], in0=r_[:, 2, 9:10], scalar1=1.0, scalar2=None, op0=ALU.add), R=["rt"], W=["rt"])
                sc.op("dve", lambda e: e.reciprocal(out=r_[:, 2, 11:12], in_=r_[:, 2, 10:11]), R=["rt"], W=["rt"])
                sc.op("dve", lambda e: e.tensor_tensor(out=r_[:, 2, 12:13], in0=r_[:, 2, 11:12], in1=r_[:, 0, 2:3], op=ALU.mult), R=["rt"], W=["rt"])
                sc.op("dve", lambda e: e.tensor_tensor(out=r_[:, 2, 13:14], in0=r_[:, 0, 2:3], in1=r_[:, 2, 12:13], op=ALU.subtract), R=["rt"], W=["rt"])
                sc.op("dve", lambda e: e.tensor_scalar(out=r_[:, 3, 0:32], in0=r_[:, 3, 0:32], scalar1=r_[:, 2, 12:13], scalar2=None, op0=ALU.mult), R=["rt"], W=["rt"])
                sc.op("dve", lambda e, t=t: e.scalar_tensor_tensor(out=gates.ap()[:, t, :], in0=r_[:, 4, 0:32], scalar=r_[:, 2, 13:14], in1=r_[:, 3, 0:32],
                                                                   op0=ALU.mult, op1=ALU.add), R=["rt"], W=[("gates", t)])

            def load_expert(ei, b):
                sc.dma("pool", lambda e: e.dma_start(out=wA[b].ap()[:, 0, :, :], in_=w1[layer, ei].rearrange("(c p) h -> p c h", p=128)), W=[f"wA{b}"])
                sc.dma("pool", lambda e: e.dma_start(out=wA[b].ap()[:, 1, :, :], in_=w3[layer, ei].rearrange("(c p) h -> p c h", p=128)), W=[f"wA{b}"])
                sc.dma("pool", lambda e: e.dma_start(out=wB[b].ap(), in_=w2[layer, ei].rearrange("(c p) d -> p c d", p=128)), W=[f"wB{b}"])

            src = xr[2 * layer + 1]
            dst = xr[2 * layer + 2]
            last = (layer == DEPTH - 1)
            ngroups = NT // TG
            it = 0
            load_expert(0, 0)
            for G in range(ngroups):
                for ei in range(NE):
                    b = it % 2
                    nxt = it + 1
                    if nxt < ngroups * NE:
                        load_expert(nxt % NE, nxt % 2)
                    for half in range(TG // 4):
                        tok0 = (G * TG + half * 4) * 128
                        hb = (it * (TG // 4) + half) % 2
                        for m in range(4):
                            for which in range(2):
                                pb = 2 * which + (m % 2)
                                for c in range(8):
                                    sc.op("pe", lambda e, c=c, m=m, which=which, pb=pb: e.matmul(
                                        bk(pb)[:, :], lhsT=wA[b].ap()[:, which, c, m * 128:(m + 1) * 128], rhs=xT.ap()[:, c, tok0:tok0 + 512],
                                        start=(c == 0), stop=(c == 7)),
                                        R=[f"wA{b}"] + [("xT", tok0 // 128 + q) for q in range(4)], W=[f"bank{pb}"])
                            sc.op("act", lambda e, m=m, hb=hb: e.activation(out=sl[hb].ap()[:, m, :], in_=bk(m % 2)[:, :], func=AF.Silu),
                                  R=[f"bank{m % 2}"], W=[f"sl{hb}"])
                            sc.op("dve", lambda e, m=m, hb=hb: e.tensor_tensor(out=gg[hb].ap()[:, m, :], in0=sl[hb].ap()[:, m, :], in1=bk(2 + m % 2)[:, :], op=ALU.mult),
                                  R=[f"sl{hb}", f"bank{2 + m % 2}"], W=[f"gg{hb}"])
                        for tt in range(4):
                            tl = half * 4 + tt
                            tglob = G * TG + tl
                            for hh in range(2):
                                pb = 4 + hh
                                for m in range(4):
                                    sc.op("pe", lambda e, m=m, tt=tt, hh=hh, pb=pb: e.matmul(
                                        bk(pb)[:, :], lhsT=gg[hb].ap()[:, m, tt * 128:(tt + 1) * 128], rhs=wB[b].ap()[:, m, hh * 512:(hh + 1) * 512],
                                        start=(m == 0), stop=(m == 3)), R=[f"gg{hb}", f"wB{b}"], W=[f"bank{pb}"])
                                if ei == 0:
                                    sc.op("dve", lambda e, tl=tl, hh=hh, pb=pb, tglob=tglob: e.tensor_scalar(
                                        out=acc.ap()[:, tl, hh * 512:(hh + 1) * 512], in0=bk(pb)[:, :], scalar1=gates.ap()[:, tglob, ei:ei + 1], scalar2=None, op0=ALU.mult),
                                        R=[f"bank{pb}", ("gates", tglob)], W=[("acc", tl)])
                                else:
                                    sc.op("dve", lambda e, tl=tl, hh=hh, pb=pb, tglob=tglob, ei=ei: e.scalar_tensor_tensor(
                                        out=acc.ap()[:, tl, hh * 512:(hh + 1) * 512], in0=bk(pb)[:, :], scalar=gates.ap()[:, tglob, ei:ei + 1],
                                        in1=acc.ap()[:, tl, hh * 512:(hh + 1) * 512], op0=ALU.mult, op1=ALU.add),
                                        R=[f"bank{pb}", ("gates", tglob), ("acc", tl)], W=[("acc", tl)])
                    it += 1
                for tl in range(TG):
                    t = G * TG + tl
                    b2 = t % 2
                    sc.dma("sp", lambda e, b2=b2, t=t: e.dma_start(out=xres[b2].ap(), in_=src[t * 128:(t + 1) * 128, :]), W=[f"xres{b2}"], pool="xr", n=4)
                    sc.op("dve", lambda e, b2=b2, tl=tl: e.scalar_tensor_tensor(out=acc.ap()[:, tl, :], in0=xres[b2].ap(), scalar=float(ALPHA), in1=acc.ap()[:, tl, :],
                                                                                op0=ALU.mult, op1=ALU.add), R=[f"xres{b2}", ("acc", tl)], W=[("acc", tl)])
                    ln_tile(acc.ap()[:, tl, :], ("acc", tl), gb, stat, t, dst, None if last else (xbf[b2].ap(), f"xbf{b2}"), 6 + b2)
            sc.barrier()

    def moe_layer_sparse(layer):
        src = xr[2 * layer + 1]
        dst = xr[2 * layer + 2]
        last = (layer == DEPTH - 1)
        w13rows = [w1.rearrange("l e d h -> (l e d) h"), w3.rearrange("l e d h -> (l e d) h")]
        w2rows = w2.rearrange("l e h d -> (l e h) d")
        with ExitStack() as es:
            ent = es.enter_context
            WT = ent(sbt("WT", [128, NT, 2], F32))
            posi = ent(sbt("posi", [128, 2, NT], I32))
            widx = ent(sbt("widx", [128, 2, NSLOT], I32))
            with ExitStack() as es1:
                ent1 = es1.enter_context
                M1 = ent1(sbt("M1", [128, NT, NE], F32))
                M2 = ent1(sbt("M2", [128, NT, NE], F32))
                wr = ent1(sbt("wr", [128, 8, 36], BF16))
                br = ent1(sbt("br", [128, 36], F32))
                lg = ent1(sbt("lg", [128, NT, 36], F32))
                rs = ent1(sbt("rs", [128, 6, NT], F32))
                r4 = ent1(sbt("r4", [128, 3, NT, 4], F32))
                elm = ent1(sbt("elm", [128, NT, NE], F32))
                Abf = ent1(sbt("Abf", [128, NT, NE], BF16))
                ones_bf = ent1(sbt("ones_bf", [128, 128], BF16))
                cntS = ent1(sbt("cntS", [128, NE], F32))
                nsl = ent1(sbt("nsl", [128, NE], F32))
                offe = ent1(sbt("offe", [128, NE], F32))
                posb = ent1(sbt("posb", [128, NE], F32))
                posfull = ent1(sbt("posfull", [128, NT, NE], F32))
                ptmp = ent1(sbt("ptmp", [128, NT, NE], F32))
                posf = ent1(sbt("posf", [128, 2, NT], F32))
                sidx = ent1(sbt("sidx", [128, NSLOT], F32))
                pidx = ent1(sbt("pidx", [128, 4], F32))
                cmp_ = ent1(sbt("cmp", [128, NSLOT, NE], F32))
                eidf = ent1(sbt("eidf", [128, 3, NSLOT], F32))
                xres = [ent1(sbt(f"xres{i}", [128, D], F32)) for i in range(4)]
                xbf = [ent1(sbt(f"xbf{i}", [128, D], BF16)) for i in range(8)]

                sc.dma("pool", lambda e: e.dma_start(out=wr.ap()[:, :, 0:4], in_=w_group[layer].rearrange("(c p) g -> p c g", p=128)), W=["wr"])
                sc.dma("pool", lambda e: e.dma_start(out=wr.ap()[:, :, 4:36], in_=w_expert[layer].rearrange("(c p) g -> p c g", p=128)), W=["wr"])
                sc.dma("sp", lambda e: e.dma_start(out=br.ap()[:, 0:4], in_=b_group[layer:layer + 1, :].partition_broadcast(128)), W=["br"])
                sc.dma("sp", lambda e: e.dma_start(out=br.ap()[:, 4:36], in_=b_expert[layer:layer + 1, :].partition_broadcast(128)), W=["br"])
                sc.dma("sp", lambda e: e.dma_start(out=sidx.ap(), in_=c_sidx), W=["sidx"])
                sc.dma("sp", lambda e: e.dma_start(out=pidx.ap()[:, 0:1], in_=c_pidx), W=["pidx"])
                sc.op("dve", lambda e: e.memset(ones_bf.ap(), 1.0), W=["ones_bf"])
                for t in range(NT):
                    pb = 6 + (t % 2)
                    for c in range(8):
                        sc.op("pe", lambda e, c=c, t=t, pb=pb: e.matmul(bk(pb)[:, 0:36], lhsT=xT.ap()[:, c, t * 128:(t + 1) * 128], rhs=wr.ap()[:, c, :],
                                                                        start=(c == 0), stop=(c == 7)), R=[("xT", t), "wr"], W=[f"bank{pb}"])
                    sc.op("dve", lambda e, t=t, pb=pb: e.tensor_tensor(out=lg.ap()[:, t, :], in0=bk(pb)[:, 0:36], in1=br.ap(), op=ALU.add),
                          R=[f"bank{pb}", "br"], W=["lg"])
                LG = lg.ap()
                gmax = rs.ap()[:, 0, :]
                gsum = rs.ap()[:, 1, :]
                ggate = rs.ap()[:, 2, :]
                m1 = rs.ap()[:, 3, :]
                m2 = rs.ap()[:, 4, :]
                rr = rs.ap()[:, 5, :]
                gm = r4.ap()[:, 0, :, :]
                dd = r4.ap()[:, 1, :, :]
                pen = r4.ap()[:, 2, :, :]
                sc.op("dve", lambda e: e.tensor_reduce(out=gmax, in_=LG[:, :, 0:4], axis=AX.X, op=ALU.max), R=["lg"], W=["rs"])
                sc.op("dve", lambda e: e.tensor_tensor(out=gm, in0=LG[:, :, 0:4], in1=gmax.unsqueeze(2).to_broadcast([128, NT, 4]), op=ALU.is_equal), R=["lg", "rs"], W=["r4"])
                sc.op("dve", lambda e: e.tensor_tensor(out=dd, in0=LG[:, :, 0:4], in1=gmax.unsqueeze(2).to_broadcast([128, NT, 4]), op=ALU.subtract), R=["lg", "rs"], W=["r4"])
                sc.op("act", lambda e: e.activation(out=dd, in_=dd, func=AF.Exp), R=["r4"], W=["r4"])
                sc.op("dve", lambda e: e.tensor_reduce(out=gsum, in_=dd, axis=AX.X, op=ALU.add), R=["r4"], W=["rs"])
                sc.op("dve", lambda e: e.reciprocal(out=ggate, in_=gsum), R=["rs"], W=["rs"])
                sc.op("dve", lambda e: e.tensor_scalar(out=pen, in0=gm, scalar1=-1.0, scalar2=1e30, op0=ALU.add, op1=ALU.mult), R=["r4"], W=["r4"])
                elm4 = elm.ap().rearrange("p t (g e) -> p t g e", g=4)
                sc.op("dve", lambda e: e.tensor_tensor(out=elm4, in0=LG[:, :, 4:36].rearrange("p t (g e) -> p t g e", g=4),
                                                       in1=pen.unsqueeze(3).to_broadcast([128, NT, 4, 8]), op=ALU.add), R=["lg", "r4"], W=["elm"])
                sc.op("dve", lambda e: e.tensor_reduce(out=m1, in_=elm.ap(), axis=AX.X, op=ALU.max), R=["elm"], W=["rs"])
                sc.op("dve", lambda e: e.tensor_tensor(out=M1.ap(), in0=elm.ap(), in1=m1.unsqueeze(2).to_broadcast([128, NT, NE]), op=ALU.is_equal), R=["elm", "rs"], W=["M1"])
                sc.op("dve", lambda e: e.scalar_tensor_tensor(out=elm.ap(), in0=M1.ap(), scalar=-1e30, in1=elm.ap(), op0=ALU.mult, op1=ALU.add), R=["elm", "M1"], W=["elm"])
                sc.op("dve", lambda e: e.tensor_reduce(out=m2, in_=elm.ap(), axis=AX.X, op=ALU.max), R=["elm"], W=["rs"])
                sc.op("dve", lambda e: e.tensor_tensor(out=M2.ap(), in0=elm.ap(), in1=m2.unsqueeze(2).to_broadcast([128, NT, NE]), op=ALU.is_equal), R=["elm", "rs"], W=["M2"])
                sc.op("dve", lambda e: e.tensor_tensor(out=rr, in0=m2, in1=m1, op=ALU.subtract), R=["rs"], W=["rs"])
                sc.op("act", lambda e: e.activation(out=rr, in_=rr, func=AF.Exp), R=["rs"], W=["rs"])
                sc.op("dve", lambda e: e.tensor_scalar(out=rr, in0=rr, scalar1=1.0, scalar2=None, op0=ALU.add), R=["rs"], W=["rs"])
                sc.op("dve", lambda e: e.reciprocal(out=rr, in_=rr), R=["rs"], W=["rs"])
                sc.op("dve", lambda e: e.tensor_tensor(out=WT.ap()[:, :, 0], in0=rr, in1=ggate, op=ALU.mult), R=["rs"], W=["WT"])
                sc.op("dve", lambda e: e.tensor_tensor(out=WT.ap()[:, :, 1], in0=ggate, in1=WT.ap()[:, :, 0], op=ALU.subtract), R=["rs", "WT"], W=["WT"])
                sc.op("dve", lambda e: e.tensor_tensor(out=Abf.ap(), in0=M1.ap(), in1=M2.ap(), op=ALU.add), R=["M1", "M2"], W=["Abf"])
                for t in range(NT):
                    rb_ = t // 16
                    col = (t % 16) * 32
                    for tp in range(t):
                        sc.op("pe", lambda e, tp=tp, rb_=rb_, col=col: e.matmul(bk(rb_)[:, col:col + 32], lhsT=ones_bf.ap(), rhs=Abf.ap()[:, tp, :],
                                                                                  start=(tp == 0), stop=False, skip_group_check=True),
                              R=["Abf", "ones_bf"], W=[f"bank{rb_}"])
                    sc.op("pe", lambda e, t=t, rb_=rb_, col=col: e.matmul(bk(rb_)[:, col:col + 32], lhsT=tri_low.ap(), rhs=Abf.ap()[:, t, :],
                                                                          start=(t == 0), stop=True, skip_group_check=True),
                          R=["Abf", "tri_low"], W=[f"bank{rb_}"])
                for t in range(NT):
                    sc.op("pe", lambda e, t=t: e.matmul(bk(2)[:, 0:32], lhsT=ones_bf.ap(), rhs=Abf.ap()[:, t, :], start=(t == 0), stop=(t == NT - 1)),
                          R=["Abf", "ones_bf"], W=["bank2"])
                sc.op("dve", lambda e: e.tensor_copy(out=cntS.ap(), in_=bk(2)[:, 0:32]), R=["bank2"], W=["cntS"])
                sc.op("dve", lambda e: e.tensor_scalar(out=nsl.ap(), in0=cntS.ap(), scalar1=0.0, scalar2=None, op0=ALU.is_gt), R=["cntS"], W=["nsl"])
                for k in range(1, KMAX):
                    sc.op("dve", lambda e, k=k: e.scalar_tensor_tensor(out=nsl.ap(), in0=cntS.ap(), scalar=float(k * SL), in1=nsl.ap(), op0=ALU.is_gt, op1=ALU.add),
                          R=["cntS", "nsl"], W=["nsl"])
                sc.op("dve", lambda e: e.tensor_copy(out=offe.ap()[:, 0:1], in_=nsl.ap()[:, 0:1]), R=["nsl"], W=["offe"])
                for ei in range(1, NE):
                    sc.op("dve", lambda e, ei=ei: e.tensor_tensor(out=offe.ap()[:, ei:ei + 1], in0=offe.ap()[:, ei - 1:ei], in1=nsl.ap()[:, ei:ei + 1], op=ALU.add),
                          R=["nsl", "offe"], W=["offe"])
                sc.op("dve", lambda e: e.tensor_tensor(out=posb.ap(), in0=offe.ap(), in1=nsl.ap(), op=ALU.subtract), R=["offe", "nsl"], W=["posb"])
                sc.op("dve", lambda e: e.tensor_scalar(out=posb.ap(), in0=posb.ap(), scalar1=float(SL), scalar2=None, op0=ALU.mult), R=["posb"], W=["posb"])
                for hh in range(2):
                    sc.op("dve", lambda e, hh=hh: e.tensor_tensor(out=posfull.ap()[:, 16 * hh:16 * hh + 16, :], in0=bk(hh)[:, :].rearrange("p (a f) -> p a f", a=16),
                                                                  in1=posb.ap().unsqueeze(1).to_broadcast([128, 16, NE]), op=ALU.add),
                          R=[f"bank{hh}", "posb"], W=["posfull"])
                for ci, M_ in enumerate((M1, M2)):
                    mres = "M1" if ci == 0 else "M2"
                    sc.op("dve", lambda e, M_=M_: e.tensor_tensor(out=ptmp.ap(), in0=posfull.ap(), in1=M_.ap(), op=ALU.mult), R=["posfull", mres], W=["ptmp"])
                    sc.op("dve", lambda e, ci=ci: e.tensor_reduce(out=posf.ap()[:, ci, :], in_=ptmp.ap(), axis=AX.X, op=ALU.add), R=["ptmp"], W=["posf"])
                sc.op("dve", lambda e: e.tensor_scalar(out=posf.ap(), in0=posf.ap(), scalar1=float(NPOS - 1), scalar2=None, op0=ALU.min), R=["posf"], W=["posf"])
                sc.op("dve", lambda e: e.tensor_copy(out=posi.ap(), in_=posf.ap()), R=["posf"], W=["posi"])
                sc.op("dve", lambda e: e.tensor_tensor(out=cmp_.ap(), in0=offe.ap().unsqueeze(1).to_broadcast([128, NSLOT, NE]),
                                                       in1=sidx.ap().unsqueeze(2).to_broadcast([128, NSLOT, NE]), op=ALU.is_le), R=["offe", "sidx"], W=["cmp"])
                sc.op("dve", lambda e: e.tensor_reduce(out=eidf.ap()[:, 0, :], in_=cmp_.ap(), axis=AX.X, op=ALU.add), R=["cmp"], W=["eidf"])
                sc.op("dve", lambda e: e.tensor_scalar(out=eidf.ap()[:, 0, :], in0=eidf.ap()[:, 0, :], scalar1=float(NE - 1), scalar2=None, op0=ALU.min), R=["eidf"], W=["eidf"])
                sc.op("dve", lambda e: e.tensor_scalar(out=pidx.ap()[:, 1:2], in0=pidx.ap()[:, 0:1], scalar1=float(layer * NE * D), scalar2=None, op0=ALU.add), R=["pidx"], W=["pidx"])
                sc.op("dve", lambda e: e.tensor_scalar(out=pidx.ap()[:, 2:3], in0=pidx.ap()[:, 0:1], scalar1=float(layer * NE * HID), scalar2=None, op0=ALU.add), R=["pidx"], W=["pidx"])
                sc.op("dve", lambda e: e.tensor_scalar(out=eidf.ap()[:, 1, :], in0=eidf.ap()[:, 0, :], scalar1=128.0, scalar2=pidx.ap()[:, 0:1], op0=ALU.mult, op1=ALU.add),
                      R=["eidf", "pidx"], W=["eidf"])
                sc.op("dve", lambda e: e.tensor_scalar(out=eidf.ap()[:, 2, :], in0=eidf.ap()[:, 0, :], scalar1=128.0, scalar2=pidx.ap()[:, 0:1], op0=ALU.mult, op1=ALU.add),
                      R=["eidf", "pidx"], W=["eidf"])
                sc.op("dve", lambda e: e.tensor_copy(out=widx.ap(), in_=eidf.ap()[:, 1:3, :]), R=["eidf"], W=["widx"])
                for t in range(NT):
                    b = t % 4
                    b8 = t % 8
                    sc.dma("sp", lambda e, b=b, t=t: e.dma_start(out=xres[b].ap(), in_=src[t * 128:(t + 1) * 128, :]), W=[f"xres{b}"], pool="xr", n=4)
                    sc.op("act", lambda e, b=b, b8=b8: e.activation(out=xbf[b8].ap(), in_=xres[b].ap(), func=AF.Copy), R=[f"xres{b}"], W=[f"xbf{b8}"])
                    for ci in range(2):
                        sc.dma("pool", lambda e, b8=b8, t=t, ci=ci: e.indirect_dma_start(
                            out=Xs, out_offset=bass.IndirectOffsetOnAxis(ap=posi.ap()[:, ci, t:t + 1], axis=0), in_=xbf[b8].ap(), in_offset=None),
                            R=[f"xbf{b8}", "posi"], W=[("Xs", t, ci)], pool="sct", n=8)
                sc.barrier()

            with ExitStack() as es2:
                ent2 = es2.enter_context
                wA = [ent2(sbt(f"wA{i}", [128, 2, 8, HID], BF16)) for i in range(2)]
                wB = [ent2(sbt(f"wB{i}", [128, 4, D], BF16)) for i in range(2)]
                xs = [[ent2(sbt(f"xs{i}_{k}", [128, D], BF16)) for k in range(SL // 128)] for i in range(2)]
                xsT = [ent2(sbt(f"xsT{i}", [128, 8, SL], BF16)) for i in range(2)]
                sl = [ent2(sbt(f"sl{i}", [128, 4, SL], BF16)) for i in range(2)]
                gg = [ent2(sbt(f"gg{i}", [128, 4, SL], BF16)) for i in range(2)]
                ys = [ent2(sbt(f"ys{i}", [128, D], F32)) for i in range(2)]
                NS3 = SL // 128

                def load_slot(s_):
                    b = s_ % 2
                    for which in range(2):
                        sc.dma("pool", lambda e, which=which: e.indirect_dma_start(
                            out=wA[b].ap()[:, which, :, :].rearrange("p c h -> p (c h)"), out_offset=None, in_=Wc[which],
                            in_offset=bass.IndirectOffsetOnAxis(ap=widx.ap()[:, 0, s_:s_ + 1], axis=0)),
                            R=["widx"], W=[(f"wA{b}", which)], pool="wgt", n=12)
                    sc.dma("pool", lambda e: e.indirect_dma_start(
                        out=wB[b].ap().rearrange("p c d -> p (c d)"), out_offset=None, in_=Wc[2],
                        in_offset=bass.IndirectOffsetOnAxis(ap=widx.ap()[:, 0, s_:s_ + 1], axis=0)),
                        R=["widx"], W=[f"wB{b}"], pool="wgt", n=12)
                    for k in range(NS3):
                        r0 = s_ * SL + k * 128
                        sc.dma("sp", lambda e, k=k, r0=r0: e.dma_start(out=xs[b][k].ap(), in_=Xs[r0:r0 + 128, :]), R=["Xs"], W=[f"xs{b}_{k}"], pool="xs", n=6)

                load_slot(0)
                for s_ in range(NSLOT):
                    b = s_ % 2
                    if s_ + 1 < NSLOT:
                        load_slot(s_ + 1)
                    for k in range(NS3):
                        tb = 6 + (k % 2)
                        for c in range(8):
                            sc.op("pe", lambda e, c=c, k=k, tb=tb: e.transpose(out=bkh(tb)[:, c * 128:(c + 1) * 128], in_=xs[b][k].ap()[:, c * 128:(c + 1) * 128],
                                                                              identity=ident.ap()), R=[f"xs{b}_{k}", "ident"], W=[f"bank{tb}"])
                        sc.op("act" if k % 2 == 0 else "dve",
                              lambda e, k=k, tb=tb: (e.activation(out=xsT[b].ap()[:, :, k * 128:(k + 1) * 128], in_=bkh(tb)[:, :].rearrange("p (c f) -> p c f", c=8), func=AF.Copy)
                                                     if k % 2 == 0 else
                                                     e.tensor_copy(out=xsT[b].ap()[:, :, k * 128:(k + 1) * 128], in_=bkh(tb)[:, :].rearrange("p (c f) -> p c f", c=8))),
                              R=[f"bank{tb}"], W=[f"xsT{b}"])
                    for m in range(4):
                        for which in range(2):
                            pb = 2 * which + (m % 2)
                            for c in range(8):
                                sc.op("pe", lambda e, c=c, m=m, which=which, pb=pb: e.matmul(
                                    bk(pb)[:, 0:SL], lhsT=wA[b].ap()[:, which, c, m * 128:(m + 1) * 128], rhs=xsT[b].ap()[:, c, :],
                                    start=(c == 0), stop=(c == 7)), R=[(f"wA{b}", which), f"xsT{b}"], W=[f"bank{pb}"])
                        sc.op("act", lambda e, m=m: e.activation(out=sl[b].ap()[:, m, :], in_=bk(m % 2)[:, 0:SL], func=AF.Silu), R=[f"bank{m % 2}"], W=[f"sl{b}"])
                        sc.op("dve", lambda e, m=m: e.tensor_tensor(out=gg[b].ap()[:, m, :], in0=sl[b].ap()[:, m, :], in1=bk(2 + m % 2)[:, 0:SL], op=ALU.mult),
                              R=[f"sl{b}", f"bank{2 + m % 2}"], W=[f"gg{b}"])
                    for k in range(NS3):
                        yb_ = (s_ * NS3 + k) % 2
                        for hh in range(2):
                            pb = 4 + hh
                            for m in range(4):
                                sc.op("pe", lambda e, m=m, k=k, hh=hh, pb=pb: e.matmul(
                                    bk(pb)[:, :], lhsT=gg[b].ap()[:, m, k * 128:(k + 1) * 128], rhs=wB[b].ap()[:, m, hh * 512:(hh + 1) * 512],
                                    start=(m == 0), stop=(m == 3)), R=[f"gg{b}", f"wB{b}"], W=[f"bank{pb}"])
                            if hh == 0:
                                sc.op("act", lambda e, yb_=yb_, pb=pb: e.activation(out=ys[yb_].ap()[:, 0:512], in_=bk(pb)[:, :], func=AF.Copy), R=[f"bank{pb}"], W=[f"ys{yb_}"])
                            else:
                                sc.op("dve", lambda e, yb_=yb_, pb=pb: e.tensor_copy(out=ys[yb_].ap()[:, 512:1024], in_=bk(pb)[:, :]), R=[f"bank{pb}"], W=[f"ys{yb_}"])
                        r0 = s_ * SL + k * 128
                        sc.dma("sp", lambda e, yb_=yb_, r0=r0: e.dma_start(out=Ys[r0:r0 + 128, :], in_=ys[yb_].ap()), R=[f"ys{yb_}"], W=[("Ys", s_, k)], pool="ys", n=4)
                sc.barrier()

            with ExitStack() as es3:
                ent3 = es3.enter_context
                gb = ent3(sbt("gb", [128, 2, D], F32))
                NB3 = 4
                stat = [ent3(sbt(f"stat{i}", [128, 20], F32)) for i in range(NB3)]
                g1 = [ent3(sbt(f"g1_{i}", [128, D], F32)) for i in range(NB3)]
                g2 = [ent3(sbt(f"g2_{i}", [128, D], F32)) for i in range(NB3)]
                xres = [ent3(sbt(f"xres{i}", [128, D], F32)) for i in range(NB3)]
                yb = [ent3(sbt(f"yb{i}", [128, D], F32)) for i in range(NB3)]
                xbf = [ent3(sbt(f"xbf{i}", [128, D], BF16)) for i in range(NB3)]
                load_gb(gb, ln_ffn_g, ln_ffn_b, layer)
                def m3_fetch(t):
                    b = t % NB3
                    sc.dma("pool", lambda e: e.indirect_dma_start(out=g1[b].ap(), out_offset=None, in_=Ys,
                                                                  in_offset=bass.IndirectOffsetOnAxis(ap=posi.ap()[:, 0, t:t + 1], axis=0)),
                           R=["Ys", "posi"], W=[f"g1_{b}"], pool="gth", n=8)
                    sc.dma("pool", lambda e: e.indirect_dma_start(out=g2[b].ap(), out_offset=None, in_=Ys,
                                                                  in_offset=bass.IndirectOffsetOnAxis(ap=posi.ap()[:, 1, t:t + 1], axis=0)),
                           R=["Ys", "posi"], W=[f"g2_{b}"], pool="gth", n=8)
                    sc.dma("sp", lambda e: e.dma_start(out=xres[b].ap(), in_=src[t * 128:(t + 1) * 128, :]), W=[f"xres{b}"], pool="xr", n=4)

                def m3_produce_a(t):
                    b = t % NB3
                    sc.op("act", lambda e: e.activation(out=g1[b].ap(), in_=g1[b].ap(), func=AF.Copy, scale=WT.ap()[:, t, 0:1]), R=[f"g1_{b}", "WT"], W=[f"g1_{b}"])

                def m3_produce(t):
                    b = t % NB3
                    sc.op("dve", lambda e: e.scalar_tensor_tensor(out=g2[b].ap(), in0=g2[b].ap(), scalar=WT.ap()[:, t, 1:2], in1=g1[b].ap(), op0=ALU.mult, op1=ALU.add),
                          R=[f"g2_{b}", f"g1_{b}", "WT"], W=[f"g2_{b}"])
                    sc.op("dve", lambda e: e.scalar_tensor_tensor(out=yb[b].ap(), in0=xres[b].ap(), scalar=float(ALPHA), in1=g2[b].ap(), op0=ALU.mult, op1=ALU.add),
                          R=[f"xres{b}", f"g2_{b}"], W=[f"yb{b}"])

                ln_pipeline(m3_fetch, m3_produce_a, m3_produce, yb, (lambda b: f"yb{b}"), stat, gb, dst, None if last else xbf)
                sc.barrier()

    stages = []
    for layer in range(DEPTH):
        stages.append(("att", layer))
        stages.append(("moe", layer))
    for i, (kind, layer) in enumerate(stages):
        if stop_after is not None and i > stop_after:
            break
        if kind == "att":
            attention_layer(layer)
        elif SPARSE:
            moe_layer_sparse(layer)
        else:
            moe_layer(layer)

    sc.final_wait("sp")
    return nc


_CACHE = {}


def _prep_common(inputs):
    f = lambda a: np.ascontiguousarray(np.asarray(a), dtype=np.float32)
    m = {
        "rel_table": f(inputs["rel_table"]),
        "da_wq": f(inputs["da_wq"][0]), "da_wk": f(inputs["da_wk"][0]), "da_wv": f(inputs["da_wv"][0]), "da_wo": f(inputs["da_wo"][0]),
        "sb_wq": f(inputs["sb_wq"][0]), "sb_wk": f(inputs["sb_wk"][0]), "sb_wv": f(inputs["sb_wv"][0]), "sb_wo": f(inputs["sb_wo"][0]),
        "da_l": f(np.stack([inputs["da_lq1"][0], inputs["da_lk1"][0], inputs["da_lq2"][0], inputs["da_lk2"][0]], axis=0)),
        "da_subln_g": f(inputs["da_subln_g"]),
        "ln_mix_g": f(inputs["ln_mix_g"]), "ln_mix_b": f(inputs["ln_mix_b"]),
        "ln_ffn_g": f(inputs["ln_ffn_g"]), "ln_ffn_b": f(inputs["ln_ffn_b"]),
        "moe_w_group": f(inputs["moe_w_group"]), "moe_b_group": f(inputs["moe_b_group"]),
        "moe_w_expert": f(inputs["moe_w_expert"]), "moe_b_expert": f(inputs["moe_b_expert"]),
        "moe_w1": f(inputs["moe_w1"]), "moe_w3": f(inputs["moe_w3"]), "moe_w2": f(inputs["moe_w2"]),
    }
    m.update(_consts_host())
    return m


def kernel(**inputs):
    if "nc" not in _CACHE:
        _CACHE["nc"] = build_program()
    nc = _CACHE["nc"]
    common = _prep_common(inputs)
    x = np.asarray(inputs["x"], dtype=np.float32)
    in_maps = []
    for b in range(8):
        m = dict(common)
        m["x"] = np.ascontiguousarray(x[b])
        in_maps.append(m)
    res = run_bass_kernel_spmd(nc, in_maps, core_ids=list(range(8)))
    return np.stack([np.asarray(r["out"], dtype=np.float32) for r in res.results], axis=0)
```

```python
import math
from contextlib import ExitStack
import numpy as np
import ml_dtypes
import concourse.bass as bass
import concourse.mybir as mybir
from concourse.bass_utils import run_bass_kernel_spmd

F32 = mybir.dt.float32
BF16 = mybir.dt.bfloat16
AF = mybir.ActivationFunctionType
ALU = mybir.AluOpType
AX = mybir.AxisListType

S = 4096
D = 1024
NT = S // 128
DEPTH = 2
NE = 32
HID = 512
ALPHA = (2 * DEPTH) ** 0.25
LN_EPS = 1e-5
RMS_EPS = 1e-6
GW = 383
SL = 256
NSLOT = 63
NPOS = NSLOT * SL
KMAX = (S + SL - 1) // SL
I32 = mybir.dt.int32
SPARSE = True


class Sched:
    LIMIT = 30000
    NDMA = 24

    def __init__(self, nc):
        self.nc = nc
        self.eng = {"pe": nc.tensor, "act": nc.scalar, "dve": nc.vector, "pool": nc.gpsimd, "sp": nc.sync}
        self.esem = {}
        self.ecnt = {}
        self.nsem = 0
        for e in self.eng:
            self._newsem(e)
        self.seen = {e: {} for e in self.eng}
        self.lastw = {}
        self.readers = {}
        self.dpools = {}

    def _newsem(self, e):
        self.esem[e] = self.nc.alloc_semaphore(f"es_{e}_{self.nsem}")
        self.nsem += 1
        self.ecnt[e] = 0

    def _wait(self, e, dep):
        sem, val, weng = dep
        if weng == e and e == "pe":
            return
        k = sem.num
        if self.seen[e].get(k, 0) >= val:
            return
        self.eng[e].wait_ge(sem, val)
        self.seen[e][k] = val

    def _deps(self, e, R, W):
        for r in R:
            w = self.lastw.get(r)
            if w:
                for tok in w.values():
                    self._wait(e, tok)
        for w_ in W:
            lw = self.lastw.get(w_)
            if lw:
                for tok in lw.values():
                    self._wait(e, tok)
            rd = self.readers.get(w_)
            if rd:
                for tok in rd.values():
                    self._wait(e, tok)

    def _book(self, tok, R, W):
        for r in R:
            self.readers.setdefault(r, {})[tok[0].num] = tok
        for w_ in W:
            self.lastw.setdefault(w_, {})[tok[0].num] = tok
            self.readers[w_] = {}

    def op(self, e, fn, R=(), W=()):
        self._deps(e, R, W)
        if self.ecnt[e] >= self.LIMIT:
            self._newsem(e)
        inst = fn(self.eng[e])
        self.ecnt[e] += 1
        inst.then_inc(self.esem[e], 1)
        self._book((self.esem[e], self.ecnt[e], e), R, W)

    def dma(self, q, fn, R=(), W=(), pool="misc", n=6):
        if pool not in self.dpools:
            self.dpools[pool] = dict(sems=[self.nc.alloc_semaphore(f"dq_{pool}_{i}") for i in range(n)], val=[0] * n, nxt=0)
        P = self.dpools[pool]
        i = P["nxt"]
        P["nxt"] = (i + 1) % len(P["sems"])
        sem = P["sems"][i]
        if P["val"][i] > 0:
            self._wait(q, (sem, P["val"][i], "dma"))
        self._deps(q, R, W)
        inst = fn(self.eng[q])
        P["val"][i] += 16
        inst.then_inc(sem, 16)
        self._book((sem, P["val"][i], "dma"), R, W)

    def _dma_toks(self):
        toks = []
        for P in self.dpools.values():
            for sem, v in zip(P["sems"], P["val"]):
                if v > 0:
                    toks.append((sem, v, "dma"))
        return toks

    def barrier(self):
        toks = [(self.esem[e], self.ecnt[e], "x") for e in self.eng if self.ecnt[e] > 0]
        toks += self._dma_toks()
        for e in self.eng:
            for t in toks:
                self._wait(e, t)
        self.lastw = {}
        self.readers = {}

    def final_wait(self, e="sp"):
        for t in self._dma_toks():
            self._wait(e, t)
        for o in self.eng:
            if o != e and self.ecnt[o] > 0:
                self._wait(e, (self.esem[o], self.ecnt[o], o))


def _consts_host():
    ident = np.eye(128, dtype=np.float32)
    kk = np.arange(128)[:, None]
    qq = np.arange(128)[None, :]
    tri_incl = (kk >= np.arange(128)[None, :]).astype(np.float32)
    tri_low = (kk < np.arange(128)[None, :]).astype(np.float32)
    mask_le = (kk <= qq).astype(np.float32)
    mask_lt = (kk < qq).astype(np.float32)
    n = np.maximum(np.arange(GW) - 127, 0)
    nf = np.maximum(n, 1).astype(np.float32)
    large = 16 + (np.log(nf / np.float32(16)) / np.float32(math.log(128 / 16)) * np.float32(16)).astype(np.int32)
    large = np.minimum(large, 31)
    bucket = np.where(n < 16, n, large)
    oh = (bucket[None, :] == np.arange(32)[:, None]).astype(np.float32)
    bf = ml_dtypes.bfloat16
    return {
        "c_ident": ident.astype(bf), "c_tri_incl": tri_incl.astype(bf), "c_tri_low": tri_low.astype(bf),
        "c_mask_le": mask_le, "c_mask_lt": mask_lt.astype(bf), "c_oh": oh,
        "c_pidx": np.arange(128, dtype=np.float32).reshape(128, 1),
        "c_sidx": np.tile(np.arange(NSLOT, dtype=np.float32)[None, :], (128, 1)),
    }


def build_program(stop_after=None, debug=False):
    nc = bass.Bass("TRN2", target_bir_lowering=False)
    sc = Sched(nc)

    def din(name, shape, dt=F32):
        return nc.dram_tensor(name, list(shape), dt, kind="ExternalInput").ap()

    x_in = din("x", [S, D])
    rel_table = din("rel_table", [32, 8])
    attw = [
        dict(wq=din("da_wq", [D, D]), wk=din("da_wk", [D, D]), wv=din("da_wv", [D, D]), wo=din("da_wo", [D, D])),
        dict(wq=din("sb_wq", [D, D]), wk=din("sb_wk", [D, D]), wv=din("sb_wv", [D, D]), wo=din("sb_wo", [D, D])),
    ]
    da_l = din("da_l", [4, 64])
    da_g = din("da_subln_g", [1, 128])
    ln_mix_g = din("ln_mix_g", [DEPTH, D]); ln_mix_b = din("ln_mix_b", [DEPTH, D])
    ln_ffn_g = din("ln_ffn_g", [DEPTH, D]); ln_ffn_b = din("ln_ffn_b", [DEPTH, D])
    w_group = din("moe_w_group", [DEPTH, D, 4]); b_group = din("moe_b_group", [DEPTH, 4])
    w_expert = din("moe_w_expert", [DEPTH, D, NE]); b_expert = din("moe_b_expert", [DEPTH, NE])
    w1 = din("moe_w1", [DEPTH, NE, D, HID]); w3 = din("moe_w3", [DEPTH, NE, D, HID]); w2 = din("moe_w2", [DEPTH, NE, HID, D])
    c_ident = din("c_ident", [128, 128], BF16); c_tri_incl = din("c_tri_incl", [128, 128], BF16)
    c_tri_low = din("c_tri_low", [128, 128], BF16); c_mask_le = din("c_mask_le", [128, 128])
    c_mask_lt = din("c_mask_lt", [128, 128], BF16); c_oh = din("c_oh", [32, GW])
    c_pidx = din("c_pidx", [128, 1]); c_sidx = din("c_sidx", [128, NSLOT])
    Xs = nc.dram_tensor("Xs", [NPOS, D], BF16, kind="Internal").ap()
    Ys = nc.dram_tensor("Ys", [NPOS, D], F32, kind="Internal").ap()
    Wc = [nc.dram_tensor(f"Wc{i}", [NE * 128, 4096], BF16, kind="Internal").ap() for i in range(3)]

    out = nc.dram_tensor("out", [S, D], F32, kind="ExternalOutput").ap()
    skind = "ExternalOutput" if debug else "Internal"
    xr = [x_in] + [nc.dram_tensor(f"xr{i}", [S, D], F32, kind=skind).ap() for i in (1, 2, 3)] + [out]
    gd = nc.dram_tensor("gd", [8, 128, GW], F32, kind="Internal")

    PP = [nc.alloc_psum_tensor(f"pp{i}", [128, 1024], F32) for i in range(4)]

    _uid = [0]

    def sbt(name, shape, dt):
        _uid[0] += 1
        return nc.sbuf_tensor(f"{name}_u{_uid[0]}", shape, dt)

    def bk(i):
        return PP[i // 2].ap()[:, (i % 2) * 512:(i % 2) * 512 + 512]

    def bkh(i):
        return PP[i // 2].bitcast(BF16).ap()[:, (i % 2) * 1024:(i % 2) * 1024 + 1024]

    xT = nc.alloc_sbuf_tensor("xT", [128, 8, S], BF16)
    ident = nc.alloc_sbuf_tensor("ident", [128, 128], BF16)
    tri_incl = nc.alloc_sbuf_tensor("tri_incl", [128, 128], BF16)
    tri_low = nc.alloc_sbuf_tensor("tri_low", [128, 128], BF16)
    mask_le = nc.alloc_sbuf_tensor("mask_le", [128, 128], F32)
    mask_lt = nc.alloc_sbuf_tensor("mask_lt", [128, 128], BF16)
    cst = nc.alloc_sbuf_tensor("cst", [128, 8], F32)
    sc.dma("sp", lambda e: e.dma_start(out=ident.ap(), in_=c_ident), W=["ident"])
    sc.dma("sp", lambda e: e.dma_start(out=tri_incl.ap(), in_=c_tri_incl), W=["tri_incl"])
    sc.dma("sp", lambda e: e.dma_start(out=tri_low.ap(), in_=c_tri_low), W=["tri_low"])
    sc.dma("sp", lambda e: e.dma_start(out=mask_le.ap(), in_=c_mask_le), W=["mask_le"])
    sc.dma("sp", lambda e: e.dma_start(out=mask_lt.ap(), in_=c_mask_lt), W=["mask_lt"])
    sc.op("dve", lambda e: e.memset(cst.ap()[:, 0:1], LN_EPS), W=["cst"])
    sc.op("dve", lambda e: e.memset(cst.ap()[:, 1:2], RMS_EPS), W=["cst"])
    sc.op("dve", lambda e: e.memset(cst.ap()[:, 2:3], 1.0), W=["cst"])

    def make_xT_tile(src_bf, src_res, t, bank):
        for c in range(8):
            sc.op("pe", lambda e, c=c: e.transpose(out=bkh(bank)[:, c * 128:(c + 1) * 128], in_=src_bf[:, c * 128:(c + 1) * 128],
                                                    identity=ident.ap()),
                  R=[src_res, "ident"], W=[f"bank{bank}"])
        sc.op("act", lambda e: e.activation(out=xT.ap()[:, :, t * 128:(t + 1) * 128],
                                            in_=bkh(bank)[:, :].rearrange("p (c f) -> p c f", c=8), func=AF.Copy),
              R=[f"bank{bank}"], W=[("xT", t)])

    def ln_tile(y, yres, gb, stat, t, dst, xbf, lnbank):
        sres = ("stat", stat.name)
        st6 = stat.ap()[:, 0:12].rearrange("p (a b) -> p a b", a=2)
        for hh in range(2):
            sc.op("dve", lambda e, hh=hh: e.bn_stats(out=st6[:, hh, :], in_=y[:, hh * 512:(hh + 1) * 512]), R=[yres], W=[sres])
        sc.op("dve", lambda e: e.bn_aggr(out=stat.ap()[:, 12:14], in_=stat.ap()[:, 0:12]), R=[sres], W=[sres])
        sc.op("act", lambda e: e.activation(out=stat.ap()[:, 14:15], in_=stat.ap()[:, 13:14], func=AF.Ln, bias=float(LN_EPS), scale=1.0),
              R=[sres], W=[sres])
        sc.op("act", lambda e: e.activation(out=stat.ap()[:, 15:16], in_=stat.ap()[:, 14:15], func=AF.Exp, scale=-0.5), R=[sres], W=[sres])
        sc.op("dve", lambda e: e.tensor_scalar(out=stat.ap()[:, 16:17], in0=stat.ap()[:, 12:13], scalar1=stat.ap()[:, 15:16], scalar2=-1.0,
                                               op0=ALU.mult, op1=ALU.mult), R=[sres], W=[sres])
        sc.op("act", lambda e: e.activation(out=y, in_=y, func=AF.Identity, bias=stat.ap()[:, 16:17], scale=stat.ap()[:, 15:16]), R=[yres, sres], W=[yres])
        sc.op("dve", lambda e: e.tensor_tensor(out=y, in0=y, in1=gb.ap()[:, 0, :], op=ALU.mult), R=[yres, "gb0"], W=[yres])
        sc.op("dve", lambda e: e.tensor_tensor(out=y, in0=y, in1=gb.ap()[:, 1, :], op=ALU.add), R=[yres, "gb1"], W=[yres])
        sc.dma("sp", lambda e: e.dma_start(out=dst[t * 128:(t + 1) * 128, :], in_=y), R=[yres], W=[], pool="st", n=4)
        if xbf is not None:
            xb, xbres = xbf
            sc.op("act", lambda e: e.activation(out=xb, in_=y, func=AF.Copy), R=[yres], W=[xbres])
            make_xT_tile(xb, xbres, t, lnbank)

    def ln_s1(y, yres, stat):
        sres = ("stat", stat.name)
        st6 = stat.ap()[:, 0:12].rearrange("p (a b) -> p a b", a=2)
        for hh in range(2):
            sc.op("dve", lambda e, hh=hh: e.bn_stats(out=st6[:, hh, :], in_=y[:, hh * 512:(hh + 1) * 512]), R=[yres], W=[sres])
        sc.op("dve", lambda e: e.bn_aggr(out=stat.ap()[:, 12:14], in_=stat.ap()[:, 0:12]), R=[sres], W=[sres])

    def ln_s2(y, yres, stat):
        sres = ("stat", stat.name)
        sc.op("act", lambda e: e.activation(out=stat.ap()[:, 14:15], in_=stat.ap()[:, 13:14], func=AF.Ln, bias=float(LN_EPS), scale=1.0),
              R=[sres], W=[sres])
        sc.op("act", lambda e: e.activation(out=stat.ap()[:, 15:16], in_=stat.ap()[:, 14:15], func=AF.Exp, scale=-0.5), R=[sres], W=[sres])
        sc.op("dve", lambda e: e.tensor_scalar(out=stat.ap()[:, 16:17], in0=stat.ap()[:, 12:13], scalar1=stat.ap()[:, 15:16], scalar2=-1.0,
                                               op0=ALU.mult, op1=ALU.mult), R=[sres], W=[sres])
        sc.op("act", lambda e: e.activation(out=y, in_=y, func=AF.Identity, bias=stat.ap()[:, 16:17], scale=stat.ap()[:, 15:16]), R=[yres, sres], W=[yres])

    def ln_s3(y, yres, gb, t, dst, xbf):
        sc.op("dve", lambda e: e.tensor_tensor(out=y, in0=y, in1=gb.ap()[:, 0, :], op=ALU.mult), R=[yres, "gb0"], W=[yres])
        sc.op("dve", lambda e: e.tensor_tensor(out=y, in0=y, in1=gb.ap()[:, 1, :], op=ALU.add), R=[yres, "gb1"], W=[yres])
        sc.dma("sp", lambda e: e.dma_start(out=dst[t * 128:(t + 1) * 128, :], in_=y), R=[yres], W=[], pool="st", n=4)
        if xbf is not None:
            xb, xbres = xbf
            sc.op("act", lambda e: e.activation(out=xb, in_=y, func=AF.Copy), R=[yres], W=[xbres])

    def ln_pipeline(pre, produce_a, produce, yb, ybres, stat, gb, dst, xbf):
        NB = len(yb)
        for i in range(-5, NT + 2):
            if pre is not None and 0 <= i + 5 < NT:
                pre(i + 5)
            if produce_a is not None and 0 <= i + 3 < NT:
                produce_a(i + 3)
            if 0 <= i + 1 < NT:
                t = i + 1
                ln_s2(yb[t % NB].ap(), ybres(t % NB), stat[t % NB])
            if 0 <= i + 2 < NT:
                t = i + 2
                produce(t)
                ln_s1(yb[t % NB].ap(), ybres(t % NB), stat[t % NB])
            if 0 <= i < NT:
                t = i
                ln_s3(yb[t % NB].ap(), ybres(t % NB), gb, t, dst, None if xbf is None else (xbf[t % NB].ap(), f"xbf{t % NB}"))
            if xbf is not None and 0 <= i - 1 < NT:
                t = i - 1
                make_xT_tile(xbf[t % NB].ap(), f"xbf{t % NB}", t, 6 + (t % 2))

    def load_gb(gb, g_ap, b_ap, layer):
        sc.dma("sp", lambda e: e.dma_start(out=gb.ap()[:, 0, :], in_=g_ap[layer:layer + 1, :].partition_broadcast(128)), W=["gb0"])
        sc.dma("sp", lambda e: e.dma_start(out=gb.ap()[:, 1, :], in_=b_ap[layer:layer + 1, :].partition_broadcast(128)), W=["gb1"])

    with ExitStack() as es:
        xld = [es.enter_context(sbt(f"xld{i}", [128, D], BF16)) for i in range(2)]
        for t in range(NT):
            b = t % 2
            sc.dma("pool", lambda e, b=b, t=t: e.dma_start(out=xld[b].ap(), in_=x_in[t * 128:(t + 1) * 128, :]), W=[f"xld{b}"])
            make_xT_tile(xld[b].ap(), f"xld{b}", t, 6 + b)
        sc.barrier()

    def attention_layer(layer):
        kind = "diff" if layer % 2 == 0 else "sb"
        W_ = attw[layer % 2]
        VW = 130 if kind == "diff" else 128
        with ExitStack() as es:
            oT = es.enter_context(sbt("oT", [128, 8, S], BF16))
            with ExitStack() as es2:
                ent = es2.enter_context
                qT = ent(sbt("qT", [128, 2, S], BF16))
                kT = ent(sbt("kT", [128, S], BF16))
                V = ent(sbt("V", [128, NT, VW], BF16))
                wqkv = [ent(sbt(f"wqkv{i}", [128, 3, 8, 128], BF16)) for i in range(2)]
                NB = 4
                if kind == "diff":
                    Pt = [ent(sbt(f"Pt{i}", [128, 512], BF16)) for i in range(NB)]
                    Ep = ent(sbt("Ep", [128, 8, 2, 128], F32))
                    gtmp = ent(sbt("gtmp", [128, GW], F32))
                    rb = ent(sbt("rb", [32, 128], F32))
                    rtab = ent(sbt("rtab", [32, 8], F32))
                    oh = ent(sbt("oh", [32, GW], F32))
                    ones32 = ent(sbt("ones32", [32, 128], F32))
                    cfar = ent(sbt("cfar", [128, 16], F32))
                    lam = ent(sbt("lam", [128, 8], F32))
                    lvec = ent(sbt("lvec", [128, 4, 64], F32))
                    gsub = ent(sbt("gsub", [128, 128], F32))
                    R1 = ent(sbt("R1", [128, 4, 128], F32))
                    ot = [ent(sbt(f"ot{i}", [128, 128], F32)) for i in range(4)]
                    sm = [ent(sbt(f"sm{i}", [128, 8], F32)) for i in range(4)]
                    junk = ent(sbt("junk", [128, 128], F32))
                else:
                    eb = [ent(sbt(f"eb{i}", [128, 512], BF16)) for i in range(1)]
                    spb = [ent(sbt(f"spb{i}", [128, 512], BF16)) for i in range(1)]
                    gb_ = [ent(sbt(f"gb_{i}", [128, 512], BF16)) for i in range(1)]
                    wb = [ent(sbt(f"wb{i}", [128, 512], BF16)) for i in range(1)]
                    eb2 = [ent(sbt(f"eb2_{i}", [128, 2, 512], BF16)) for i in range(3)]
                    sp2 = [ent(sbt(f"sp2_{i}", [128, 2, 512], BF16)) for i in range(3)]
                    g2b = [ent(sbt(f"g2b_{i}", [128, 2, 512], BF16)) for i in range(2)]
                    w2b = [ent(sbt(f"w2b_{i}", [128, 2, 512], BF16)) for i in range(2)]
                onb = [ent(sbt(f"onb{i}", [128, 128], BF16)) for i in range(4)]

                sc.op("pool", lambda e: e.memset(qT.ap()[:, 0, :], 0.0), W=["qT"])
                sc.op("dve", lambda e: e.memset(qT.ap()[:, 1, :], 0.0), W=["qT"])
                lam_init = 0.8 - 0.6 * math.exp(-0.3 * layer)
                if kind == "diff":
                    sc.dma("sp", lambda e: e.dma_start(out=lvec.ap(), in_=da_l.partition_broadcast(128)), W=["lvec"])
                    sc.dma("sp", lambda e: e.dma_start(out=gsub.ap(), in_=da_g[0:1, :].partition_broadcast(128)), W=["gsub"])
                    sc.op("dve", lambda e: e.tensor_scalar(out=gsub.ap(), in0=gsub.ap(), scalar1=float(1.0 - lam_init), scalar2=None, op0=ALU.mult),
                          R=["gsub"], W=["gsub"])
                    sc.op("dve", lambda e: e.tensor_tensor(out=lvec.ap()[:, 0, :], in0=lvec.ap()[:, 0, :], in1=lvec.ap()[:, 1, :], op=ALU.mult), R=["lvec"], W=["lvec"])
                    sc.op("dve", lambda e: e.tensor_tensor(out=lvec.ap()[:, 2, :], in0=lvec.ap()[:, 2, :], in1=lvec.ap()[:, 3, :], op=ALU.mult), R=["lvec"], W=["lvec"])
                    sc.op("dve", lambda e: e.tensor_reduce(out=lam.ap()[:, 0:1], in_=lvec.ap()[:, 0, :], axis=AX.X, op=ALU.add), R=["lvec"], W=["lam"])
                    sc.op("dve", lambda e: e.tensor_reduce(out=lam.ap()[:, 1:2], in_=lvec.ap()[:, 2, :], axis=AX.X, op=ALU.add), R=["lvec"], W=["lam"])
                    sc.op("act", lambda e: e.activation(out=lam.ap()[:, 2:4], in_=lam.ap()[:, 0:2], func=AF.Exp), R=["lam"], W=["lam"])
                    sc.op("dve", lambda e: e.scalar_tensor_tensor(out=lam.ap()[:, 4:5], in0=lam.ap()[:, 3:4], scalar=float(-lam_init), in1=lam.ap()[:, 2:3],
                                                                  op0=ALU.add, op1=ALU.subtract), R=["lam"], W=["lam"])
                    sc.dma("sp", lambda e: e.dma_start(out=rtab.ap(), in_=rel_table), W=["rtab"])
                    sc.dma("sp", lambda e: e.dma_start(out=oh.ap(), in_=c_oh), W=["oh"])
                    sc.dma("sp", lambda e: e.dma_start(out=cfar.ap()[:, 0:8], in_=rel_table[31:32, :].partition_broadcast(128)), W=["cfar"])
                    sc.op("dve", lambda e: e.tensor_scalar(out=cfar.ap()[:, 8:16], in0=cfar.ap()[:, 0:8], scalar1=-1.0, scalar2=None, op0=ALU.mult), R=["cfar"], W=["cfar"])
                    sc.op("dve", lambda e: e.memset(ones32.ap(), 1.0), W=["ones32"])
                    for h in range(8):
                        sc.op("dve", lambda e, h=h: e.tensor_scalar(out=rb.ap(), in0=ones32.ap(), scalar1=rtab.ap()[:, h:h + 1], scalar2=None, op0=ALU.mult),
                              R=["ones32", "rtab"], W=["rb"])
                        sc.op("pe", lambda e: e.matmul(bk(6)[:, 0:GW], lhsT=rb.ap(), rhs=oh.ap(), start=True, stop=True), R=["rb", "oh"], W=["bank6"])
                        sc.op("act", lambda e: e.activation(out=gtmp.ap(), in_=bk(6)[:, 0:GW], func=AF.Copy), R=["bank6"], W=["gtmp"])
                        sc.dma("sp", lambda e, h=h: e.dma_start(out=gd.ap()[h], in_=gtmp.ap()), R=["gtmp"], W=["gd"])
                        for Dd in range(2):
                            src = bass.AP(tensor=gd.ap().tensor, offset=h * 128 * GW + Dd * 128 + 127, ap=[[GW - 1, 128], [1, 128]])
                            sc.dma("sp", lambda e, h=h, Dd=Dd, src=src: e.dma_start(out=Ep.ap()[:, h, Dd, :], in_=src), R=["gd"], W=["Ep"])
                            sc.op("act", lambda e, h=h, Dd=Dd: e.activation(out=Ep.ap()[:, h, Dd, :], in_=Ep.ap()[:, h, Dd, :], func=AF.Exp,
                                                                             bias=cfar.ap()[:, 8 + h:9 + h], scale=1.0), R=["Ep", "cfar"], W=["Ep"])
                        sc.op("dve", lambda e, h=h: e.tensor_tensor(out=Ep.ap()[:, h, 0, :], in0=Ep.ap()[:, h, 0, :], in1=mask_le.ap(), op=ALU.mult),
                              R=["Ep", "mask_le"], W=["Ep"])
                    sc.op("dve", lambda e: e.memset(V.ap()[:, :, 128:130], 1.0), W=["Vones"])

                def load_w(j):
                    b = j % 2
                    for i, nm in enumerate(("wq", "wk", "wv")):
                        src = W_[nm].rearrange("(c p) f -> p c f", p=128)[:, :, j * 128:(j + 1) * 128]
                        sc.dma("pool", lambda e, i=i, src=src, b=b: e.dma_start(out=wqkv[b].ap()[:, i, :, :], in_=src), W=[(f"wqkv{b}", i)], pool="wqkv", n=6)

                def project(j):
                    b = j % 2
                    wres = f"wqkv{b}"
                    for which, dstT, scale in ((0, qT, 0.125), (1, kT, None)):
                        for tc in range(8):
                            pb = 6 + (tc % 2)
                            for c in range(8):
                                sc.op("pe", lambda e, c=c, tc=tc, pb=pb, which=which: e.matmul(
                                    bk(pb)[:, :], lhsT=wqkv[b].ap()[:, which, c, :], rhs=xT.ap()[:, c, tc * 512:(tc + 1) * 512],
                                    start=(c == 0), stop=(c == 7)),
                                    R=[(wres, which)] + [("xT", t) for t in range(4 * tc, 4 * tc + 4)], W=[f"bank{pb}"])
                            if scale is not None:
                                sc.op("act", lambda e, tc=tc, pb=pb: e.activation(out=qT.ap()[0:64, 0, tc * 512:(tc + 1) * 512], in_=bk(pb)[0:64, :], func=AF.Copy, scale=scale),
                                      R=[f"bank{pb}"], W=["qT"])
                                sc.op("dve", lambda e, tc=tc, pb=pb: e.tensor_scalar(out=qT.ap()[64:128, 1, tc * 512:(tc + 1) * 512], in0=bk(pb)[64:128, :], scalar1=float(scale), scalar2=None,
                                                                                  op0=ALU.mult), R=[f"bank{pb}"], W=["qT"])
                            else:
                                sc.op("dve", lambda e, tc=tc, pb=pb: e.tensor_copy(out=dstT.ap()[:, tc * 512:(tc + 1) * 512], in_=bk(pb)[:, :]),
                                      R=[f"bank{pb}"], W=["kT"])
                    for tg in range(8):
                        pb = 6 + (tg % 2)
                        for tt in range(4):
                            t = 4 * tg + tt
                            for c in range(8):
                                sc.op("pe", lambda e, c=c, t=t, tt=tt, pb=pb: e.matmul(
                                    bk(pb)[:, tt * 128:(tt + 1) * 128], lhsT=xT.ap()[:, c, t * 128:(t + 1) * 128], rhs=wqkv[b].ap()[:, 2, c, :],
                                    start=(c == 0), stop=(c == 7), skip_group_check=True),
                                    R=[(wres, 2), ("xT", t)], W=[f"bank{pb}"])
                        sc.op("act" if tg % 2 == 0 else "dve",
                              lambda e, tg=tg, pb=pb: (e.activation(out=V.ap()[:, 4 * tg:4 * tg + 4, 0:128], in_=bk(pb)[:, :].rearrange("p (a f) -> p a f", a=4), func=AF.Copy)
                                                       if tg % 2 == 0 else
                                                       e.tensor_copy(out=V.ap()[:, 4 * tg:4 * tg + 4, 0:128], in_=bk(pb)[:, :].rearrange("p (a f) -> p a f", a=4))),
                              R=[f"bank{pb}"], W=["V"])

                def finish_tile(j, t, src_bf, src_res):
                    sc.op("pe", lambda e: e.transpose(out=bkh(7)[:, 0:128], in_=src_bf, identity=ident.ap()), R=[src_res, "ident"], W=["bank7"])
                    sc.op("dve", lambda e: e.tensor_copy(out=oT.ap()[:, j, t * 128:(t + 1) * 128], in_=bkh(7)[:, 0:128]), R=["bank7"], W=[("oT", t)])

                def attn_diff(j):
                    units = []
                    for qc in range(8):
                        for c in range(2):
                            for kt in range(4 * qc + 4):
                                units.append((qc, c, kt))
                    n = len(units)
                    gidx = {}
                    for (qc, c, kt) in units:
                        gidx.setdefault((qc, c), len(gidx))

                    def stage_A(u):
                        qc, c, kt = units[u]
                        qlo = max(0, kt - 4 * qc)
                        zb = (0, 1, 6)[u % 3]
                        sc.op("pe", lambda e: e.matmul(bk(zb)[:, qlo * 128:512], lhsT=kT.ap()[:, kt * 128:(kt + 1) * 128],
                                                       rhs=qT.ap()[:, c, qc * 512 + qlo * 128:(qc + 1) * 512], start=True, stop=True),
                              R=["qT", "kT"], W=[f"bank{zb}"])

                    def stage_B(u):
                        qc, c, kt = units[u]
                        qlo = max(0, kt - 4 * qc)
                        zb = (0, 1, 6)[u % 3]
                        pb = u % NB
                        sc.op("act", lambda e: e.activation(out=Pt[pb].ap()[:, qlo * 128:512], in_=bk(zb)[:, qlo * 128:512], func=AF.Exp),
                              R=[f"bank{zb}"], W=[f"Pt{pb}"])
                        for Dd in range(2):
                            ql = kt + Dd - 4 * qc
                            if 0 <= ql <= 3:
                                sc.op("dve", lambda e, ql=ql, Dd=Dd: e.tensor_tensor(out=Pt[pb].ap()[:, ql * 128:(ql + 1) * 128], in0=Pt[pb].ap()[:, ql * 128:(ql + 1) * 128],
                                                                                    in1=Ep.ap()[:, j, Dd, :], op=ALU.mult),
                                      R=[f"Pt{pb}", "Ep"], W=[f"Pt{pb}"])

                    def stage_H(u):
                        qc, c, kt = units[u]
                        qlo = max(0, kt - 4 * qc)
                        pb = u % NB
                        g = gidx[(qc, c)]
                        ob = 2 + 2 * (g % 2)
                        for ql in range(qlo, 4):
                            bank = ob + ql // 2
                            col = (ql % 2) * 256
                            sc.op("pe", lambda e, ql=ql, bank=bank, col=col: e.matmul(
                                bk(bank)[:, col:col + 129], lhsT=Pt[pb].ap()[:, ql * 128:(ql + 1) * 128], rhs=V.ap()[:, kt, 0:129],
                                start=(kt == 0 and ql % 2 == 0), stop=(kt == 4 * qc + ql), skip_group_check=True),
                                R=[f"Pt{pb}", "V", "Vones"], W=[f"bank{bank}"])
                        if kt == 4 * qc + 3:
                            for ql in range(4):
                                pending.append((u + 1 + ql, (lambda ql=ql, c=c, qc=qc, ob=ob: evac_chain(ql, c, qc, ob))))

                    def evac_chain(ql, c, qc, ob):
                        bank = ob + ql // 2
                        col = (ql % 2) * 256
                        s_ = sm[ql]
                        sres = f"sm{ql}"
                        sc.op("dve", lambda e: e.reciprocal(out=s_.ap()[:, 0:1], in_=bk(bank)[:, col + 128:col + 129]), R=[f"bank{bank}"], W=[sres])
                        if c == 0:
                            sc.op("dve", lambda e: e.tensor_scalar(out=R1.ap()[:, ql, :], in0=bk(bank)[:, col:col + 128], scalar1=s_.ap()[:, 0:1], scalar2=None, op0=ALU.mult),
                                  R=[f"bank{bank}", sres], W=[("R1", ql)])
                            return
                        o_ = ot[ql]
                        ores = f"ot{ql}"
                        nb_ = onb[ql]
                        nres = f"onb{ql}"
                        sc.op("dve", lambda e: e.tensor_tensor(out=s_.ap()[:, 1:2], in0=s_.ap()[:, 0:1], in1=lam.ap()[:, 4:5], op=ALU.mult), R=[sres, "lam"], W=[sres])
                        sc.op("dve", lambda e: e.scalar_tensor_tensor(out=o_.ap(), in0=bk(bank)[:, col:col + 128], scalar=s_.ap()[:, 1:2], in1=R1.ap()[:, ql, :],
                                                                      op0=ALU.mult, op1=ALU.add), R=[f"bank{bank}", sres, ("R1", ql)], W=[ores])
                        sc.op("act", lambda e: e.activation(out=junk.ap(), in_=o_.ap(), func=AF.Square, accum_out=s_.ap()[:, 2:3]), R=[ores], W=[sres, "junk"])
                        sc.op("act", lambda e: e.activation(out=s_.ap()[:, 3:4], in_=s_.ap()[:, 2:3], func=AF.Ln, bias=float(RMS_EPS), scale=1.0 / 128.0),
                              R=[sres, "cst"], W=[sres])
                        sc.op("act", lambda e: e.activation(out=s_.ap()[:, 4:5], in_=s_.ap()[:, 3:4], func=AF.Exp, scale=-0.5), R=[sres], W=[sres])
                        def part2():
                            sc.op("dve", lambda e: e.scalar_tensor_tensor(out=nb_.ap(), in0=o_.ap(), scalar=s_.ap()[:, 4:5], in1=gsub.ap(), op0=ALU.mult, op1=ALU.mult),
                                  R=[ores, sres, "gsub"], W=[nres])
                            pending.append((cur[0] + 2, (lambda: finish_tile(j, 4 * qc + ql, nb_.ap(), nres))))
                        pending.append((cur[0] + 2, part2))

                    pending = []
                    cur = [0]

                    def run_pending(i, flush=False):
                        k = 0
                        while k < len(pending):
                            if flush or pending[k][0] <= i:
                                fn = pending.pop(k)[1]
                                fn()
                            else:
                                k += 1

                    for i in range(-3, n):
                        cur[0] = i
                        if 0 <= i + 3 < n:
                            stage_A(i + 3)
                        if 0 <= i + 2 < n:
                            stage_B(i + 2)
                        if 0 <= i < n:
                            stage_H(i)
                        run_pending(i)
                    cur[0] = n
                    while pending:
                        run_pending(n, flush=True)

                def attn_sb2(j):
                    pairs = [(qc, kt) for qc in range(8) for kt in range(4 * qc + 3, -1, -1)]
                    n = len(pairs)

                    def geo(p):
                        qc, kt = pairs[p]
                        return qc, kt, max(0, kt - 4 * qc)

                    def zb(p, c):
                        return (0, 1)[c] if p % 2 == 0 else (6, 7)[c]

                    def pair_ps(ti, lo):
                        return PP[ti].ap().rearrange("p (b f) -> p b f", b=2)[:, :, lo:512]

                    def st_A(p):
                        qc, kt, qlo = geo(p)
                        lo = qlo * 128
                        for c in range(2):
                            z = zb(p, c)
                            sc.op("pe", lambda e, c=c, z=z: e.matmul(bk(z)[:, lo:512], lhsT=kT.ap()[:, kt * 128:(kt + 1) * 128],
                                                                     rhs=qT.ap()[:, c, qc * 512 + lo:(qc + 1) * 512], start=True, stop=True),
                                  R=["qT", "kT"], W=[f"bank{z}"])

                    def st_B(p):
                        qc, kt, qlo = geo(p)
                        lo = qlo * 128
                        b = p % 3
                        sc.op("act", lambda e: e.activation(out=eb2[b].ap()[:, :, lo:512], in_=pair_ps(0 if p % 2 == 0 else 3, lo), func=AF.Exp),
                              R=[f"bank{zb(p, 0)}", f"bank{zb(p, 1)}"], W=[f"eb{b}"])
                        if kt >= 4 * qc:
                            sc.op("dve", lambda e: e.tensor_tensor(out=eb2[b].ap()[:, :, lo:lo + 128], in0=eb2[b].ap()[:, :, lo:lo + 128],
                                                                  in1=mask_lt.ap().unsqueeze(1).to_broadcast([128, 2, 128]), op=ALU.mult),
                                  R=[f"eb{b}", "mask_lt"], W=[f"eb{b}"])

                    def st_C(p):
                        qc, kt, qlo = geo(p)
                        lo = qlo * 128
                        b = p % 3
                        sc.op("act", lambda e: e.activation(out=sp2[b].ap()[:, :, lo:512], in_=eb2[b].ap()[:, :, lo:512], func=AF.Ln, bias=1.0, scale=1.0),
                              R=[f"eb{b}"], W=[f"spb{b}"])

                    def st_D(p):
                        qc, kt, qlo = geo(p)
                        lo = qlo * 128
                        b = p % 3
                        for c in range(2):
                            sc.op("pe", lambda e, c=c: e.matmul(bk(2 + c)[:, lo:512], lhsT=tri_incl.ap(), rhs=sp2[b].ap()[:, c, lo:512],
                                                                start=(kt == 4 * qc + 3), stop=False, skip_group_check=True),
                                  R=[f"spb{b}", "tri_incl"], W=[f"bank{2 + c}"])

                    def st_E(p):
                        qc, kt, qlo = geo(p)
                        lo = qlo * 128
                        b2 = p % 2
                        sc.op("act", lambda e: e.activation(out=g2b[b2].ap()[:, :, lo:512], in_=pair_ps(1, lo), func=AF.Exp, scale=-1.0),
                              R=["bank2", "bank3"], W=[f"gb_{b2}"])

                    def st_F(p):
                        qc, kt, qlo = geo(p)
                        lo = qlo * 128
                        b = p % 3
                        if kt == 0:
                            return
                        for c in range(2):
                            sc.op("pe", lambda e, c=c: e.matmul(bk(2 + c)[:, lo:512], lhsT=tri_low.ap(), rhs=sp2[b].ap()[:, c, lo:512],
                                                                start=False, stop=False, skip_group_check=True),
                                  R=[f"spb{b}", "tri_low"], W=[f"bank{2 + c}"])

                    def st_G(p):
                        qc, kt, qlo = geo(p)
                        lo = qlo * 128
                        b = p % 3
                        b2 = p % 2
                        sc.op("dve", lambda e: e.tensor_tensor(out=w2b[b2].ap()[:, :, lo:512], in0=eb2[b].ap()[:, :, lo:512], in1=g2b[b2].ap()[:, :, lo:512], op=ALU.mult),
                              R=[f"eb{b}", f"gb_{b2}"], W=[f"wb{b2}"])

                    def st_H(p):
                        qc, kt, qlo = geo(p)
                        b2 = p % 2
                        ob = 4 + (qc % 2)
                        for c in range(2):
                            for ql in range(qlo, 4):
                                col = (c * 4 + ql) * 64
                                sc.op("pe", lambda e, c=c, ql=ql, col=col: e.matmul(
                                    bk(ob)[:, col:col + 64], lhsT=w2b[b2].ap()[:, c, ql * 128:(ql + 1) * 128], rhs=V.ap()[:, kt, c * 64:(c + 1) * 64],
                                    start=(kt == 4 * qc + 3 and c == 0), stop=(kt == 0), skip_group_check=True),
                                    R=[f"wb{b2}", "V"], W=[f"bank{ob}"])
                        if kt == 0:
                            for ql in range(4):
                                pending.append((cur[0] + 1 + ql, (lambda ql=ql, qc=qc, ob=ob: evac_sb(ql, qc, ob))))

                    def evac_sb(ql, qc, ob):
                        nb_ = onb[ql]
                        nres = f"onb{ql}"
                        src = bass.AP(tensor=bk(ob).tensor, offset=bk(ob).offset + ql * 64, ap=[list(bk(ob).ap[0]), [256, 2], [1, 64]])
                        sc.op("dve", lambda e: e.tensor_copy(out=nb_.ap().rearrange("p (a f) -> p a f", a=2), in_=src), R=[f"bank{ob}"], W=[nres])
                        pending.append((cur[0] + 2, (lambda: finish_tile(j, 4 * qc + ql, nb_.ap(), nres))))

                    pending = []
                    cur = [0]

                    def run_pending(i, flush=False):
                        k = 0
                        while k < len(pending):
                            if flush or pending[k][0] <= i:
                                fn = pending.pop(k)[1]
                                fn()
                            else:
                                k += 1

                    for i in range(-2, n + 1):
                        cur[0] = i
                        if 0 <= i + 2 < n:
                            st_A(i + 2)
                        if 0 <= i + 1 < n:
                            st_B(i + 1)
                        if 0 <= i - 1 < n:
                            st_F(i - 1)
                        if 0 <= i < n:
                            st_D(i)
                            st_E(i)
                        if 0 <= i + 1 < n:
                            st_C(i + 1)
                        if 0 <= i < n:
                            st_G(i)
                        if 0 <= i - 1 < n:
                            st_H(i - 1)
                        run_pending(i)
                    cur[0] = n + 1
                    while pending:
                        run_pending(n + 1, flush=True)

                def attn_sb(j):
                    units = []
                    for qc in range(8):
                        for kt in range(4 * qc + 3, -1, -1):
                            for c in range(2):
                                units.append((qc, c, kt))
                    n = len(units)

                    def geo(u):
                        qc, c, kt = units[u]
                        return qc, c, kt, max(0, kt - 4 * qc)

                    def stage_A(u):
                        qc, c, kt, qlo = geo(u)
                        zb = (0, 1, 6)[u % 3]
                        sc.op("pe", lambda e: e.matmul(bk(zb)[:, qlo * 128:512], lhsT=kT.ap()[:, kt * 128:(kt + 1) * 128],
                                                       rhs=qT.ap()[:, c, qc * 512 + qlo * 128:(qc + 1) * 512], start=True, stop=True),
                              R=["qT", "kT"], W=[f"bank{zb}"])

                    def stage_B(u):
                        qc, c, kt, qlo = geo(u)
                        zb = (0, 1, 6)[u % 3]
                        b = u % NB
                        sc.op("act", lambda e: e.activation(out=eb[b].ap()[:, qlo * 128:512], in_=bk(zb)[:, qlo * 128:512], func=AF.Exp),
                              R=[f"bank{zb}"], W=[f"eb{b}"])
                        if kt >= 4 * qc:
                            sc.op("dve", lambda e: e.tensor_tensor(out=eb[b].ap()[:, qlo * 128:(qlo + 1) * 128], in0=eb[b].ap()[:, qlo * 128:(qlo + 1) * 128],
                                                                  in1=mask_lt.ap(), op=ALU.mult), R=[f"eb{b}", "mask_lt"], W=[f"eb{b}"])

                    def stage_C(u):
                        qc, c, kt, qlo = geo(u)
                        b = u % NB
                        sc.op("act", lambda e: e.activation(out=spb[b].ap()[:, qlo * 128:512], in_=eb[b].ap()[:, qlo * 128:512], func=AF.Ln, bias=1.0, scale=1.0),
                              R=[f"eb{b}"], W=[f"spb{b}"])

                    def stage_D(u):
                        qc, c, kt, qlo = geo(u)
                        b = u % NB
                        cb = 2 + c
                        sc.op("pe", lambda e: e.matmul(bk(cb)[:, qlo * 128:512], lhsT=tri_incl.ap(), rhs=spb[b].ap()[:, qlo * 128:512],
                                                       start=(kt == 4 * qc + 3), stop=False, skip_group_check=True),
                              R=[f"spb{b}", "tri_incl"], W=[f"bank{cb}"])

                    def stage_E(u):
                        qc, c, kt, qlo = geo(u)
                        b = u % NB
                        cb = 2 + c
                        sc.op("act", lambda e: e.activation(out=gb_[b].ap()[:, qlo * 128:512], in_=bk(cb)[:, qlo * 128:512], func=AF.Exp, scale=-1.0),
                              R=[f"bank{cb}"], W=[f"gb_{b}"])

                    def stage_F(u):
                        qc, c, kt, qlo = geo(u)
                        b = u % NB
                        cb = 2 + c
                        if kt == 0:
                            return
                        sc.op("pe", lambda e: e.matmul(bk(cb)[:, qlo * 128:512], lhsT=tri_low.ap(), rhs=spb[b].ap()[:, qlo * 128:512],
                                                       start=False, stop=False, skip_group_check=True),
                              R=[f"spb{b}", "tri_low"], W=[f"bank{cb}"])

                    def stage_G(u):
                        qc, c, kt, qlo = geo(u)
                        b = u % NB
                        sc.op("dve", lambda e: e.tensor_tensor(out=wb[b].ap()[:, qlo * 128:512], in0=eb[b].ap()[:, qlo * 128:512], in1=gb_[b].ap()[:, qlo * 128:512], op=ALU.mult),
                              R=[f"eb{b}", f"gb_{b}"], W=[f"wb{b}"])

                    def stage_H(u):
                        qc, c, kt, qlo = geo(u)
                        b = u % NB
                        ob = 4 + (qc % 2)
                        for ql in range(qlo, 4):
                            col = (c * 4 + ql) * 64
                            sc.op("pe", lambda e, ql=ql, col=col: e.matmul(
                                bk(ob)[:, col:col + 64], lhsT=wb[b].ap()[:, ql * 128:(ql + 1) * 128], rhs=V.ap()[:, kt, c * 64:(c + 1) * 64],
                                start=(kt == 4 * qc + 3 and c == 0), stop=(kt == 0), skip_group_check=True),
                                R=[f"wb{b}", "V"], W=[f"bank{ob}"])
                        if kt == 0 and c == 1:
                            for ql in range(4):
                                pending.append((cur[0] + 1 + ql, (lambda ql=ql, qc=qc, ob=ob: evac_sb(ql, qc, ob))))

                    def evac_sb(ql, qc, ob):
                        nb_ = onb[ql]
                        nres = f"onb{ql}"
                        src = bass.AP(tensor=bk(ob).tensor, offset=bk(ob).offset + ql * 64, ap=[list(bk(ob).ap[0]), [256, 2], [1, 64]])
                        sc.op("dve", lambda e: e.tensor_copy(out=nb_.ap().rearrange("p (a f) -> p a f", a=2), in_=src), R=[f"bank{ob}"], W=[nres])
                        pending.append((cur[0] + 2, (lambda: finish_tile(j, 4 * qc + ql, nb_.ap(), nres))))

                    pending = []
                    cur = [0]

                    def run_pending(i, flush=False):
                        k = 0
                        while k < len(pending):
                            if flush or pending[k][0] <= i:
                                fn = pending.pop(k)[1]
                                fn()
                            else:
                                k += 1

                    for i in range(-4, n + 1):
                        cur[0] = i
                        if 0 <= i + 3 < n:
                            stage_A(i + 3)
                        if 0 <= i + 2 < n:
                            stage_B(i + 2)
                        if 0 <= i + 1 < n:
                            stage_D(i + 1)
                            stage_E(i + 1)
                        if 0 <= i + 2 < n:
                            stage_C(i + 2)
                        if 0 <= i < n:
                            stage_F(i)
                            stage_G(i)
                        if 0 <= i - 1 < n:
                            stage_H(i - 1)
                        run_pending(i)
                    cur[0] = n + 1
                    while pending:
                        run_pending(n + 1, flush=True)

                def convert_experts(j):
                    for ei in range(4 * j, 4 * j + 4):
                        for wi, (wsrc, nch) in enumerate(((w1, 8), (w3, 8), (w2, 4))):
                            dstv = Wc[wi][ei * 128:(ei + 1) * 128, :].rearrange("p (c h) -> p c h", c=nch)
                            srcv = wsrc[layer, ei].rearrange("(c p) h -> p c h", p=128)
                            sc.dma("pool", lambda e, dstv=dstv, srcv=srcv: e.dma_start(out=dstv, in_=srcv), R=[("oT", NT - 1)], W=[("Wc", wi, ei)], pool="cv", n=8)

                load_w(0)
                for j in range(8):
                    if j + 1 < 8:
                        load_w(j + 1)
                    project(j)
                    if kind == "diff":
                        attn_diff(j)
                    else:
                        attn_sb2(j)
                    if j < 7:
                        convert_experts(j)
                    if j == 6:
                        convert_experts(7)
                sc.barrier()

            with ExitStack() as es3:
                ent = es3.enter_context
                wo = ent(sbt("wo", [128, 8, D], BF16))
                gb = ent(sbt("gb", [128, 2, D], F32))
                NBW = 4
                stat = [ent(sbt(f"stat{i}", [128, 20], F32)) for i in range(NBW)]
                xres = [ent(sbt(f"xres{i}", [128, D], F32)) for i in range(NBW)]
                yb = [ent(sbt(f"yb{i}", [128, D], F32)) for i in range(NBW)]
                xbf = [ent(sbt(f"xbf{i}", [128, D], BF16)) for i in range(NBW)]
                for c in range(8):
                    sc.dma("pool", lambda e, c=c: e.dma_start(out=wo.ap()[:, c, :], in_=W_["wo"][c * 128:(c + 1) * 128, :]), W=[("wo", c)], pool="wo", n=8)
                load_gb(gb, ln_mix_g, ln_mix_b, layer)
                src = xr[2 * layer]
                dst = xr[2 * layer + 1]
                def wo_produce_a(t):
                    b = t % NBW
                    sc.dma("sp", lambda e: e.dma_start(out=xres[b].ap(), in_=src[t * 128:(t + 1) * 128, :]), W=[f"xres{b}"], pool="xr", n=4)
                    for hh in range(2):
                        pb = 2 * (t % 2) + hh
                        for jj in range(8):
                            sc.op("pe", lambda e, jj=jj, hh=hh, pb=pb: e.matmul(
                                bk(pb)[:, :], lhsT=oT.ap()[:, jj, t * 128:(t + 1) * 128], rhs=wo.ap()[:, jj, hh * 512:(hh + 1) * 512],
                                start=(jj == 0), stop=(jj == 7)), R=[("oT", t), ("wo", jj)], W=[f"bank{pb}"])

                def wo_produce(t):
                    b = t % NBW
                    for hh in range(2):
                        pb = 2 * (t % 2) + hh
                        sc.op("dve", lambda e, hh=hh, pb=pb: e.scalar_tensor_tensor(
                            out=yb[b].ap()[:, hh * 512:(hh + 1) * 512], in0=xres[b].ap()[:, hh * 512:(hh + 1) * 512], scalar=float(ALPHA),
                            in1=bk(pb)[:, :], op0=ALU.mult, op1=ALU.add), R=[f"xres{b}", f"bank{pb}"], W=[f"yb{b}"])

                ln_pipeline(None, wo_produce_a, wo_produce, yb, (lambda b: f"yb{b}"), stat, gb, dst, xbf)
                sc.barrier()

    def moe_layer(layer):
        with ExitStack() as es:
            ent = es.enter_context
            TG = 8
            acc = ent(sbt("acc", [128, TG, D], F32))
            wA = [ent(sbt(f"wA{i}", [128, 2, 8, HID], BF16)) for i in range(2)]
            wB = [ent(sbt(f"wB{i}", [128, 4, D], BF16)) for i in range(2)]
            sl = [ent(sbt(f"sl{i}", [128, 4, 512], BF16)) for i in range(2)]
            gg = [ent(sbt(f"gg{i}", [128, 4, 512], BF16)) for i in range(2)]
            wr = ent(sbt("wr", [128, 8, 36], BF16))
            br = ent(sbt("br", [128, 36], F32))
            lg = ent(sbt("lg", [128, NT, 36], F32))
            gates = ent(sbt("gates", [128, NT, NE], F32))
            rt = ent(sbt("rt", [128, 8, 64], F32))
            gb = ent(sbt("gb", [128, 2, D], F32))
            stat = ent(sbt("stat", [128, 20], F32))
            xres = [ent(sbt(f"xres{i}", [128, D], F32)) for i in range(2)]
            xbf = [ent(sbt(f"xbf{i}", [128, D], BF16)) for i in range(2)]

            sc.dma("pool", lambda e: e.dma_start(out=wr.ap()[:, :, 0:4], in_=w_group[layer].rearrange("(c p) g -> p c g", p=128)), W=["wr"])
            sc.dma("pool", lambda e: e.dma_start(out=wr.ap()[:, :, 4:36], in_=w_expert[layer].rearrange("(c p) g -> p c g", p=128)), W=["wr"])
            sc.dma("sp", lambda e: e.dma_start(out=br.ap()[:, 0:4], in_=b_group[layer:layer + 1, :].partition_broadcast(128)), W=["br"])
            sc.dma("sp", lambda e: e.dma_start(out=br.ap()[:, 4:36], in_=b_expert[layer:layer + 1, :].partition_broadcast(128)), W=["br"])
            load_gb(gb, ln_ffn_g, ln_ffn_b, layer)
            for t in range(NT):
                pb = 6 + (t % 2)
                for c in range(8):
                    sc.op("pe", lambda e, c=c, t=t, pb=pb: e.matmul(bk(pb)[:, 0:36], lhsT=xT.ap()[:, c, t * 128:(t + 1) * 128], rhs=wr.ap()[:, c, :],
                                                                    start=(c == 0), stop=(c == 7)), R=[("xT", t), "wr"], W=[f"bank{pb}"])
                sc.op("dve", lambda e, t=t, pb=pb: e.tensor_tensor(out=lg.ap()[:, t, :], in0=bk(pb)[:, 0:36], in1=br.ap(), op=ALU.add),
                      R=[f"bank{pb}", "br"], W=["lg"])
            for t in range(NT):
                r_ = rt.ap()
                L = lg.ap()[:, t, :]
                ops = []
                sc.op("dve", lambda e, L=L: e.tensor_reduce(out=r_[:, 0, 0:1], in_=L[:, 0:4], axis=AX.X, op=ALU.max), R=["lg"], W=["rt"])
                sc.op("dve", lambda e, L=L: e.tensor_scalar(out=r_[:, 0, 4:8], in0=L[:, 0:4], scalar1=r_[:, 0, 0:1], scalar2=None, op0=ALU.is_equal), R=["lg", "rt"], W=["rt"])
                sc.op("dve", lambda e, L=L: e.tensor_scalar(out=r_[:, 0, 8:12], in0=L[:, 0:4], scalar1=r_[:, 0, 0:1], scalar2=None, op0=ALU.subtract), R=["lg", "rt"], W=["rt"])
                sc.op("act", lambda e: e.activation(out=r_[:, 0, 8:12], in_=r_[:, 0, 8:12], func=AF.Exp, accum_out=r_[:, 0, 1:2]), R=["rt"], W=["rt"])
                sc.op("dve", lambda e: e.reciprocal(out=r_[:, 0, 2:3], in_=r_[:, 0, 1:2]), R=["rt"], W=["rt"])
                sc.op("dve", lambda e: e.tensor_scalar(out=r_[:, 0, 12:16], in0=r_[:, 0, 4:8], scalar1=-1.0, scalar2=1e30, op0=ALU.add, op1=ALU.mult), R=["rt"], W=["rt"])
                sc.op("dve", lambda e, L=L: e.tensor_tensor(out=r_[:, 1, 0:32].rearrange("p (g e) -> p g e", g=4), in0=L[:, 4:36].rearrange("p (g e) -> p g e", g=4),
                                                            in1=r_[:, 0, 12:16].unsqueeze(2).to_broadcast([128, 4, 8]), op=ALU.add), R=["lg", "rt"], W=["rt"])
                sc.op("dve", lambda e: e.max(out=r_[:, 2, 0:8], in_=r_[:, 1, 0:32]), R=["rt"], W=["rt"])
                sc.op("dve", lambda e: e.tensor_scalar(out=r_[:, 3, 0:32], in0=r_[:, 1, 0:32], scalar1=r_[:, 2, 0:1], scalar2=None, op0=ALU.is_equal), R=["rt"], W=["rt"])
                sc.op("dve", lambda e: e.tensor_scalar(out=r_[:, 4, 0:32], in0=r_[:, 1, 0:32], scalar1=r_[:, 2, 1:2], scalar2=None, op0=ALU.is_equal), R=["rt"], W=["rt"])
                sc.op("dve", lambda e: e.tensor_tensor(out=r_[:, 2, 8:9], in0=r_[:, 2, 1:2], in1=r_[:, 2, 0:1], op=ALU.subtract), R=["rt"], W=["rt"])
                sc.op("act", lambda e: e.activation(out=r_[:, 2, 9:10], in_=r_[:, 2, 8:9], func=AF.Exp), R=["rt"], W=["rt"])
                sc.op("dve", lambda e: e.tensor_scalar(out=r_[:, 2, 10:11], in0=r_[:, 2, 9:10], scalar1=1.0, scalar2=None, op0=ALU.add), R=["rt"], W=["rt"])
                sc.op("dve", lambda e: e.reciprocal(out=r_[:, 2, 11:12], in_=r_[:, 2, 10:11]), R=["rt"], W=["rt"])
                sc.op("dve", lambda e: e.tensor_tensor(out=r_[:, 2, 12:13], in0=r_[:, 2, 11:12], in1=r_[:, 0, 2:3], op=ALU.mult), R=["rt"], W=["rt"])
                sc.op("dve", lambda e: e.tensor_tensor(out=r_[:, 2, 13:14], in0=r_[:, 0, 2:3], in1=r_[:, 2, 12:13], op=ALU.subtract), R=["rt"], W=["rt"])
                sc.op("dve", lambda e: e.tensor_scalar(out=r_[:, 3, 0:32], in0=r_[:, 3, 0:32], scalar1=r_[:, 2, 12:13], scalar2=None, op0=ALU.mult), R=["rt"], W=["rt"])
                sc.op("dve", lambda e, t=t: e.scalar_tensor_tensor(out=gates.ap()[:, t, :], in0=r_[:, 4, 0:32], scalar=r_[:, 2, 13:14], in1=r_[:, 3, 0:32],
                                                                   op0=ALU.mult, op1=ALU.add), R=["rt"], W=[("gates", t)])

            def load_expert(ei, b):
                sc.dma("pool", lambda e: e.dma_start(out=wA[b].ap()[:, 0, :, :], in_=w1[layer, ei].rearrange("(c p) h -> p c h", p=128)), W=[f"wA{b}"])
                sc.dma("pool", lambda e: e.dma_start(out=wA[b].ap()[:, 1, :, :], in_=w3[layer, ei].rearrange("(c p) h -> p c h", p=128)), W=[f"wA{b}"])
                sc.dma("pool", lambda e: e.dma_start(out=wB[b].ap(), in_=w2[layer, ei].rearrange("(c p) d -> p c d", p=128)), W=[f"wB{b}"])

            src = xr[2 * layer + 1]
            dst = xr[2 * layer + 2]
            last = (layer == DEPTH - 1)
            ngroups = NT // TG
            it = 0
            load_expert(0, 0)
            for G in range(ngroups):
                for ei in range(NE):
                    b = it % 2
                    nxt = it + 1
                    if nxt < ngroups * NE:
                        load_expert(nxt % NE, nxt % 2)
                    for half in range(TG // 4):
                        tok0 = (G * TG + half * 4) * 128
                        hb = (it * (TG // 4) + half) % 2
                        for m in range(4):
                            for which in range(2):
                                pb = 2 * which + (m % 2)
                                for c in range(8):
                                    sc.op("pe", lambda e, c=c, m=m, which=which, pb=pb: e.matmul(
                                        bk(pb)[:, :], lhsT=wA[b].ap()[:, which, c, m * 128:(m + 1) * 128], rhs=xT.ap()[:, c, tok0:tok0 + 512],
                                        start=(c == 0), stop=(c == 7)),
                                        R=[f"wA{b}"] + [("xT", tok0 // 128 + q) for q in range(4)], W=[f"bank{pb}"])
                            sc.op("act", lambda e, m=m, hb=hb: e.activation(out=sl[hb].ap()[:, m, :], in_=bk(m % 2)[:, :], func=AF.Silu),
                                  R=[f"bank{m % 2}"], W=[f"sl{hb}"])
                            sc.op("dve", lambda e, m=m, hb=hb: e.tensor_tensor(out=gg[hb].ap()[:, m, :], in0=sl[hb].ap()[:, m, :], in1=bk(2 + m % 2)[:, :], op=ALU.mult),
                                  R=[f"sl{hb}", f"bank{2 + m % 2}"], W=[f"gg{hb}"])
                        for tt in range(4):
                            tl = half * 4 + tt
                            tglob = G * TG + tl
                            for hh in range(2):
                                pb = 4 + hh
                                for m in range(4):
                                    sc.op("pe", lambda e, m=m, tt=tt, hh=hh, pb=pb: e.matmul(
                                        bk(pb)[:, :], lhsT=gg[hb].ap()[:, m, tt * 128:(tt + 1) * 128], rhs=wB[b].ap()[:, m, hh * 512:(hh + 1) * 512],
                                        start=(m == 0), stop=(m == 3)), R=[f"gg{hb}", f"wB{b}"], W=[f"bank{pb}"])
                                if ei == 0:
                                    sc.op("dve", lambda e, tl=tl, hh=hh, pb=pb, tglob=tglob: e.tensor_scalar(
                                        out=acc.ap()[:, tl, hh * 512:(hh + 1) * 512], in0=bk(pb)[:, :], scalar1=gates.ap()[:, tglob, ei:ei + 1], scalar2=None, op0=ALU.mult),
                                        R=[f"bank{pb}", ("gates", tglob)], W=[("acc", tl)])
                                else:
                                    sc.op("dve", lambda e, tl=tl, hh=hh, pb=pb, tglob=tglob, ei=ei: e.scalar_tensor_tensor(
                                        out=acc.ap()[:, tl, hh * 512:(hh + 1) * 512], in0=bk(pb)[:, :], scalar=gates.ap()[:, tglob, ei:ei + 1],
                                        in1=acc.ap()[:, tl, hh * 512:(hh + 1) * 512], op0=ALU.mult, op1=ALU.add),
                                        R=[f"bank{pb}", ("gates", tglob), ("acc", tl)], W=[("acc", tl)])
                    it += 1
                for tl in range(TG):
                    t = G * TG + tl
                    b2 = t % 2
                    sc.dma("sp", lambda e, b2=b2, t=t: e.dma_start(out=xres[b2].ap(), in_=src[t * 128:(t + 1) * 128, :]), W=[f"xres{b2}"], pool="xr", n=4)
                    sc.op("dve", lambda e, b2=b2, tl=tl: e.scalar_tensor_tensor(out=acc.ap()[:, tl, :], in0=xres[b2].ap(), scalar=float(ALPHA), in1=acc.ap()[:, tl, :],
                                                                                op0=ALU.mult, op1=ALU.add), R=[f"xres{b2}", ("acc", tl)], W=[("acc", tl)])
                    ln_tile(acc.ap()[:, tl, :], ("acc", tl), gb, stat, t, dst, None if last else (xbf[b2].ap(), f"xbf{b2}"), 6 + b2)
            sc.barrier()

    def moe_layer_sparse(layer):
        src = xr[2 * layer + 1]
        dst = xr[2 * layer + 2]
        last = (layer == DEPTH - 1)
        w13rows = [w1.rearrange("l e d h -> (l e d) h"), w3.rearrange("l e d h -> (l e d) h")]
        w2rows = w2.rearrange("l e h d -> (l e h) d")
        with ExitStack() as es:
            ent = es.enter_context
            WT = ent(sbt("WT", [128, NT, 2], F32))
            posi = ent(sbt("posi", [128, 2, NT], I32))
            widx = ent(sbt("widx", [128, 2, NSLOT], I32))
            with ExitStack() as es1:
                ent1 = es1.enter_context
                M1 = ent1(sbt("M1", [128, NT, NE], F32))
                M2 = ent1(sbt("M2", [128, NT, NE], F32))
                wr = ent1(sbt("wr", [128, 8, 36], BF16))
                br = ent1(sbt("br", [128, 36], F32))
                lg = ent1(sbt("lg", [128, NT, 36], F32))
                rs = ent1(sbt("rs", [128, 6, NT], F32))
                r4 = ent1(sbt("r4", [128, 3, NT, 4], F32))
                elm = ent1(sbt("elm", [128, NT, NE], F32))
                Abf = ent1(sbt("Abf", [128, NT, NE], BF16))
                ones_bf = ent1(sbt("ones_bf", [128, 128], BF16))
                cntS = ent1(sbt("cntS", [128, NE], F32))
                nsl = ent1(sbt("nsl", [128, NE], F32))
                offe = ent1(sbt("offe", [128, NE], F32))
                posb = ent1(sbt("posb", [128, NE], F32))
                posfull = ent1(sbt("posfull", [128, NT, NE], F32))
                ptmp = ent1(sbt("ptmp", [128, NT, NE], F32))
                posf = ent1(sbt("posf", [128, 2, NT], F32))
                sidx = ent1(sbt("sidx", [128, NSLOT], F32))
                pidx = ent1(sbt("pidx", [128, 4], F32))
                cmp_ = ent1(sbt("cmp", [128, NSLOT, NE], F32))
                eidf = ent1(sbt("eidf", [128, 3, NSLOT], F32))
                xres = [ent1(sbt(f"xres{i}", [128, D], F32)) for i in range(4)]
                xbf = [ent1(sbt(f"xbf{i}", [128, D], BF16)) for i in range(8)]

                sc.dma("pool", lambda e: e.dma_start(out=wr.ap()[:, :, 0:4], in_=w_group[layer].rearrange("(c p) g -> p c g", p=128)), W=["wr"])
                sc.dma("pool", lambda e: e.dma_start(out=wr.ap()[:, :, 4:36], in_=w_expert[layer].rearrange("(c p) g -> p c g", p=128)), W=["wr"])
                sc.dma("sp", lambda e: e.dma_start(out=br.ap()[:, 0:4], in_=b_group[layer:layer + 1, :].partition_broadcast(128)), W=["br"])
                sc.dma("sp", lambda e: e.dma_start(out=br.ap()[:, 4:36], in_=b_expert[layer:layer + 1, :].partition_broadcast(128)), W=["br"])
                sc.dma("sp", lambda e: e.dma_start(out=sidx.ap(), in_=c_sidx), W=["sidx"])
                sc.dma("sp", lambda e: e.dma_start(out=pidx.ap()[:, 0:1], in_=c_pidx), W=["pidx"])
                sc.op("dve", lambda e: e.memset(ones_bf.ap(), 1.0), W=["ones_bf"])
                for t in range(NT):
                    pb = 6 + (t % 2)
                    for c in range(8):
                        sc.op("pe", lambda e, c=c, t=t, pb=pb: e.matmul(bk(pb)[:, 0:36], lhsT=xT.ap()[:, c, t * 128:(t + 1) * 128], rhs=wr.ap()[:, c, :],
                                                                        start=(c == 0), stop=(c == 7)), R=[("xT", t), "wr"], W=[f"bank{pb}"])
                    sc.op("dve", lambda e, t=t, pb=pb: e.tensor_tensor(out=lg.ap()[:, t, :], in0=bk(pb)[:, 0:36], in1=br.ap(), op=ALU.add),
                          R=[f"bank{pb}", "br"], W=["lg"])
                LG = lg.ap()
                gmax = rs.ap()[:, 0, :]
                gsum = rs.ap()[:, 1, :]
                ggate = rs.ap()[:, 2, :]
                m1 = rs.ap()[:, 3, :]
                m2 = rs.ap()[:, 4, :]
                rr = rs.ap()[:, 5, :]
                gm = r4.ap()[:, 0, :, :]
                dd = r4.ap()[:, 1, :, :]
                pen = r4.ap()[:, 2, :, :]
                sc.op("dve", lambda e: e.tensor_reduce(out=gmax, in_=LG[:, :, 0:4], axis=AX.X, op=ALU.max), R=["lg"], W=["rs"])
                sc.op("dve", lambda e: e.tensor_tensor(out=gm, in0=LG[:, :, 0:4], in1=gmax.unsqueeze(2).to_broadcast([128, NT, 4]), op=ALU.is_equal), R=["lg", "rs"], W=["r4"])
                sc.op("dve", lambda e: e.tensor_tensor(out=dd, in0=LG[:, :, 0:4], in1=gmax.unsqueeze(2).to_broadcast([128, NT, 4]), op=ALU.subtract), R=["lg", "rs"], W=["r4"])
                sc.op("act", lambda e: e.activation(out=dd, in_=dd, func=AF.Exp), R=["r4"], W=["r4"])
                sc.op("dve", lambda e: e.tensor_reduce(out=gsum, in_=dd, axis=AX.X, op=ALU.add), R=["r4"], W=["rs"])
                sc.op("dve", lambda e: e.reciprocal(out=ggate, in_=gsum), R=["rs"], W=["rs"])
                sc.op("dve", lambda e: e.tensor_scalar(out=pen, in0=gm, scalar1=-1.0, scalar2=1e30, op0=ALU.add, op1=ALU.mult), R=["r4"], W=["r4"])
                elm4 = elm.ap().rearrange("p t (g e) -> p t g e", g=4)
                sc.op("dve", lambda e: e.tensor_tensor(out=elm4, in0=LG[:, :, 4:36].rearrange("p t (g e) -> p t g e", g=4),
                                                       in1=pen.unsqueeze(3).to_broadcast([128, NT, 4, 8]), op=ALU.add), R=["lg", "r4"], W=["elm"])
                sc.op("dve", lambda e: e.tensor_reduce(out=m1, in_=elm.ap(), axis=AX.X, op=ALU.max), R=["elm"], W=["rs"])
                sc.op("dve", lambda e: e.tensor_tensor(out=M1.ap(), in0=elm.ap(), in1=m1.unsqueeze(2).to_broadcast([128, NT, NE]), op=ALU.is_equal), R=["elm", "rs"], W=["M1"])
                sc.op("dve", lambda e: e.scalar_tensor_tensor(out=elm.ap(), in0=M1.ap(), scalar=-1e30, in1=elm.ap(), op0=ALU.mult, op1=ALU.add), R=["elm", "M1"], W=["elm"])
                sc.op("dve", lambda e: e.tensor_reduce(out=m2, in_=elm.ap(), axis=AX.X, op=ALU.max), R=["elm"], W=["rs"])
                sc.op("dve", lambda e: e.tensor_tensor(out=M2.ap(), in0=elm.ap(), in1=m2.unsqueeze(2).to_broadcast([128, NT, NE]), op=ALU.is_equal), R=["elm", "rs"], W=["M2"])
                sc.op("dve", lambda e: e.tensor_tensor(out=rr, in0=m2, in1=m1, op=ALU.subtract), R=["rs"], W=["rs"])
                sc.op("act", lambda e: e.activation(out=rr, in_=rr, func=AF.Exp), R=["rs"], W=["rs"])
                sc.op("dve", lambda e: e.tensor_scalar(out=rr, in0=rr, scalar1=1.0, scalar2=None, op0=ALU.add), R=["rs"], W=["rs"])
                sc.op("dve", lambda e: e.reciprocal(out=rr, in_=rr), R=["rs"], W=["rs"])
                sc.op("dve", lambda e: e.tensor_tensor(out=WT.ap()[:, :, 0], in0=rr, in1=ggate, op=ALU.mult), R=["rs"], W=["WT"])
                sc.op("dve", lambda e: e.tensor_tensor(out=WT.ap()[:, :, 1], in0=ggate, in1=WT.ap()[:, :, 0], op=ALU.subtract), R=["rs", "WT"], W=["WT"])
                sc.op("dve", lambda e: e.tensor_tensor(out=Abf.ap(), in0=M1.ap(), in1=M2.ap(), op=ALU.add), R=["M1", "M2"], W=["Abf"])
                for t in range(NT):
                    rb_ = t // 16
                    col = (t % 16) * 32
                    for tp in range(t):
                        sc.op("pe", lambda e, tp=tp, rb_=rb_, col=col: e.matmul(bk(rb_)[:, col:col + 32], lhsT=ones_bf.ap(), rhs=Abf.ap()[:, tp, :],
                                                                                  start=(tp == 0), stop=False, skip_group_check=True),
                              R=["Abf", "ones_bf"], W=[f"bank{rb_}"])
                    sc.op("pe", lambda e, t=t, rb_=rb_, col=col: e.matmul(bk(rb_)[:, col:col + 32], lhsT=tri_low.ap(), rhs=Abf.ap()[:, t, :],
                                                                          start=(t == 0), stop=True, skip_group_check=True),
                          R=["Abf", "tri_low"], W=[f"bank{rb_}"])
                for t in range(NT):
                    sc.op("pe", lambda e, t=t: e.matmul(bk(2)[:, 0:32], lhsT=ones_bf.ap(), rhs=Abf.ap()[:, t, :], start=(t == 0), stop=(t == NT - 1)),
                          R=["Abf", "ones_bf"], W=["bank2"])
                sc.op("dve", lambda e: e.tensor_copy(out=cntS.ap(), in_=bk(2)[:, 0:32]), R=["bank2"], W=["cntS"])
                sc.op("dve", lambda e: e.tensor_scalar(out=nsl.ap(), in0=cntS.ap(), scalar1=0.0, scalar2=None, op0=ALU.is_gt), R=["cntS"], W=["nsl"])
                for k in range(1, KMAX):
                    sc.op("dve", lambda e, k=k: e.scalar_tensor_tensor(out=nsl.ap(), in0=cntS.ap(), scalar=float(k * SL), in1=nsl.ap(), op0=ALU.is_gt, op1=ALU.add),
                          R=["cntS", "nsl"], W=["nsl"])
                sc.op("dve", lambda e: e.tensor_copy(out=offe.ap()[:, 0:1], in_=nsl.ap()[:, 0:1]), R=["nsl"], W=["offe"])
                for ei in range(1, NE):
                    sc.op("dve", lambda e, ei=ei: e.tensor_tensor(out=offe.ap()[:, ei:ei + 1], in0=offe.ap()[:, ei - 1:ei], in1=nsl.ap()[:, ei:ei + 1], op=ALU.add),
                          R=["nsl", "offe"], W=["offe"])
                sc.op("dve", lambda e: e.tensor_tensor(out=posb.ap(), in0=offe.ap(), in1=nsl.ap(), op=ALU.subtract), R=["offe", "nsl"], W=["posb"])
                sc.op("dve", lambda e: e.tensor_scalar(out=posb.ap(), in0=posb.ap(), scalar1=float(SL), scalar2=None, op0=ALU.mult), R=["posb"], W=["posb"])
                for hh in range(2):
                    sc.op("dve", lambda e, hh=hh: e.tensor_tensor(out=posfull.ap()[:, 16 * hh:16 * hh + 16, :], in0=bk(hh)[:, :].rearrange("p (a f) -> p a f", a=16),
                                                                  in1=posb.ap().unsqueeze(1).to_broadcast([128, 16, NE]), op=ALU.add),
                          R=[f"bank{hh}", "posb"], W=["posfull"])
                for ci, M_ in enumerate((M1, M2)):
                    mres = "M1" if ci == 0 else "M2"
                    sc.op("dve", lambda e, M_=M_: e.tensor_tensor(out=ptmp.ap(), in0=posfull.ap(), in1=M_.ap(), op=ALU.mult), R=["posfull", mres], W=["ptmp"])
                    sc.op("dve", lambda e, ci=ci: e.tensor_reduce(out=posf.ap()[:, ci, :], in_=ptmp.ap(), axis=AX.X, op=ALU.add), R=["ptmp"], W=["posf"])
                sc.op("dve", lambda e: e.tensor_scalar(out=posf.ap(), in0=posf.ap(), scalar1=float(NPOS - 1), scalar2=None, op0=ALU.min), R=["posf"], W=["posf"])
                sc.op("dve", lambda e: e.tensor_copy(out=posi.ap(), in_=posf.ap()), R=["posf"], W=["posi"])
                sc.op("dve", lambda e: e.tensor_tensor(out=cmp_.ap(), in0=offe.ap().unsqueeze(1).to_broadcast([128, NSLOT, NE]),
                                                       in1=sidx.ap().unsqueeze(2).to_broadcast([128, NSLOT, NE]), op=ALU.is_le), R=["offe", "sidx"], W=["cmp"])
                sc.op("dve", lambda e: e.tensor_reduce(out=eidf.ap()[:, 0, :], in_=cmp_.ap(), axis=AX.X, op=ALU.add), R=["cmp"], W=["eidf"])
                sc.op("dve", lambda e: e.tensor_scalar(out=eidf.ap()[:, 0, :], in0=eidf.ap()[:, 0, :], scalar1=float(NE - 1), scalar2=None, op0=ALU.min), R=["eidf"], W=["eidf"])
                sc.op("dve", lambda e: e.tensor_scalar(out=pidx.ap()[:, 1:2], in0=pidx.ap()[:, 0:1], scalar1=float(layer * NE * D), scalar2=None, op0=ALU.add), R=["pidx"], W=["pidx"])
                sc.op("dve", lambda e: e.tensor_scalar(out=pidx.ap()[:, 2:3], in0=pidx.ap()[:, 0:1], scalar1=float(layer * NE * HID), scalar2=None, op0=ALU.add), R=["pidx"], W=["pidx"])
                sc.op("dve", lambda e: e.tensor_scalar(out=eidf.ap()[:, 1, :], in0=eidf.ap()[:, 0, :], scalar1=128.0, scalar2=pidx.ap()[:, 0:1], op0=ALU.mult, op1=ALU.add),
                      R=["eidf", "pidx"], W=["eidf"])
                sc.op("dve", lambda e: e.tensor_scalar(out=eidf.ap()[:, 2, :], in0=eidf.ap()[:, 0, :], scalar1=128.0, scalar2=pidx.ap()[:, 0:1], op0=ALU.mult, op1=ALU.add),
                      R=["eidf", "pidx"], W=["eidf"])
                sc.op("dve", lambda e: e.tensor_copy(out=widx.ap(), in_=eidf.ap()[:, 1:3, :]), R=["eidf"], W=["widx"])
                for t in range(NT):
                    b = t % 4
                    b8 = t % 8
                    sc.dma("sp", lambda e, b=b, t=t: e.dma_start(out=xres[b].ap(), in_=src[t * 128:(t + 1) * 128, :]), W=[f"xres{b}"], pool="xr", n=4)
                    sc.op("act", lambda e, b=b, b8=b8: e.activation(out=xbf[b8].ap(), in_=xres[b].ap(), func=AF.Copy), R=[f"xres{b}"], W=[f"xbf{b8}"])
                    for ci in range(2):
                        sc.dma("pool", lambda e, b8=b8, t=t, ci=ci: e.indirect_dma_start(
                            out=Xs, out_offset=bass.IndirectOffsetOnAxis(ap=posi.ap()[:, ci, t:t + 1], axis=0), in_=xbf[b8].ap(), in_offset=None),
                            R=[f"xbf{b8}", "posi"], W=[("Xs", t, ci)], pool="sct", n=8)
                sc.barrier()

            with ExitStack() as es2:
                ent2 = es2.enter_context
                wA = [ent2(sbt(f"wA{i}", [128, 2, 8, HID], BF16)) for i in range(2)]
                wB = [ent2(sbt(f"wB{i}", [128, 4, D], BF16)) for i in range(2)]
                xs = [[ent2(sbt(f"xs{i}_{k}", [128, D], BF16)) for k in range(SL // 128)] for i in range(2)]
                xsT = [ent2(sbt(f"xsT{i}", [128, 8, SL], BF16)) for i in range(2)]
                sl = [ent2(sbt(f"sl{i}", [128, 4, SL], BF16)) for i in range(2)]
                gg = [ent2(sbt(f"gg{i}", [128, 4, SL], BF16)) for i in range(2)]
                ys = [ent2(sbt(f"ys{i}", [128, D], F32)) for i in range(2)]
                NS3 = SL // 128

                def load_slot(s_):
                    b = s_ % 2
                    for which in range(2):
                        sc.dma("pool", lambda e, which=which: e.indirect_dma_start(
                            out=wA[b].ap()[:, which, :, :].rearrange("p c h -> p (c h)"), out_offset=None, in_=Wc[which],
                            in_offset=bass.IndirectOffsetOnAxis(ap=widx.ap()[:, 0, s_:s_ + 1], axis=0)),
                            R=["widx"], W=[(f"wA{b}", which)], pool="wgt", n=12)
                    sc.dma("pool", lambda e: e.indirect_dma_start(
                        out=wB[b].ap().rearrange("p c d -> p (c d)"), out_offset=None, in_=Wc[2],
                        in_offset=bass.IndirectOffsetOnAxis(ap=widx.ap()[:, 0, s_:s_ + 1], axis=0)),
                        R=["widx"], W=[f"wB{b}"], pool="wgt", n=12)
                    for k in range(NS3):
                        r0 = s_ * SL + k * 128
                        sc.dma("sp", lambda e, k=k, r0=r0: e.dma_start(out=xs[b][k].ap(), in_=Xs[r0:r0 + 128, :]), R=["Xs"], W=[f"xs{b}_{k}"], pool="xs", n=6)

                load_slot(0)
                for s_ in range(NSLOT):
                    b = s_ % 2
                    if s_ + 1 < NSLOT:
                        load_slot(s_ + 1)
                    for k in range(NS3):
                        tb = 6 + (k % 2)
                        for c in range(8):
                            sc.op("pe", lambda e, c=c, k=k, tb=tb: e.transpose(out=bkh(tb)[:, c * 128:(c + 1) * 128], in_=xs[b][k].ap()[:, c * 128:(c + 1) * 128],
                                                                              identity=ident.ap()), R=[f"xs{b}_{k}", "ident"], W=[f"bank{tb}"])
                        sc.op("act" if k % 2 == 0 else "dve",
                              lambda e, k=k, tb=tb: (e.activation(out=xsT[b].ap()[:, :, k * 128:(k + 1) * 128], in_=bkh(tb)[:, :].rearrange("p (c f) -> p c f", c=8), func=AF.Copy)
                                                     if k % 2 == 0 else
                                                     e.tensor_copy(out=xsT[b].ap()[:, :, k * 128:(k + 1) * 128], in_=bkh(tb)[:, :].rearrange("p (c f) -> p c f", c=8))),
                              R=[f"bank{tb}"], W=[f"xsT{b}"])
                    for m in range(4):
                        for which in range(2):
                            pb = 2 * which + (m % 2)
                            for c in range(8):
                                sc.op("pe", lambda e, c=c, m=m, which=which, pb=pb: e.matmul(
                                    bk(pb)[:, 0:SL], lhsT=wA[b].ap()[:, which, c, m * 128:(m + 1) * 128], rhs=xsT[b].ap()[:, c, :],
                                    start=(c == 0), stop=(c == 7)), R=[(f"wA{b}", which), f"xsT{b}"], W=[f"bank{pb}"])
                        sc.op("act", lambda e, m=m: e.activation(out=sl[b].ap()[:, m, :], in_=bk(m % 2)[:, 0:SL], func=AF.Silu), R=[f"bank{m % 2}"], W=[f"sl{b}"])
                        sc.op("dve", lambda e, m=m: e.tensor_tensor(out=gg[b].ap()[:, m, :], in0=sl[b].ap()[:, m, :], in1=bk(2 + m % 2)[:, 0:SL], op=ALU.mult),
                              R=[f"sl{b}", f"bank{2 + m % 2}"], W=[f"gg{b}"])
                    for k in range(NS3):
                        yb_ = (s_ * NS3 + k) % 2
                        for hh in range(2):
                            pb = 4 + hh
                            for m in range(4):
                                sc.op("pe", lambda e, m=m, k=k, hh=hh, pb=pb: e.matmul(
                                    bk(pb)[:, :], lhsT=gg[b].ap()[:, m, k * 128:(k + 1) * 128], rhs=wB[b].ap()[:, m, hh * 512:(hh + 1) * 512],
                                    start=(m == 0), stop=(m == 3)), R=[f"gg{b}", f"wB{b}"], W=[f"bank{pb}"])
                            if hh == 0:
                                sc.op("act", lambda e, yb_=yb_, pb=pb: e.activation(out=ys[yb_].ap()[:, 0:512], in_=bk(pb)[:, :], func=AF.Copy), R=[f"bank{pb}"], W=[f"ys{yb_}"])
                            else:
                                sc.op("dve", lambda e, yb_=yb_, pb=pb: e.tensor_copy(out=ys[yb_].ap()[:, 512:1024], in_=bk(pb)[:, :]), R=[f"bank{pb}"], W=[f"ys{yb_}"])
                        r0 = s_ * SL + k * 128
                        sc.dma("sp", lambda e, yb_=yb_, r0=r0: e.dma_start(out=Ys[r0:r0 + 128, :], in_=ys[yb_].ap()), R=[f"ys{yb_}"], W=[("Ys", s_, k)], pool="ys", n=4)
                sc.barrier()

            with ExitStack() as es3:
                ent3 = es3.enter_context
                gb = ent3(sbt("gb", [128, 2, D], F32))
                NB3 = 4
                stat = [ent3(sbt(f"stat{i}", [128, 20], F32)) for i in range(NB3)]
                g1 = [ent3(sbt(f"g1_{i}", [128, D], F32)) for i in range(NB3)]
                g2 = [ent3(sbt(f"g2_{i}", [128, D], F32)) for i in range(NB3)]
                xres = [ent3(sbt(f"xres{i}", [128, D], F32)) for i in range(NB3)]
                yb = [ent3(sbt(f"yb{i}", [128, D], F32)) for i in range(NB3)]
                xbf = [ent3(sbt(f"xbf{i}", [128, D], BF16)) for i in range(NB3)]
                load_gb(gb, ln_ffn_g, ln_ffn_b, layer)
                def m3_fetch(t):
                    b = t % NB3
                    sc.dma("pool", lambda e: e.indirect_dma_start(out=g1[b].ap(), out_offset=None, in_=Ys,
                                                                  in_offset=bass.IndirectOffsetOnAxis(ap=posi.ap()[:, 0, t:t + 1], axis=0)),
                           R=["Ys", "posi"], W=[f"g1_{b}"], pool="gth", n=8)
                    sc.dma("pool", lambda e: e.indirect_dma_start(out=g2[b].ap(), out_offset=None, in_=Ys,
                                                                  in_offset=bass.IndirectOffsetOnAxis(ap=posi.ap()[:, 1, t:t + 1], axis=0)),
                           R=["Ys", "posi"], W=[f"g2_{b}"], pool="gth", n=8)
                    sc.dma("sp", lambda e: e.dma_start(out=xres[b].ap(), in_=src[t * 128:(t + 1) * 128, :]), W=[f"xres{b}"], pool="xr", n=4)

                def m3_produce_a(t):
                    b = t % NB3
                    sc.op("act", lambda e: e.activation(out=g1[b].ap(), in_=g1[b].ap(), func=AF.Copy, scale=WT.ap()[:, t, 0:1]), R=[f"g1_{b}", "WT"], W=[f"g1_{b}"])

                def m3_produce(t):
                    b = t % NB3
                    sc.op("dve", lambda e: e.scalar_tensor_tensor(out=g2[b].ap(), in0=g2[b].ap(), scalar=WT.ap()[:, t, 1:2], in1=g1[b].ap(), op0=ALU.mult, op1=ALU.add),
                          R=[f"g2_{b}", f"g1_{b}", "WT"], W=[f"g2_{b}"])
                    sc.op("dve", lambda e: e.scalar_tensor_tensor(out=yb[b].ap(), in0=xres[b].ap(), scalar=float(ALPHA), in1=g2[b].ap(), op0=ALU.mult, op1=ALU.add),
                          R=[f"xres{b}", f"g2_{b}"], W=[f"yb{b}"])

                ln_pipeline(m3_fetch, m3_produce_a, m3_produce, yb, (lambda b: f"yb{b}"), stat, gb, dst, None if last else xbf)
                sc.barrier()

    stages = []
    for layer in range(DEPTH):
        stages.append(("att", layer))
        stages.append(("moe", layer))
    for i, (kind, layer) in enumerate(stages):
        if stop_after is not None and i > stop_after:
            break
        if kind == "att":
            attention_layer(layer)
        elif SPARSE:
            moe_layer_sparse(layer)
        else:
            moe_layer(layer)

    sc.final_wait("sp")
    return nc


_CACHE = {}


def _prep_common(inputs):
    f = lambda a: np.ascontiguousarray(np.asarray(a), dtype=np.float32)
    m = {
        "rel_table": f(inputs["rel_table"]),
        "da_wq": f(inputs["da_wq"][0]), "da_wk": f(inputs["da_wk"][0]), "da_wv": f(inputs["da_wv"][0]), "da_wo": f(inputs["da_wo"][0]),
        "sb_wq": f(inputs["sb_wq"][0]), "sb_wk": f(inputs["sb_wk"][0]), "sb_wv": f(inputs["sb_wv"][0]), "sb_wo": f(inputs["sb_wo"][0]),
        "da_l": f(np.stack([inputs["da_lq1"][0], inputs["da_lk1"][0], inputs["da_lq2"][0], inputs["da_lk2"][0]], axis=0)),
        "da_subln_g": f(inputs["da_subln_g"]),
        "ln_mix_g": f(inputs["ln_mix_g"]), "ln_mix_b": f(inputs["ln_mix_b"]),
        "ln_ffn_g": f(inputs["ln_ffn_g"]), "ln_ffn_b": f(inputs["ln_ffn_b"]),
        "moe_w_group": f(inputs["moe_w_group"]), "moe_b_group": f(inputs["moe_b_group"]),
        "moe_w_expert": f(inputs["moe_w_expert"]), "moe_b_expert": f(inputs["moe_b_expert"]),
        "moe_w1": f(inputs["moe_w1"]), "moe_w3": f(inputs["moe_w3"]), "moe_w2": f(inputs["moe_w2"]),
    }
    m.update(_consts_host())
    return m


def kernel(**inputs):
    if "nc" not in _CACHE:
        _CACHE["nc"] = build_program()
    nc = _CACHE["nc"]
    common = _prep_common(inputs)
    x = np.asarray(inputs["x"], dtype=np.float32)
    in_maps = []
    for b in range(8):
        m = dict(common)
        m["x"] = np.ascontiguousarray(x[b])
        in_maps.append(m)
    res = run_bass_kernel_spmd(nc, in_maps, core_ids=list(range(8)))
    return np.stack([np.asarray(r["out"], dtype=np.float32) for r in res.results], axis=0)
```

```python
import math
from contextlib import ExitStack
import numpy as np
import ml_dtypes
import concourse.bass as bass
import concourse.mybir as mybir
from concourse.bass_utils import run_bass_kernel_spmd

F32 = mybir.dt.float32
BF16 = mybir.dt.bfloat16
AF = mybir.ActivationFunctionType
ALU = mybir.AluOpType
AX = mybir.AxisListType

S = 4096
D = 1024
NT = S // 128
DEPTH = 2
NE = 32
HID = 512
ALPHA = (2 * DEPTH) ** 0.25
LN_EPS = 1e-5
RMS_EPS = 1e-6
GW = 383
SL = 256
NSLOT = 63
NPOS = NSLOT * SL
KMAX = (S + SL - 1) // SL
I32 = mybir.dt.int32
SPARSE = True


class Sched:
    LIMIT = 30000
    NDMA = 24

    def __init__(self, nc):
        self.nc = nc
        self.eng = {"pe": nc.tensor, "act": nc.scalar, "dve": nc.vector, "pool": nc.gpsimd, "sp": nc.sync}
        self.esem = {}
        self.ecnt = {}
        self.nsem = 0
        for e in self.eng:
            self._newsem(e)
        self.seen = {e: {} for e in self.eng}
        self.lastw = {}
        self.readers = {}
        self.dpools = {}

    def _newsem(self, e):
        self.esem[e] = self.nc.alloc_semaphore(f"es_{e}_{self.nsem}")
        self.nsem += 1
        self.ecnt[e] = 0

    def _wait(self, e, dep):
        sem, val, weng = dep
        if weng == e and e == "pe":
            return
        k = sem.num
        if self.seen[e].get(k, 0) >= val:
            return
        self.eng[e].wait_ge(sem, val)
        self.seen[e][k] = val

    def _deps(self, e, R, W):
        for r in R:
            w = self.lastw.get(r)
            if w:
                for tok in w.values():
                    self._wait(e, tok)
        for w_ in W:
            lw = self.lastw.get(w_)
            if lw:
                for tok in lw.values():
                    self._wait(e, tok)
            rd = self.readers.get(w_)
            if rd:
                for tok in rd.values():
                    self._wait(e, tok)

    def _book(self, tok, R, W):
        for r in R:
            self.readers.setdefault(r, {})[tok[0].num] = tok
        for w_ in W:
            self.lastw.setdefault(w_, {})[tok[0].num] = tok
            self.readers[w_] = {}

    def op(self, e, fn, R=(), W=()):
        self._deps(e, R, W)
        if self.ecnt[e] >= self.LIMIT:
            self._newsem(e)
        inst = fn(self.eng[e])
        self.ecnt[e] += 1
        inst.then_inc(self.esem[e], 1)
        self._book((self.esem[e], self.ecnt[e], e), R, W)

    def dma(self, q, fn, R=(), W=(), pool="misc", n=6):
        pool = f"{pool}_{q}"
        if pool not in self.dpools:
            self.dpools[pool] = dict(sems=[self.nc.alloc_semaphore(f"dq_{pool}_{i}") for i in range(n)], val=[0] * n, nxt=0)
        P = self.dpools[pool]
        i = P["nxt"]
        P["nxt"] = (i + 1) % len(P["sems"])
        sem = P["sems"][i]
        if P["val"][i] > 0:
            self._wait(q, (sem, P["val"][i], "dma"))
        self._deps(q, R, W)
        inst = fn(self.eng[q])
        P["val"][i] += 16
        inst.then_inc(sem, 16)
        self._book((sem, P["val"][i], "dma"), R, W)

    def _dma_toks(self):
        toks = []
        for P in self.dpools.values():
            for sem, v in zip(P["sems"], P["val"]):
                if v > 0:
                    toks.append((sem, v, "dma"))
        return toks

    def barrier(self):
        toks = [(self.esem[e], self.ecnt[e], "x") for e in self.eng if self.ecnt[e] > 0]
        toks += self._dma_toks()
        for e in self.eng:
            for t in toks:
                self._wait(e, t)
        self.lastw = {}
        self.readers = {}

    def final_wait(self, e="sp"):
        for t in self._dma_toks():
            self._wait(e, t)
        for o in self.eng:
            if o != e and self.ecnt[o] > 0:
                self._wait(e, (self.esem[o], self.ecnt[o], o))


def _consts_host():
    ident = np.eye(128, dtype=np.float32)
    kk = np.arange(128)[:, None]
    qq = np.arange(128)[None, :]
    tri_incl = (kk >= np.arange(128)[None, :]).astype(np.float32)
    tri_low = (kk < np.arange(128)[None, :]).astype(np.float32)
    mask_le = (kk <= qq).astype(np.float32)
    mask_lt = (kk < qq).astype(np.float32)
    n = np.maximum(np.arange(GW) - 127, 0)
    nf = np.maximum(n, 1).astype(np.float32)
    large = 16 + (np.log(nf / np.float32(16)) / np.float32(math.log(128 / 16)) * np.float32(16)).astype(np.int32)
    large = np.minimum(large, 31)
    bucket = np.where(n < 16, n, large)
    oh = (bucket[None, :] == np.arange(32)[:, None]).astype(np.float32)
    bf = ml_dtypes.bfloat16
    return {
        "c_ident": ident.astype(bf), "c_tri_incl": tri_incl.astype(bf), "c_tri_low": tri_low.astype(bf),
        "c_mask_le": mask_le, "c_mask_lt": mask_lt.astype(bf), "c_oh": oh,
        "c_pidx": np.arange(128, dtype=np.float32).reshape(128, 1),
        "c_sidx": np.tile(np.arange(NSLOT, dtype=np.float32)[None, :], (128, 1)),
    }


def build_program(stop_after=None, debug=False):
    nc = bass.Bass("TRN2", target_bir_lowering=False)
    sc = Sched(nc)

    def din(name, shape, dt=F32):
        return nc.dram_tensor(name, list(shape), dt, kind="ExternalInput").ap()

    x_in = din("x", [S, D])
    rel_table = din("rel_table", [32, 8])
    attw = [
        dict(wq=din("da_wq", [D, D]), wk=din("da_wk", [D, D]), wv=din("da_wv", [D, D]), wo=din("da_wo", [D, D])),
        dict(wq=din("sb_wq", [D, D]), wk=din("sb_wk", [D, D]), wv=din("sb_wv", [D, D]), wo=din("sb_wo", [D, D])),
    ]
    da_l = din("da_l", [4, 64])
    da_g = din("da_subln_g", [1, 128])
    ln_mix_g = din("ln_mix_g", [DEPTH, D]); ln_mix_b = din("ln_mix_b", [DEPTH, D])
    ln_ffn_g = din("ln_ffn_g", [DEPTH, D]); ln_ffn_b = din("ln_ffn_b", [DEPTH, D])
    w_group = din("moe_w_group", [DEPTH, D, 4]); b_group = din("moe_b_group", [DEPTH, 4])
    w_expert = din("moe_w_expert", [DEPTH, D, NE]); b_expert = din("moe_b_expert", [DEPTH, NE])
    w1 = din("moe_w1", [DEPTH, NE, D, HID]); w3 = din("moe_w3", [DEPTH, NE, D, HID]); w2 = din("moe_w2", [DEPTH, NE, HID, D])
    c_ident = din("c_ident", [128, 128], BF16); c_tri_incl = din("c_tri_incl", [128, 128], BF16)
    c_tri_low = din("c_tri_low", [128, 128], BF16); c_mask_le = din("c_mask_le", [128, 128])
    c_mask_lt = din("c_mask_lt", [128, 128], BF16); c_oh = din("c_oh", [32, GW])
    c_pidx = din("c_pidx", [128, 1]); c_sidx = din("c_sidx", [128, NSLOT])
    Xs = nc.dram_tensor("Xs", [NPOS, D], BF16, kind="Internal").ap()
    Ys = nc.dram_tensor("Ys", [NPOS, D], F32, kind="Internal").ap()
    Wc = [nc.dram_tensor(f"Wc{i}", [NE * 128, 4096], BF16, kind="Internal").ap() for i in range(3)]

    out = nc.dram_tensor("out", [S, D], F32, kind="ExternalOutput").ap()
    skind = "ExternalOutput" if debug else "Internal"
    xr = [x_in] + [nc.dram_tensor(f"xr{i}", [S, D], F32, kind=skind).ap() for i in (1, 2, 3)] + [out]
    gd = nc.dram_tensor("gd", [8, 128, GW], F32, kind="Internal")

    PP = [nc.alloc_psum_tensor(f"pp{i}", [128, 1024], F32) for i in range(4)]

    _uid = [0]

    def sbt(name, shape, dt):
        _uid[0] += 1
        return nc.sbuf_tensor(f"{name}_u{_uid[0]}", shape, dt)

    def bk(i):
        return PP[i // 2].ap()[:, (i % 2) * 512:(i % 2) * 512 + 512]

    def bkh(i):
        return PP[i // 2].bitcast(BF16).ap()[:, (i % 2) * 1024:(i % 2) * 1024 + 1024]

    xT = nc.alloc_sbuf_tensor("xT", [128, 8, S], BF16)
    ident = nc.alloc_sbuf_tensor("ident", [128, 128], BF16)
    tri_incl = nc.alloc_sbuf_tensor("tri_incl", [128, 128], BF16)
    tri_low = nc.alloc_sbuf_tensor("tri_low", [128, 128], BF16)
    mask_le = nc.alloc_sbuf_tensor("mask_le", [128, 128], F32)
    mask_lt = nc.alloc_sbuf_tensor("mask_lt", [128, 128], BF16)
    cst = nc.alloc_sbuf_tensor("cst", [128, 8], F32)
    sc.dma("sp", lambda e: e.dma_start(out=ident.ap(), in_=c_ident), W=["ident"])
    sc.dma("sp", lambda e: e.dma_start(out=tri_incl.ap(), in_=c_tri_incl), W=["tri_incl"])
    sc.dma("sp", lambda e: e.dma_start(out=tri_low.ap(), in_=c_tri_low), W=["tri_low"])
    sc.dma("sp", lambda e: e.dma_start(out=mask_le.ap(), in_=c_mask_le), W=["mask_le"])
    sc.dma("sp", lambda e: e.dma_start(out=mask_lt.ap(), in_=c_mask_lt), W=["mask_lt"])
    sc.op("dve", lambda e: e.memset(cst.ap()[:, 0:1], LN_EPS), W=["cst"])
    sc.op("dve", lambda e: e.memset(cst.ap()[:, 1:2], RMS_EPS), W=["cst"])
    sc.op("dve", lambda e: e.memset(cst.ap()[:, 2:3], 1.0), W=["cst"])

    def make_xT_tile(src_bf, src_res, t, bank):
        for c in range(8):
            sc.op("pe", lambda e, c=c: e.transpose(out=bkh(bank)[:, c * 128:(c + 1) * 128], in_=src_bf[:, c * 128:(c + 1) * 128],
                                                    identity=ident.ap()),
                  R=[src_res, "ident"], W=[f"bank{bank}"])
        sc.op("act", lambda e: e.activation(out=xT.ap()[:, :, t * 128:(t + 1) * 128],
                                            in_=bkh(bank)[:, :].rearrange("p (c f) -> p c f", c=8), func=AF.Copy),
              R=[f"bank{bank}"], W=[("xT", t)])

    def ln_tile(y, yres, gb, stat, t, dst, xbf, lnbank):
        sres = ("stat", stat.name)
        st6 = stat.ap()[:, 0:12].rearrange("p (a b) -> p a b", a=2)
        for hh in range(2):
            sc.op("dve", lambda e, hh=hh: e.bn_stats(out=st6[:, hh, :], in_=y[:, hh * 512:(hh + 1) * 512]), R=[yres], W=[sres])
        sc.op("dve", lambda e: e.bn_aggr(out=stat.ap()[:, 12:14], in_=stat.ap()[:, 0:12]), R=[sres], W=[sres])
        sc.op("act", lambda e: e.activation(out=stat.ap()[:, 14:15], in_=stat.ap()[:, 13:14], func=AF.Ln, bias=float(LN_EPS), scale=1.0),
              R=[sres], W=[sres])
        sc.op("act", lambda e: e.activation(out=stat.ap()[:, 15:16], in_=stat.ap()[:, 14:15], func=AF.Exp, scale=-0.5), R=[sres], W=[sres])
        sc.op("dve", lambda e: e.tensor_scalar(out=stat.ap()[:, 16:17], in0=stat.ap()[:, 12:13], scalar1=stat.ap()[:, 15:16], scalar2=-1.0,
                                               op0=ALU.mult, op1=ALU.mult), R=[sres], W=[sres])
        sc.op("act", lambda e: e.activation(out=y, in_=y, func=AF.Identity, bias=stat.ap()[:, 16:17], scale=stat.ap()[:, 15:16]), R=[yres, sres], W=[yres])
        sc.op("dve", lambda e: e.tensor_tensor(out=y, in0=y, in1=gb.ap()[:, 0, :], op=ALU.mult), R=[yres, "gb0"], W=[yres])
        sc.op("dve", lambda e: e.tensor_tensor(out=y, in0=y, in1=gb.ap()[:, 1, :], op=ALU.add), R=[yres, "gb1"], W=[yres])
        sc.dma("sp", lambda e: e.dma_start(out=dst[t * 128:(t + 1) * 128, :], in_=y), R=[yres], W=[], pool="st", n=4)
        if xbf is not None:
            xb, xbres = xbf
            sc.op("act", lambda e: e.activation(out=xb, in_=y, func=AF.Copy), R=[yres], W=[xbres])
            make_xT_tile(xb, xbres, t, lnbank)

    def ln_s1(y, yres, stat):
        sres = ("stat", stat.name)
        st6 = stat.ap()[:, 0:12].rearrange("p (a b) -> p a b", a=2)
        for hh in range(2):
            sc.op("dve", lambda e, hh=hh: e.bn_stats(out=st6[:, hh, :], in_=y[:, hh * 512:(hh + 1) * 512]), R=[yres], W=[sres])
        sc.op("dve", lambda e: e.bn_aggr(out=stat.ap()[:, 12:14], in_=stat.ap()[:, 0:12]), R=[sres], W=[sres])

    def ln_s2(y, yres, stat):
        sres = ("stat", stat.name)
        sc.op("act", lambda e: e.activation(out=stat.ap()[:, 14:15], in_=stat.ap()[:, 13:14], func=AF.Ln, bias=float(LN_EPS), scale=1.0),
              R=[sres], W=[sres])
        sc.op("act", lambda e: e.activation(out=stat.ap()[:, 15:16], in_=stat.ap()[:, 14:15], func=AF.Exp, scale=-0.5), R=[sres], W=[sres])
        sc.op("dve", lambda e: e.tensor_scalar(out=stat.ap()[:, 16:17], in0=stat.ap()[:, 12:13], scalar1=stat.ap()[:, 15:16], scalar2=-1.0,
                                               op0=ALU.mult, op1=ALU.mult), R=[sres], W=[sres])
        sc.op("act", lambda e: e.activation(out=y, in_=y, func=AF.Identity, bias=stat.ap()[:, 16:17], scale=stat.ap()[:, 15:16]), R=[yres, sres], W=[yres])

    def ln_s3(y, yres, gb, t, dst, xbf):
        sc.op("dve", lambda e: e.tensor_tensor(out=y, in0=y, in1=gb.ap()[:, 0, :], op=ALU.mult), R=[yres, "gb0"], W=[yres])
        sc.op("dve", lambda e: e.tensor_tensor(out=y, in0=y, in1=gb.ap()[:, 1, :], op=ALU.add), R=[yres, "gb1"], W=[yres])
        sc.dma("sp", lambda e: e.dma_start(out=dst[t * 128:(t + 1) * 128, :], in_=y), R=[yres], W=[], pool="st", n=4)

    def ln_s3b(y, yres, xbf):
        xb, xbres = xbf
        sc.op("act", lambda e: e.activation(out=xb, in_=y, func=AF.Copy), R=[yres], W=[xbres])

    def ln_pipeline(pre, produce_a, produce, yb, ybres, stat, gb, dst, xbf):
        NB = len(yb)
        for i in range(-5, NT + 3):
            if pre is not None and 0 <= i + 5 < NT:
                pre(i + 5)
            if produce_a is not None and 0 <= i + 3 < NT:
                produce_a(i + 3)
            if 0 <= i + 1 < NT:
                t = i + 1
                ln_s2(yb[t % NB].ap(), ybres(t % NB), stat[t % NB])
            if xbf is not None and 0 <= i - 1 < NT:
                t = i - 1
                ln_s3b(yb[t % NB].ap(), ybres(t % NB), (xbf[t % NB].ap(), f"xbf{t % NB}"))
            if xbf is not None and 0 <= i - 2 < NT:
                t = i - 2
                make_xT_tile(xbf[t % NB].ap(), f"xbf{t % NB}", t, 6 + (t % 2))
            if 0 <= i + 2 < NT:
                t = i + 2
                produce(t)
                ln_s1(yb[t % NB].ap(), ybres(t % NB), stat[t % NB])
            if 0 <= i < NT:
                t = i
                ln_s3(yb[t % NB].ap(), ybres(t % NB), gb, t, dst, None)

    def load_gb(gb, g_ap, b_ap, layer):
        sc.dma("sp", lambda e: e.dma_start(out=gb.ap()[:, 0, :], in_=g_ap[layer:layer + 1, :].partition_broadcast(128)), W=["gb0"])
        sc.dma("sp", lambda e: e.dma_start(out=gb.ap()[:, 1, :], in_=b_ap[layer:layer + 1, :].partition_broadcast(128)), W=["gb1"])

    with ExitStack() as es:
        xld = [es.enter_context(sbt(f"xld{i}", [128, D], BF16)) for i in range(2)]
        for t in range(NT):
            b = t % 2
            sc.dma("pool", lambda e, b=b, t=t: e.dma_start(out=xld[b].ap(), in_=x_in[t * 128:(t + 1) * 128, :]), W=[f"xld{b}"])
            make_xT_tile(xld[b].ap(), f"xld{b}", t, 6 + b)
        sc.barrier()

    def attention_layer(layer):
        kind = "diff" if layer % 2 == 0 else "sb"
        W_ = attw[layer % 2]
        VW = 130 if kind == "diff" else 128
        with ExitStack() as es:
            oT = es.enter_context(sbt("oT", [128, 8, S], BF16))
            with ExitStack() as es2:
                ent = es2.enter_context
                qT = ent(sbt("qT", [128, 2, S], BF16))
                kT = ent(sbt("kT", [128, S], BF16))
                V = ent(sbt("V", [128, NT, VW], BF16))
                wqkv = [ent(sbt(f"wqkv{i}", [128, 3, 8, 128], BF16)) for i in range(2)]
                NB = 4
                if kind == "diff":
                    Pt = [ent(sbt(f"Pt{i}", [128, 512], BF16)) for i in range(NB)]
                    Ep = ent(sbt("Ep", [128, 8, 2, 128], F32))
                    gtmp = ent(sbt("gtmp", [128, GW], F32))
                    rb = ent(sbt("rb", [32, 128], F32))
                    rtab = ent(sbt("rtab", [32, 8], F32))
                    oh = ent(sbt("oh", [32, GW], F32))
                    ones32 = ent(sbt("ones32", [32, 128], F32))
                    cfar = ent(sbt("cfar", [128, 16], F32))
                    lam = ent(sbt("lam", [128, 8], F32))
                    lvec = ent(sbt("lvec", [128, 4, 64], F32))
                    gsub = ent(sbt("gsub", [128, 128], F32))
                    R1 = ent(sbt("R1", [128, 4, 128], F32))
                    ot = [ent(sbt(f"ot{i}", [128, 128], F32)) for i in range(4)]
                    sm = [ent(sbt(f"sm{i}", [128, 8], F32)) for i in range(4)]
                    junk = ent(sbt("junk", [128, 128], F32))
                else:
                    eb = [ent(sbt(f"eb{i}", [128, 512], BF16)) for i in range(1)]
                    spb = [ent(sbt(f"spb{i}", [128, 512], BF16)) for i in range(1)]
                    gb_ = [ent(sbt(f"gb_{i}", [128, 512], BF16)) for i in range(1)]
                    wb = [ent(sbt(f"wb{i}", [128, 512], BF16)) for i in range(1)]
                    eb2 = [ent(sbt(f"eb2_{i}", [128, 2, 512], BF16)) for i in range(3)]
                    sp2 = [ent(sbt(f"sp2_{i}", [128, 2, 512], BF16)) for i in range(3)]
                    g2b = [ent(sbt(f"g2b_{i}", [128, 2, 512], BF16)) for i in range(2)]
                    w2b = [ent(sbt(f"w2b_{i}", [128, 2, 512], BF16)) for i in range(2)]
                onb = [ent(sbt(f"onb{i}", [128, 128], BF16)) for i in range(4)]

                sc.op("pool", lambda e: e.memset(qT.ap()[:, 0, :], 0.0), W=["qT"])
                sc.op("dve", lambda e: e.memset(qT.ap()[:, 1, :], 0.0), W=["qT"])
                lam_init = 0.8 - 0.6 * math.exp(-0.3 * layer)
                if kind == "diff":
                    sc.dma("sp", lambda e: e.dma_start(out=lvec.ap(), in_=da_l.partition_broadcast(128)), W=["lvec"])
                    sc.dma("sp", lambda e: e.dma_start(out=gsub.ap(), in_=da_g[0:1, :].partition_broadcast(128)), W=["gsub"])
                    sc.op("dve", lambda e: e.tensor_scalar(out=gsub.ap(), in0=gsub.ap(), scalar1=float(1.0 - lam_init), scalar2=None, op0=ALU.mult),
                          R=["gsub"], W=["gsub"])
                    sc.op("dve", lambda e: e.tensor_tensor(out=lvec.ap()[:, 0, :], in0=lvec.ap()[:, 0, :], in1=lvec.ap()[:, 1, :], op=ALU.mult), R=["lvec"], W=["lvec"])
                    sc.op("dve", lambda e: e.tensor_tensor(out=lvec.ap()[:, 2, :], in0=lvec.ap()[:, 2, :], in1=lvec.ap()[:, 3, :], op=ALU.mult), R=["lvec"], W=["lvec"])
                    sc.op("dve", lambda e: e.tensor_reduce(out=lam.ap()[:, 0:1], in_=lvec.ap()[:, 0, :], axis=AX.X, op=ALU.add), R=["lvec"], W=["lam"])
                    sc.op("dve", lambda e: e.tensor_reduce(out=lam.ap()[:, 1:2], in_=lvec.ap()[:, 2, :], axis=AX.X, op=ALU.add), R=["lvec"], W=["lam"])
                    sc.op("act", lambda e: e.activation(out=lam.ap()[:, 2:4], in_=lam.ap()[:, 0:2], func=AF.Exp), R=["lam"], W=["lam"])
                    sc.op("dve", lambda e: e.scalar_tensor_tensor(out=lam.ap()[:, 4:5], in0=lam.ap()[:, 3:4], scalar=float(-lam_init), in1=lam.ap()[:, 2:3],
                                                                  op0=ALU.add, op1=ALU.subtract), R=["lam"], W=["lam"])
                    sc.dma("sp", lambda e: e.dma_start(out=rtab.ap(), in_=rel_table), W=["rtab"])
                    sc.dma("sp", lambda e: e.dma_start(out=oh.ap(), in_=c_oh), W=["oh"])
                    sc.dma("sp", lambda e: e.dma_start(out=cfar.ap()[:, 0:8], in_=rel_table[31:32, :].partition_broadcast(128)), W=["cfar"])
                    sc.op("dve", lambda e: e.tensor_scalar(out=cfar.ap()[:, 8:16], in0=cfar.ap()[:, 0:8], scalar1=-1.0, scalar2=None, op0=ALU.mult), R=["cfar"], W=["cfar"])
                    sc.op("dve", lambda e: e.memset(ones32.ap(), 1.0), W=["ones32"])
                    for h in range(8):
                        sc.op("dve", lambda e, h=h: e.tensor_scalar(out=rb.ap(), in0=ones32.ap(), scalar1=rtab.ap()[:, h:h + 1], scalar2=None, op0=ALU.mult),
                              R=["ones32", "rtab"], W=["rb"])
                        sc.op("pe", lambda e: e.matmul(bk(6)[:, 0:GW], lhsT=rb.ap(), rhs=oh.ap(), start=True, stop=True), R=["rb", "oh"], W=["bank6"])
                        sc.op("act", lambda e: e.activation(out=gtmp.ap(), in_=bk(6)[:, 0:GW], func=AF.Copy), R=["bank6"], W=["gtmp"])
                        sc.dma("sp", lambda e, h=h: e.dma_start(out=gd.ap()[h], in_=gtmp.ap()), R=["gtmp"], W=["gd"])
                        for Dd in range(2):
                            src = bass.AP(tensor=gd.ap().tensor, offset=h * 128 * GW + Dd * 128 + 127, ap=[[GW - 1, 128], [1, 128]])
                            sc.dma("sp", lambda e, h=h, Dd=Dd, src=src: e.dma_start(out=Ep.ap()[:, h, Dd, :], in_=src), R=["gd"], W=["Ep"])
                            sc.op("act", lambda e, h=h, Dd=Dd: e.activation(out=Ep.ap()[:, h, Dd, :], in_=Ep.ap()[:, h, Dd, :], func=AF.Exp,
                                                                             bias=cfar.ap()[:, 8 + h:9 + h], scale=1.0), R=["Ep", "cfar"], W=["Ep"])
                        sc.op("dve", lambda e, h=h: e.tensor_tensor(out=Ep.ap()[:, h, 0, :], in0=Ep.ap()[:, h, 0, :], in1=mask_le.ap(), op=ALU.mult),
                              R=["Ep", "mask_le"], W=["Ep"])
                    sc.op("dve", lambda e: e.memset(V.ap()[:, :, 128:130], 1.0), W=["Vones"])

                def load_w(j):
                    b = j % 2
                    for i, nm in enumerate(("wq", "wk", "wv")):
                        src = W_[nm].rearrange("(c p) f -> p c f", p=128)[:, :, j * 128:(j + 1) * 128]
                        sc.dma("pool", lambda e, i=i, src=src, b=b: e.dma_start(out=wqkv[b].ap()[:, i, :, :], in_=src), W=[(f"wqkv{b}", i)], pool="wqkv", n=6)

                def project(j):
                    b = j % 2
                    wres = f"wqkv{b}"
                    for which, dstT, scale in ((0, qT, 0.125), (1, kT, None)):
                        for tc in range(8):
                            pb = 6 + (tc % 2)
                            for c in range(8):
                                sc.op("pe", lambda e, c=c, tc=tc, pb=pb, which=which: e.matmul(
                                    bk(pb)[:, :], lhsT=wqkv[b].ap()[:, which, c, :], rhs=xT.ap()[:, c, tc * 512:(tc + 1) * 512],
                                    start=(c == 0), stop=(c == 7)),
                                    R=[(wres, which)] + [("xT", t) for t in range(4 * tc, 4 * tc + 4)], W=[f"bank{pb}"])
                            if scale is not None:
                                sc.op("act", lambda e, tc=tc, pb=pb: e.activation(out=qT.ap()[0:64, 0, tc * 512:(tc + 1) * 512], in_=bk(pb)[0:64, :], func=AF.Copy, scale=scale),
                                      R=[f"bank{pb}"], W=["qT"])
                                sc.op("dve", lambda e, tc=tc, pb=pb: e.tensor_scalar(out=qT.ap()[64:128, 1, tc * 512:(tc + 1) * 512], in0=bk(pb)[64:128, :], scalar1=float(scale), scalar2=None,
                                                                                  op0=ALU.mult), R=[f"bank{pb}"], W=["qT"])
                            else:
                                sc.op("dve", lambda e, tc=tc, pb=pb: e.tensor_copy(out=dstT.ap()[:, tc * 512:(tc + 1) * 512], in_=bk(pb)[:, :]),
                                      R=[f"bank{pb}"], W=["kT"])
                    for tg in range(8):
                        pb = 6 + (tg % 2)
                        for tt in range(4):
                            t = 4 * tg + tt
                            for c in range(8):
                                sc.op("pe", lambda e, c=c, t=t, tt=tt, pb=pb: e.matmul(
                                    bk(pb)[:, tt * 128:(tt + 1) * 128], lhsT=xT.ap()[:, c, t * 128:(t + 1) * 128], rhs=wqkv[b].ap()[:, 2, c, :],
                                    start=(c == 0), stop=(c == 7), skip_group_check=True),
                                    R=[(wres, 2), ("xT", t)], W=[f"bank{pb}"])
                        sc.op("act" if tg % 2 == 0 else "dve",
                              lambda e, tg=tg, pb=pb: (e.activation(out=V.ap()[:, 4 * tg:4 * tg + 4, 0:128], in_=bk(pb)[:, :].rearrange("p (a f) -> p a f", a=4), func=AF.Copy)
                                                       if tg % 2 == 0 else
                                                       e.tensor_copy(out=V.ap()[:, 4 * tg:4 * tg + 4, 0:128], in_=bk(pb)[:, :].rearrange("p (a f) -> p a f", a=4))),
                              R=[f"bank{pb}"], W=["V"])

                def finish_tile(j, t, src_bf, src_res):
                    sc.op("pe", lambda e: e.transpose(out=bkh(7)[:, 0:128], in_=src_bf, identity=ident.ap()), R=[src_res, "ident"], W=["bank7"])
                    sc.op("dve", lambda e: e.tensor_copy(out=oT.ap()[:, j, t * 128:(t + 1) * 128], in_=bkh(7)[:, 0:128]), R=["bank7"], W=[("oT", t)])

                def attn_diff(j):
                    units = []
                    for qc in range(8):
                        for c in range(2):
                            for kt in range(4 * qc + 4):
                                units.append((qc, c, kt))
                    n = len(units)
                    gidx = {}
                    for (qc, c, kt) in units:
                        gidx.setdefault((qc, c), len(gidx))

                    def stage_A(u):
                        qc, c, kt = units[u]
                        qlo = max(0, kt - 4 * qc)
                        zb = (0, 1, 6)[u % 3]
                        sc.op("pe", lambda e: e.matmul(bk(zb)[:, qlo * 128:512], lhsT=kT.ap()[:, kt * 128:(kt + 1) * 128],
                                                       rhs=qT.ap()[:, c, qc * 512 + qlo * 128:(qc + 1) * 512], start=True, stop=True),
                              R=["qT", "kT"], W=[f"bank{zb}"])

                    def stage_B(u):
                        qc, c, kt = units[u]
                        qlo = max(0, kt - 4 * qc)
                        zb = (0, 1, 6)[u % 3]
                        pb = u % NB
                        sc.op("act", lambda e: e.activation(out=Pt[pb].ap()[:, qlo * 128:512], in_=bk(zb)[:, qlo * 128:512], func=AF.Exp),
                              R=[f"bank{zb}"], W=[f"Pt{pb}"])
                        for Dd in range(2):
                            ql = kt + Dd - 4 * qc
                            if 0 <= ql <= 3:
                                sc.op("dve", lambda e, ql=ql, Dd=Dd: e.tensor_tensor(out=Pt[pb].ap()[:, ql * 128:(ql + 1) * 128], in0=Pt[pb].ap()[:, ql * 128:(ql + 1) * 128],
                                                                                    in1=Ep.ap()[:, j, Dd, :], op=ALU.mult),
                                      R=[f"Pt{pb}", "Ep"], W=[f"Pt{pb}"])

                    def stage_H(u):
                        qc, c, kt = units[u]
                        qlo = max(0, kt - 4 * qc)
                        pb = u % NB
                        g = gidx[(qc, c)]
                        ob = 2 + 2 * (g % 2)
                        for ql in range(qlo, 4):
                            bank = ob + ql // 2
                            col = (ql % 2) * 256
                            sc.op("pe", lambda e, ql=ql, bank=bank, col=col: e.matmul(
                                bk(bank)[:, col:col + 129], lhsT=Pt[pb].ap()[:, ql * 128:(ql + 1) * 128], rhs=V.ap()[:, kt, 0:129],
                                start=(kt == 0 and ql % 2 == 0), stop=(kt == 4 * qc + ql), skip_group_check=True),
                                R=[f"Pt{pb}", "V", "Vones"], W=[f"bank{bank}"])
                        if kt == 4 * qc + 3:
                            for ql in range(4):
                                pending.append((u + 1 + ql, (lambda ql=ql, c=c, qc=qc, ob=ob: evac_chain(ql, c, qc, ob))))

                    def evac_chain(ql, c, qc, ob):
                        bank = ob + ql // 2
                        col = (ql % 2) * 256
                        s_ = sm[ql]
                        sres = f"sm{ql}"
                        sc.op("dve", lambda e: e.reciprocal(out=s_.ap()[:, 0:1], in_=bk(bank)[:, col + 128:col + 129]), R=[f"bank{bank}"], W=[sres])
                        if c == 0:
                            sc.op("dve", lambda e: e.tensor_scalar(out=R1.ap()[:, ql, :], in0=bk(bank)[:, col:col + 128], scalar1=s_.ap()[:, 0:1], scalar2=None, op0=ALU.mult),
                                  R=[f"bank{bank}", sres], W=[("R1", ql)])
                            return
                        o_ = ot[ql]
                        ores = f"ot{ql}"
                        nb_ = onb[ql]
                        nres = f"onb{ql}"
                        sc.op("dve", lambda e: e.tensor_tensor(out=s_.ap()[:, 1:2], in0=s_.ap()[:, 0:1], in1=lam.ap()[:, 4:5], op=ALU.mult), R=[sres, "lam"], W=[sres])
                        sc.op("dve", lambda e: e.scalar_tensor_tensor(out=o_.ap(), in0=bk(bank)[:, col:col + 128], scalar=s_.ap()[:, 1:2], in1=R1.ap()[:, ql, :],
                                                                      op0=ALU.mult, op1=ALU.add), R=[f"bank{bank}", sres, ("R1", ql)], W=[ores])
                        sc.op("act", lambda e: e.activation(out=junk.ap(), in_=o_.ap(), func=AF.Square, accum_out=s_.ap()[:, 2:3]), R=[ores], W=[sres, "junk"])
                        sc.op("act", lambda e: e.activation(out=s_.ap()[:, 3:4], in_=s_.ap()[:, 2:3], func=AF.Ln, bias=float(RMS_EPS), scale=1.0 / 128.0),
                              R=[sres, "cst"], W=[sres])
                        sc.op("act", lambda e: e.activation(out=s_.ap()[:, 4:5], in_=s_.ap()[:, 3:4], func=AF.Exp, scale=-0.5), R=[sres], W=[sres])
                        def part2():
                            sc.op("dve", lambda e: e.scalar_tensor_tensor(out=nb_.ap(), in0=o_.ap(), scalar=s_.ap()[:, 4:5], in1=gsub.ap(), op0=ALU.mult, op1=ALU.mult),
                                  R=[ores, sres, "gsub"], W=[nres])
                            pending.append((cur[0] + 2, (lambda: finish_tile(j, 4 * qc + ql, nb_.ap(), nres))))
                        pending.append((cur[0] + 2, part2))

                    pending = []
                    cur = [0]

                    def run_pending(i, flush=False):
                        k = 0
                        while k < len(pending):
                            if flush or pending[k][0] <= i:
                                fn = pending.pop(k)[1]
                                fn()
                            else:
                                k += 1

                    for i in range(-3, n):
                        cur[0] = i
                        if 0 <= i + 3 < n:
                            stage_A(i + 3)
                        if 0 <= i + 2 < n:
                            stage_B(i + 2)
                        if 0 <= i < n:
                            stage_H(i)
                        run_pending(i)
                    cur[0] = n
                    while pending:
                        run_pending(n, flush=True)

                def attn_sb2(j):
                    pairs = [(qc, kt) for qc in range(8) for kt in range(4 * qc + 3, -1, -1)]
                    n = len(pairs)

                    def geo(p):
                        qc, kt = pairs[p]
                        return qc, kt, max(0, kt - 4 * qc)

                    def zb(p, c):
                        return (0, 1)[c] if p % 2 == 0 else (6, 7)[c]

                    def pair_ps(ti, lo):
                        return PP[ti].ap().rearrange("p (b f) -> p b f", b=2)[:, :, lo:512]

                    def st_A(p):
                        qc, kt, qlo = geo(p)
                        lo = qlo * 128
                        for c in range(2):
                            z = zb(p, c)
                            sc.op("pe", lambda e, c=c, z=z: e.matmul(bk(z)[:, lo:512], lhsT=kT.ap()[:, kt * 128:(kt + 1) * 128],
                                                                     rhs=qT.ap()[:, c, qc * 512 + lo:(qc + 1) * 512], start=True, stop=True),
                                  R=["qT", "kT"], W=[f"bank{z}"])

                    def st_B(p):
                        qc, kt, qlo = geo(p)
                        lo = qlo * 128
                        b = p % 3
                        sc.op("act", lambda e: e.activation(out=eb2[b].ap()[:, :, lo:512], in_=pair_ps(0 if p % 2 == 0 else 3, lo), func=AF.Exp),
                              R=[f"bank{zb(p, 0)}", f"bank{zb(p, 1)}"], W=[f"eb{b}"])
                        if kt >= 4 * qc:
                            sc.op("dve", lambda e: e.tensor_tensor(out=eb2[b].ap()[:, :, lo:lo + 128], in0=eb2[b].ap()[:, :, lo:lo + 128],
                                                                  in1=mask_lt.ap().unsqueeze(1).to_broadcast([128, 2, 128]), op=ALU.mult),
                                  R=[f"eb{b}", "mask_lt"], W=[f"eb{b}"])

                    def st_C(p):
                        qc, kt, qlo = geo(p)
                        lo = qlo * 128
                        b = p % 3
                        sc.op("act", lambda e: e.activation(out=sp2[b].ap()[:, :, lo:512], in_=eb2[b].ap()[:, :, lo:512], func=AF.Ln, bias=1.0, scale=1.0),
                              R=[f"eb{b}"], W=[f"spb{b}"])

                    def st_D(p):
                        qc, kt, qlo = geo(p)
                        lo = qlo * 128
                        b = p % 3
                        for c in range(2):
                            sc.op("pe", lambda e, c=c: e.matmul(bk(2 + c)[:, lo:512], lhsT=tri_incl.ap(), rhs=sp2[b].ap()[:, c, lo:512],
                                                                start=(kt == 4 * qc + 3), stop=False, skip_group_check=True),
                                  R=[f"spb{b}", "tri_incl"], W=[f"bank{2 + c}"])

                    def st_E(p):
                        qc, kt, qlo = geo(p)
                        lo = qlo * 128
                        b2 = p % 2
                        sc.op("act", lambda e: e.activation(out=g2b[b2].ap()[:, :, lo:512], in_=pair_ps(1, lo), func=AF.Exp, scale=-1.0),
                              R=["bank2", "bank3"], W=[f"gb_{b2}"])

                    def st_F(p):
                        qc, kt, qlo = geo(p)
                        lo = qlo * 128
                        b = p % 3
                        if kt == 0:
                            return
                        for c in range(2):
                            sc.op("pe", lambda e, c=c: e.matmul(bk(2 + c)[:, lo:512], lhsT=tri_low.ap(), rhs=sp2[b].ap()[:, c, lo:512],
                                                                start=False, stop=False, skip_group_check=True),
                                  R=[f"spb{b}", "tri_low"], W=[f"bank{2 + c}"])

                    def st_G(p):
                        qc, kt, qlo = geo(p)
                        lo = qlo * 128
                        b = p % 3
                        b2 = p % 2
                        sc.op("dve", lambda e: e.tensor_tensor(out=w2b[b2].ap()[:, :, lo:512], in0=eb2[b].ap()[:, :, lo:512], in1=g2b[b2].ap()[:, :, lo:512], op=ALU.mult),
                              R=[f"eb{b}", f"gb_{b2}"], W=[f"wb{b2}"])

                    def st_H(p):
                        qc, kt, qlo = geo(p)
                        b2 = p % 2
                        ob = 4 + (qc % 2)
                        for c in range(2):
                            for ql in range(qlo, 4):
                                col = (c * 4 + ql) * 64
                                sc.op("pe", lambda e, c=c, ql=ql, col=col: e.matmul(
                                    bk(ob)[:, col:col + 64], lhsT=w2b[b2].ap()[:, c, ql * 128:(ql + 1) * 128], rhs=V.ap()[:, kt, c * 64:(c + 1) * 64],
                                    start=(kt == 4 * qc + 3 and c == 0), stop=(kt == 0), skip_group_check=True),
                                    R=[f"wb{b2}", "V"], W=[f"bank{ob}"])
                        if kt == 0:
                            for ql in range(4):
                                pending.append((cur[0] + 1 + ql, (lambda ql=ql, qc=qc, ob=ob: evac_sb(ql, qc, ob))))

                    def evac_sb(ql, qc, ob):
                        nb_ = onb[ql]
                        nres = f"onb{ql}"
                        src = bass.AP(tensor=bk(ob).tensor, offset=bk(ob).offset + ql * 64, ap=[list(bk(ob).ap[0]), [256, 2], [1, 64]])
                        sc.op("dve", lambda e: e.tensor_copy(out=nb_.ap().rearrange("p (a f) -> p a f", a=2), in_=src), R=[f"bank{ob}"], W=[nres])
                        pending.append((cur[0] + 2, (lambda: finish_tile(j, 4 * qc + ql, nb_.ap(), nres))))

                    pending = []
                    cur = [0]

                    def run_pending(i, flush=False):
                        k = 0
                        while k < len(pending):
                            if flush or pending[k][0] <= i:
                                fn = pending.pop(k)[1]
                                fn()
                            else:
                                k += 1

                    for i in range(-2, n + 1):
                        cur[0] = i
                        if 0 <= i + 2 < n:
                            st_A(i + 2)
                        if 0 <= i + 1 < n:
                            st_B(i + 1)
                        if 0 <= i - 1 < n:
                            st_F(i - 1)
                        if 0 <= i < n:
                            st_D(i)
                            st_E(i)
                        if 0 <= i + 1 < n:
                            st_C(i + 1)
                        if 0 <= i < n:
                            st_G(i)
                        if 0 <= i - 1 < n:
                            st_H(i - 1)
                        run_pending(i)
                    cur[0] = n + 1
                    while pending:
                        run_pending(n + 1, flush=True)

                def attn_sb(j):
                    units = []
                    for qc in range(8):
                        for kt in range(4 * qc + 3, -1, -1):
                            for c in range(2):
                                units.append((qc, c, kt))
                    n = len(units)

                    def geo(u):
                        qc, c, kt = units[u]
                        return qc, c, kt, max(0, kt - 4 * qc)

                    def stage_A(u):
                        qc, c, kt, qlo = geo(u)
                        zb = (0, 1, 6)[u % 3]
                        sc.op("pe", lambda e: e.matmul(bk(zb)[:, qlo * 128:512], lhsT=kT.ap()[:, kt * 128:(kt + 1) * 128],
                                                       rhs=qT.ap()[:, c, qc * 512 + qlo * 128:(qc + 1) * 512], start=True, stop=True),
                              R=["qT", "kT"], W=[f"bank{zb}"])

                    def stage_B(u):
                        qc, c, kt, qlo = geo(u)
                        zb = (0, 1, 6)[u % 3]
                        b = u % NB
                        sc.op("act", lambda e: e.activation(out=eb[b].ap()[:, qlo * 128:512], in_=bk(zb)[:, qlo * 128:512], func=AF.Exp),
                              R=[f"bank{zb}"], W=[f"eb{b}"])
                        if kt >= 4 * qc:
                            sc.op("dve", lambda e: e.tensor_tensor(out=eb[b].ap()[:, qlo * 128:(qlo + 1) * 128], in0=eb[b].ap()[:, qlo * 128:(qlo + 1) * 128],
                                                                  in1=mask_lt.ap(), op=ALU.mult), R=[f"eb{b}", "mask_lt"], W=[f"eb{b}"])

                    def stage_C(u):
                        qc, c, kt, qlo = geo(u)
                        b = u % NB
                        sc.op("act", lambda e: e.activation(out=spb[b].ap()[:, qlo * 128:512], in_=eb[b].ap()[:, qlo * 128:512], func=AF.Ln, bias=1.0, scale=1.0),
                              R=[f"eb{b}"], W=[f"spb{b}"])

                    def stage_D(u):
                        qc, c, kt, qlo = geo(u)
                        b = u % NB
                        cb = 2 + c
                        sc.op("pe", lambda e: e.matmul(bk(cb)[:, qlo * 128:512], lhsT=tri_incl.ap(), rhs=spb[b].ap()[:, qlo * 128:512],
                                                       start=(kt == 4 * qc + 3), stop=False, skip_group_check=True),
                              R=[f"spb{b}", "tri_incl"], W=[f"bank{cb}"])

                    def stage_E(u):
                        qc, c, kt, qlo = geo(u)
                        b = u % NB
                        cb = 2 + c
                        sc.op("act", lambda e: e.activation(out=gb_[b].ap()[:, qlo * 128:512], in_=bk(cb)[:, qlo * 128:512], func=AF.Exp, scale=-1.0),
                              R=[f"bank{cb}"], W=[f"gb_{b}"])

                    def stage_F(u):
                        qc, c, kt, qlo = geo(u)
                        b = u % NB
                        cb = 2 + c
                        if kt == 0:
                            return
                        sc.op("pe", lambda e: e.matmul(bk(cb)[:, qlo * 128:512], lhsT=tri_low.ap(), rhs=spb[b].ap()[:, qlo * 128:512],
                                                       start=False, stop=False, skip_group_check=True),
                              R=[f"spb{b}", "tri_low"], W=[f"bank{cb}"])

                    def stage_G(u):
                        qc, c, kt, qlo = geo(u)
                        b = u % NB
                        sc.op("dve", lambda e: e.tensor_tensor(out=wb[b].ap()[:, qlo * 128:512], in0=eb[b].ap()[:, qlo * 128:512], in1=gb_[b].ap()[:, qlo * 128:512], op=ALU.mult),
                              R=[f"eb{b}", f"gb_{b}"], W=[f"wb{b}"])

                    def stage_H(u):
                        qc, c, kt, qlo = geo(u)
                        b = u % NB
                        ob = 4 + (qc % 2)
                        for ql in range(qlo, 4):
                            col = (c * 4 + ql) * 64
                            sc.op("pe", lambda e, ql=ql, col=col: e.matmul(
                                bk(ob)[:, col:col + 64], lhsT=wb[b].ap()[:, ql * 128:(ql + 1) * 128], rhs=V.ap()[:, kt, c * 64:(c + 1) * 64],
                                start=(kt == 4 * qc + 3 and c == 0), stop=(kt == 0), skip_group_check=True),
                                R=[f"wb{b}", "V"], W=[f"bank{ob}"])
                        if kt == 0 and c == 1:
                            for ql in range(4):
                                pending.append((cur[0] + 1 + ql, (lambda ql=ql, qc=qc, ob=ob: evac_sb(ql, qc, ob))))

                    def evac_sb(ql, qc, ob):
                        nb_ = onb[ql]
                        nres = f"onb{ql}"
                        src = bass.AP(tensor=bk(ob).tensor, offset=bk(ob).offset + ql * 64, ap=[list(bk(ob).ap[0]), [256, 2], [1, 64]])
                        sc.op("dve", lambda e: e.tensor_copy(out=nb_.ap().rearrange("p (a f) -> p a f", a=2), in_=src), R=[f"bank{ob}"], W=[nres])
                        pending.append((cur[0] + 2, (lambda: finish_tile(j, 4 * qc + ql, nb_.ap(), nres))))

                    pending = []
                    cur = [0]

                    def run_pending(i, flush=False):
                        k = 0
                        while k < len(pending):
                            if flush or pending[k][0] <= i:
                                fn = pending.pop(k)[1]
                                fn()
                            else:
                                k += 1

                    for i in range(-4, n + 1):
                        cur[0] = i
                        if 0 <= i + 3 < n:
                            stage_A(i + 3)
                        if 0 <= i + 2 < n:
                            stage_B(i + 2)
                        if 0 <= i + 1 < n:
                            stage_D(i + 1)
                            stage_E(i + 1)
                        if 0 <= i + 2 < n:
                            stage_C(i + 2)
                        if 0 <= i < n:
                            stage_F(i)
                            stage_G(i)
                        if 0 <= i - 1 < n:
                            stage_H(i - 1)
                        run_pending(i)
                    cur[0] = n + 1
                    while pending:
                        run_pending(n + 1, flush=True)

                def convert_experts(j):
                    for ei in range(4 * j, 4 * j + 4):
                        for wi, (wsrc, nch) in enumerate(((w1, 8), (w3, 8), (w2, 4))):
                            dstv = Wc[wi][ei * 128:(ei + 1) * 128, :].rearrange("p (c h) -> p c h", c=nch)
                            srcv = wsrc[layer, ei].rearrange("(c p) h -> p c h", p=128)
                            sc.dma("pool", lambda e, dstv=dstv, srcv=srcv: e.dma_start(out=dstv, in_=srcv), R=[("oT", NT - 1)], W=[("Wc", wi, ei)], pool="cv", n=8)

                load_w(0)
                for j in range(8):
                    if j + 1 < 8:
                        load_w(j + 1)
                    project(j)
                    if kind == "diff":
                        attn_diff(j)
                    else:
                        attn_sb2(j)
                    if j < 7:
                        convert_experts(j)
                    if j == 6:
                        convert_experts(7)
                sc.barrier()

            with ExitStack() as es3:
                ent = es3.enter_context
                wo = ent(sbt("wo", [128, 8, D], BF16))
                gb = ent(sbt("gb", [128, 2, D], F32))
                NBW = 4
                stat = [ent(sbt(f"stat{i}", [128, 20], F32)) for i in range(NBW)]
                xres = [ent(sbt(f"xres{i}", [128, D], F32)) for i in range(NBW)]
                yb = [ent(sbt(f"yb{i}", [128, D], F32)) for i in range(NBW)]
                xbf = [ent(sbt(f"xbf{i}", [128, D], BF16)) for i in range(NBW)]
                for c in range(8):
                    sc.dma("pool", lambda e, c=c: e.dma_start(out=wo.ap()[:, c, :], in_=W_["wo"][c * 128:(c + 1) * 128, :]), W=[("wo", c)], pool="wo", n=8)
                load_gb(gb, ln_mix_g, ln_mix_b, layer)
                src = xr[2 * layer]
                dst = xr[2 * layer + 1]
                def wo_produce_a(t):
                    b = t % NBW
                    sc.dma("sp", lambda e: e.dma_start(out=xres[b].ap(), in_=src[t * 128:(t + 1) * 128, :]), W=[f"xres{b}"], pool="xr", n=4)
                    for hh in range(2):
                        pb = 2 * (t % 2) + hh
                        for jj in range(8):
                            sc.op("pe", lambda e, jj=jj, hh=hh, pb=pb: e.matmul(
                                bk(pb)[:, :], lhsT=oT.ap()[:, jj, t * 128:(t + 1) * 128], rhs=wo.ap()[:, jj, hh * 512:(hh + 1) * 512],
                                start=(jj == 0), stop=(jj == 7)), R=[("oT", t), ("wo", jj)], W=[f"bank{pb}"])

                def wo_produce(t):
                    b = t % NBW
                    for hh in range(2):
                        pb = 2 * (t % 2) + hh
                        sc.op("dve", lambda e, hh=hh, pb=pb: e.scalar_tensor_tensor(
                            out=yb[b].ap()[:, hh * 512:(hh + 1) * 512], in0=xres[b].ap()[:, hh * 512:(hh + 1) * 512], scalar=float(ALPHA),
                            in1=bk(pb)[:, :], op0=ALU.mult, op1=ALU.add), R=[f"xres{b}", f"bank{pb}"], W=[f"yb{b}"])

                ln_pipeline(None, wo_produce_a, wo_produce, yb, (lambda b: f"yb{b}"), stat, gb, dst, xbf)
                sc.barrier()

    def moe_layer(layer):
        with ExitStack() as es:
            ent = es.enter_context
            TG = 8
            acc = ent(sbt("acc", [128, TG, D], F32))
            wA = [ent(sbt(f"wA{i}", [128, 2, 8, HID], BF16)) for i in range(2)]
            wB = [ent(sbt(f"wB{i}", [128, 4, D], BF16)) for i in range(2)]
            sl = [ent(sbt(f"sl{i}", [128, 4, 512], BF16)) for i in range(2)]
            gg = [ent(sbt(f"gg{i}", [128, 4, 512], BF16)) for i in range(2)]
            wr = ent(sbt("wr", [128, 8, 36], BF16))
            br = ent(sbt("br", [128, 36], F32))
            lg = ent(sbt("lg", [128, NT, 36], F32))
            gates = ent(sbt("gates", [128, NT, NE], F32))
            rt = ent(sbt("rt", [128, 8, 64], F32))
            gb = ent(sbt("gb", [128, 2, D], F32))
            stat = ent(sbt("stat", [128, 20], F32))
            xres = [ent(sbt(f"xres{i}", [128, D], F32)) for i in range(2)]
            xbf = [ent(sbt(f"xbf{i}", [128, D], BF16)) for i in range(2)]

            sc.dma("pool", lambda e: e.dma_start(out=wr.ap()[:, :, 0:4], in_=w_group[layer].rearrange("(c p) g -> p c g", p=128)), W=["wr"])
            sc.dma("pool", lambda e: e.dma_start(out=wr.ap()[:, :, 4:36], in_=w_expert[layer].rearrange("(c p) g -> p c g", p=128)), W=["wr"])
            sc.dma("sp", lambda e: e.dma_start(out=br.ap()[:, 0:4], in_=b_group[layer:layer + 1, :].partition_broadcast(128)), W=["br"])
            sc.dma("sp", lambda e: e.dma_start(out=br.ap()[:, 4:36], in_=b_expert[layer:layer + 1, :].partition_broadcast(128)), W=["br"])
            load_gb(gb, ln_ffn_g, ln_ffn_b, layer)
            for t in range(NT):
                pb = 6 + (t % 2)
                for c in range(8):
                    sc.op("pe", lambda e, c=c, t=t, pb=pb: e.matmul(bk(pb)[:, 0:36], lhsT=xT.ap()[:, c, t * 128:(t + 1) * 128], rhs=wr.ap()[:, c, :],
                                                                    start=(c == 0), stop=(c == 7)), R=[("xT", t), "wr"], W=[f"bank{pb}"])
                sc.op("dve", lambda e, t=t, pb=pb: e.tensor_tensor(out=lg.ap()[:, t, :], in0=bk(pb)[:, 0:36], in1=br.ap(), op=ALU.add),
                      R=[f"bank{pb}", "br"], W=["lg"])
            for t in range(NT):
                r_ = rt.ap()
                L = lg.ap()[:, t, :]
                ops = []
                sc.op("dve", lambda e, L=L: e.tensor_reduce(out=r_[:, 0, 0:1], in_=L[:, 0:4], axis=AX.X, op=ALU.max), R=["lg"], W=["rt"])
                sc.op("dve", lambda e, L=L: e.tensor_scalar(out=r_[:, 0, 4:8], in0=L[:, 0:4], scalar1=r_[:, 0, 0:1], scalar2=None, op0=ALU.is_equal), R=["lg", "rt"], W=["rt"])
                sc.op("dve", lambda e, L=L: e.tensor_scalar(out=r_[:, 0, 8:12], in0=L[:, 0:4], scalar1=r_[:, 0, 0:1], scalar2=None, op0=ALU.subtract), R=["lg", "rt"], W=["rt"])
                sc.op("act", lambda e: e.activation(out=r_[:, 0, 8:12], in_=r_[:, 0, 8:12], func=AF.Exp, accum_out=r_[:, 0, 1:2]), R=["rt"], W=["rt"])
                sc.op("dve", lambda e: e.reciprocal(out=r_[:, 0, 2:3], in_=r_[:, 0, 1:2]), R=["rt"], W=["rt"])
                sc.op("dve", lambda e: e.tensor_scalar(out=r_[:, 0, 12:16], in0=r_[:, 0, 4:8], scalar1=-1.0, scalar2=1e30, op0=ALU.add, op1=ALU.mult), R=["rt"], W=["rt"])
                sc.op("dve", lambda e, L=L: e.tensor_tensor(out=r_[:, 1, 0:32].rearrange("p (g e) -> p g e", g=4), in0=L[:, 4:36].rearrange("p (g e) -> p g e", g=4),
                                                            in1=r_[:, 0, 12:16].unsqueeze(2).to_broadcast([128, 4, 8]), op=ALU.add), R=["lg", "rt"], W=["rt"])
                sc.op("dve", lambda e: e.max(out=r_[:, 2, 0:8], in_=r_[:, 1, 0:32]), R=["rt"], W=["rt"])
                sc.op("dve", lambda e: e.tensor_scalar(out=r_[:, 3, 0:32], in0=r_[:, 1, 0:32], scalar1=r_[:, 2, 0:1], scalar2=None, op0=ALU.is_equal), R=["rt"], W=["rt"])
                sc.op("dve", lambda e: e.tensor_scalar(out=r_[:, 4, 0:32], in0=r_[:, 1, 0:32], scalar1=r_[:, 2, 1:2], scalar2=None, op0=ALU.is_equal), R=["rt"], W=["rt"])
                sc.op("dve", lambda e: e.tensor_tensor(out=r_[:, 2, 8:9], in0=r_[:, 2, 1:2], in1=r_[:, 2, 0:1], op=ALU.subtract), R=["rt"], W=["rt"])
                sc.op("act", lambda e: e.activation(out=r_[:, 2, 9:10], in_=r_[:, 2, 8:9], func=AF.Exp), R=["rt"], W=["rt"])
                sc.op("dve", lambda e: e.tensor_scalar(out=r_[:, 2, 10:11], in0=r_[:, 2, 9:10], scalar1=1.0, scalar2=None, op0=ALU.add), R=["rt"], W=["rt"])
                sc.op("dve", lambda e: e.reciprocal(out=r_[:, 2, 11:12], in_=r_[:, 2, 10:11]), R=["rt"], W=["rt"])
                sc.op("dve", lambda e: e.tensor_tensor(out=r_[:, 2, 12:13], in0=r_[:, 2, 11:12], in1=r_[:, 0, 2:3], op=ALU.mult), R=["rt"], W=["rt"])
                sc.op("dve", lambda e: e.tensor_tensor(out=r_[:, 2, 13:14], in0=r_[:, 0, 2:3], in1=r_[:, 2, 12:13], op=ALU.subtract), R=["rt"], W=["rt"])
                sc.op("dve", lambda e: e.tensor_scalar(out=r_[:, 3, 0:32], in0=r_[:, 3, 0:32], scalar1=r_[:, 2, 12:13], scalar2=None, op0=ALU.mult), R=["rt"], W=["rt"])
                sc.op("dve", lambda e, t=t: e.scalar_tensor_tensor(out=gates.ap()[:, t, :], in0=r_[:, 4, 0:32], scalar=r_[:, 2, 13:14], in1=r_[:, 3, 0:32],
                                                                   op0=ALU.mult, op1=ALU.add), R=["rt"], W=[("gates", t)])

            def load_expert(ei, b):
                sc.dma("pool", lambda e: e.dma_start(out=wA[b].ap()[:, 0, :, :], in_=w1[layer, ei].rearrange("(c p) h -> p c h", p=128)), W=[f"wA{b}"])
                sc.dma("pool", lambda e: e.dma_start(out=wA[b].ap()[:, 1, :, :], in_=w3[layer, ei].rearrange("(c p) h -> p c h", p=128)), W=[f"wA{b}"])
                sc.dma("pool", lambda e: e.dma_start(out=wB[b].ap(), in_=w2[layer, ei].rearrange("(c p) d -> p c d", p=128)), W=[f"wB{b}"])

            src = xr[2 * layer + 1]
            dst = xr[2 * layer + 2]
            last = (layer == DEPTH - 1)
            ngroups = NT // TG
            it = 0
            load_expert(0, 0)
            for G in range(ngroups):
                for ei in range(NE):
                    b = it % 2
                    nxt = it + 1
                    if nxt < ngroups * NE:
                        load_expert(nxt % NE, nxt % 2)
                    for half in range(TG // 4):
                        tok0 = (G * TG + half * 4) * 128
                        hb = (it * (TG // 4) + half) % 2
                        for m in range(4):
                            for which in range(2):
                                pb = 2 * which + (m % 2)
                                for c in range(8):
                                    sc.op("pe", lambda e, c=c, m=m, which=which, pb=pb: e.matmul(
                                        bk(pb)[:, :], lhsT=wA[b].ap()[:, which, c, m * 128:(m + 1) * 128], rhs=xT.ap()[:, c, tok0:tok0 + 512],
                                        start=(c == 0), stop=(c == 7)),
                                        R=[f"wA{b}"] + [("xT", tok0 // 128 + q) for q in range(4)], W=[f"bank{pb}"])
                            sc.op("act", lambda e, m=m, hb=hb: e.activation(out=sl[hb].ap()[:, m, :], in_=bk(m % 2)[:, :], func=AF.Silu),
                                  R=[f"bank{m % 2}"], W=[f"sl{hb}"])
                            sc.op("dve", lambda e, m=m, hb=hb: e.tensor_tensor(out=gg[hb].ap()[:, m, :], in0=sl[hb].ap()[:, m, :], in1=bk(2 + m % 2)[:, :], op=ALU.mult),
                                  R=[f"sl{hb}", f"bank{2 + m % 2}"], W=[f"gg{hb}"])
                        for tt in range(4):
                            tl = half * 4 + tt
                            tglob = G * TG + tl
                            for hh in range(2):
                                pb = 4 + hh
                                for m in range(4):
                                    sc.op("pe", lambda e, m=m, tt=tt, hh=hh, pb=pb: e.matmul(
                                        bk(pb)[:, :], lhsT=gg[hb].ap()[:, m, tt * 128:(tt + 1) * 128], rhs=wB[b].ap()[:, m, hh * 512:(hh + 1) * 512],
                                        start=(m == 0), stop=(m == 3)), R=[f"gg{hb}", f"wB{b}"], W=[f"bank{pb}"])
                                if ei == 0:
                                    sc.op("dve", lambda e, tl=tl, hh=hh, pb=pb, tglob=tglob: e.tensor_scalar(
                                        out=acc.ap()[:, tl, hh * 512:(hh + 1) * 512], in0=bk(pb)[:, :], scalar1=gates.ap()[:, tglob, ei:ei + 1], scalar2=None, op0=ALU.mult),
                                        R=[f"bank{pb}", ("gates", tglob)], W=[("acc", tl)])
                                else:
                                    sc.op("dve", lambda e, tl=tl, hh=hh, pb=pb, tglob=tglob, ei=ei: e.scalar_tensor_tensor(
                                        out=acc.ap()[:, tl, hh * 512:(hh + 1) * 512], in0=bk(pb)[:, :], scalar=gates.ap()[:, tglob, ei:ei + 1],
                                        in1=acc.ap()[:, tl, hh * 512:(hh + 1) * 512], op0=ALU.mult, op1=ALU.add),
                                        R=[f"bank{pb}", ("gates", tglob), ("acc", tl)], W=[("acc", tl)])
                    it += 1
                for tl in range(TG):
                    t = G * TG + tl
                    b2 = t % 2
                    sc.dma("sp", lambda e, b2=b2, t=t: e.dma_start(out=xres[b2].ap(), in_=src[t * 128:(t + 1) * 128, :]), W=[f"xres{b2}"], pool="xr", n=4)
                    sc.op("dve", lambda e, b2=b2, tl=tl: e.scalar_tensor_tensor(out=acc.ap()[:, tl, :], in0=xres[b2].ap(), scalar=float(ALPHA), in1=acc.ap()[:, tl, :],
                                                                                op0=ALU.mult, op1=ALU.add), R=[f"xres{b2}", ("acc", tl)], W=[("acc", tl)])
                    ln_tile(acc.ap()[:, tl, :], ("acc", tl), gb, stat, t, dst, None if last else (xbf[b2].ap(), f"xbf{b2}"), 6 + b2)
            sc.barrier()

    def moe_layer_sparse(layer):
        src = xr[2 * layer + 1]
        dst = xr[2 * layer + 2]
        last = (layer == DEPTH - 1)
        w13rows = [w1.rearrange("l e d h -> (l e d) h"), w3.rearrange("l e d h -> (l e d) h")]
        w2rows = w2.rearrange("l e h d -> (l e h) d")
        with ExitStack() as es:
            ent = es.enter_context
            WT = ent(sbt("WT", [128, NT, 2], F32))
            posi = ent(sbt("posi", [128, 2, NT], I32))
            widx = ent(sbt("widx", [128, 2, NSLOT], I32))
            with ExitStack() as es1:
                ent1 = es1.enter_context
                M1 = ent1(sbt("M1", [128, NT, NE], F32))
                M2 = ent1(sbt("M2", [128, NT, NE], F32))
                wr = ent1(sbt("wr", [128, 8, 36], BF16))
                br = ent1(sbt("br", [128, 36], F32))
                lg = ent1(sbt("lg", [128, NT, 36], F32))
                rs = ent1(sbt("rs", [128, 6, NT], F32))
                r4 = ent1(sbt("r4", [128, 3, NT, 4], F32))
                elm = ent1(sbt("elm", [128, NT, NE], F32))
                Abf = ent1(sbt("Abf", [128, NT, NE], BF16))
                ones_bf = ent1(sbt("ones_bf", [128, 128], BF16))
                cntS = ent1(sbt("cntS", [128, NE], F32))
                nsl = ent1(sbt("nsl", [128, NE], F32))
                offe = ent1(sbt("offe", [128, NE], F32))
                posb = ent1(sbt("posb", [128, NE], F32))
                posfull = ent1(sbt("posfull", [128, NT, NE], F32))
                ptmp = ent1(sbt("ptmp", [128, NT, NE], F32))
                posf = ent1(sbt("posf", [128, 2, NT], F32))
                sidx = ent1(sbt("sidx", [128, NSLOT], F32))
                pidx = ent1(sbt("pidx", [128, 4], F32))
                cmp_ = ent1(sbt("cmp", [128, NSLOT, NE], F32))
                eidf = ent1(sbt("eidf", [128, 3, NSLOT], F32))
                xres = [ent1(sbt(f"xres{i}", [128, D], F32)) for i in range(4)]
                xbf = [ent1(sbt(f"xbf{i}", [128, D], BF16)) for i in range(8)]

                sc.dma("pool", lambda e: e.dma_start(out=wr.ap()[:, :, 0:4], in_=w_group[layer].rearrange("(c p) g -> p c g", p=128)), W=["wr"])
                sc.dma("pool", lambda e: e.dma_start(out=wr.ap()[:, :, 4:36], in_=w_expert[layer].rearrange("(c p) g -> p c g", p=128)), W=["wr"])
                sc.dma("sp", lambda e: e.dma_start(out=br.ap()[:, 0:4], in_=b_group[layer:layer + 1, :].partition_broadcast(128)), W=["br"])
                sc.dma("sp", lambda e: e.dma_start(out=br.ap()[:, 4:36], in_=b_expert[layer:layer + 1, :].partition_broadcast(128)), W=["br"])
                sc.dma("sp", lambda e: e.dma_start(out=sidx.ap(), in_=c_sidx), W=["sidx"])
                sc.dma("sp", lambda e: e.dma_start(out=pidx.ap()[:, 0:1], in_=c_pidx), W=["pidx"])
                sc.op("dve", lambda e: e.memset(ones_bf.ap(), 1.0), W=["ones_bf"])
                for t in range(NT):
                    pb = 6 + (t % 2)
                    for c in range(8):
                        sc.op("pe", lambda e, c=c, t=t, pb=pb: e.matmul(bk(pb)[:, 0:36], lhsT=xT.ap()[:, c, t * 128:(t + 1) * 128], rhs=wr.ap()[:, c, :],
                                                                        start=(c == 0), stop=(c == 7)), R=[("xT", t), "wr"], W=[f"bank{pb}"])
                    sc.op("dve", lambda e, t=t, pb=pb: e.tensor_tensor(out=lg.ap()[:, t, :], in0=bk(pb)[:, 0:36], in1=br.ap(), op=ALU.add),
                          R=[f"bank{pb}", "br"], W=["lg"])
                LG = lg.ap()
                gmax = rs.ap()[:, 0, :]
                gsum = rs.ap()[:, 1, :]
                ggate = rs.ap()[:, 2, :]
                m1 = rs.ap()[:, 3, :]
                m2 = rs.ap()[:, 4, :]
                rr = rs.ap()[:, 5, :]
                gm = r4.ap()[:, 0, :, :]
                dd = r4.ap()[:, 1, :, :]
                pen = r4.ap()[:, 2, :, :]
                sc.op("dve", lambda e: e.tensor_reduce(out=gmax, in_=LG[:, :, 0:4], axis=AX.X, op=ALU.max), R=["lg"], W=["rs"])
                sc.op("dve", lambda e: e.tensor_tensor(out=gm, in0=LG[:, :, 0:4], in1=gmax.unsqueeze(2).to_broadcast([128, NT, 4]), op=ALU.is_equal), R=["lg", "rs"], W=["r4"])
                sc.op("dve", lambda e: e.tensor_tensor(out=dd, in0=LG[:, :, 0:4], in1=gmax.unsqueeze(2).to_broadcast([128, NT, 4]), op=ALU.subtract), R=["lg", "rs"], W=["r4"])
                sc.op("act", lambda e: e.activation(out=dd, in_=dd, func=AF.Exp), R=["r4"], W=["r4"])
                sc.op("dve", lambda e: e.tensor_reduce(out=gsum, in_=dd, axis=AX.X, op=ALU.add), R=["r4"], W=["rs"])
                sc.op("dve", lambda e: e.reciprocal(out=ggate, in_=gsum), R=["rs"], W=["rs"])
                sc.op("dve", lambda e: e.tensor_scalar(out=pen, in0=gm, scalar1=-1.0, scalar2=1e30, op0=ALU.add, op1=ALU.mult), R=["r4"], W=["r4"])
                elm4 = elm.ap().rearrange("p t (g e) -> p t g e", g=4)
                sc.op("dve", lambda e: e.tensor_tensor(out=elm4, in0=LG[:, :, 4:36].rearrange("p t (g e) -> p t g e", g=4),
                                                       in1=pen.unsqueeze(3).to_broadcast([128, NT, 4, 8]), op=ALU.add), R=["lg", "r4"], W=["elm"])
                sc.op("dve", lambda e: e.tensor_reduce(out=m1, in_=elm.ap(), axis=AX.X, op=ALU.max), R=["elm"], W=["rs"])
                sc.op("dve", lambda e: e.tensor_tensor(out=M1.ap(), in0=elm.ap(), in1=m1.unsqueeze(2).to_broadcast([128, NT, NE]), op=ALU.is_equal), R=["elm", "rs"], W=["M1"])
                sc.op("dve", lambda e: e.scalar_tensor_tensor(out=elm.ap(), in0=M1.ap(), scalar=-1e30, in1=elm.ap(), op0=ALU.mult, op1=ALU.add), R=["elm", "M1"], W=["elm"])
                sc.op("dve", lambda e: e.tensor_reduce(out=m2, in_=elm.ap(), axis=AX.X, op=ALU.max), R=["elm"], W=["rs"])
                sc.op("dve", lambda e: e.tensor_tensor(out=M2.ap(), in0=elm.ap(), in1=m2.unsqueeze(2).to_broadcast([128, NT, NE]), op=ALU.is_equal), R=["elm", "rs"], W=["M2"])
                sc.op("dve", lambda e: e.tensor_tensor(out=rr, in0=m2, in1=m1, op=ALU.subtract), R=["rs"], W=["rs"])
                sc.op("act", lambda e: e.activation(out=rr, in_=rr, func=AF.Exp), R=["rs"], W=["rs"])
                sc.op("dve", lambda e: e.tensor_scalar(out=rr, in0=rr, scalar1=1.0, scalar2=None, op0=ALU.add), R=["rs"], W=["rs"])
                sc.op("dve", lambda e: e.reciprocal(out=rr, in_=rr), R=["rs"], W=["rs"])
                sc.op("dve", lambda e: e.tensor_tensor(out=WT.ap()[:, :, 0], in0=rr, in1=ggate, op=ALU.mult), R=["rs"], W=["WT"])
                sc.op("dve", lambda e: e.tensor_tensor(out=WT.ap()[:, :, 1], in0=ggate, in1=WT.ap()[:, :, 0], op=ALU.subtract), R=["rs", "WT"], W=["WT"])
                sc.op("dve", lambda e: e.tensor_tensor(out=Abf.ap(), in0=M1.ap(), in1=M2.ap(), op=ALU.add), R=["M1", "M2"], W=["Abf"])
                for t in range(NT):
                    rb_ = t // 16
                    col = (t % 16) * 32
                    for tp in range(t):
                        sc.op("pe", lambda e, tp=tp, rb_=rb_, col=col: e.matmul(bk(rb_)[:, col:col + 32], lhsT=ones_bf.ap(), rhs=Abf.ap()[:, tp, :],
                                                                                  start=(tp == 0), stop=False, skip_group_check=True),
                              R=["Abf", "ones_bf"], W=[f"bank{rb_}"])
                    sc.op("pe", lambda e, t=t, rb_=rb_, col=col: e.matmul(bk(rb_)[:, col:col + 32], lhsT=tri_low.ap(), rhs=Abf.ap()[:, t, :],
                                                                          start=(t == 0), stop=True, skip_group_check=True),
                          R=["Abf", "tri_low"], W=[f"bank{rb_}"])
                for t in range(NT):
                    sc.op("pe", lambda e, t=t: e.matmul(bk(2)[:, 0:32], lhsT=ones_bf.ap(), rhs=Abf.ap()[:, t, :], start=(t == 0), stop=(t == NT - 1)),
                          R=["Abf", "ones_bf"], W=["bank2"])
                sc.op("dve", lambda e: e.tensor_copy(out=cntS.ap(), in_=bk(2)[:, 0:32]), R=["bank2"], W=["cntS"])
                sc.op("dve", lambda e: e.tensor_scalar(out=nsl.ap(), in0=cntS.ap(), scalar1=0.0, scalar2=None, op0=ALU.is_gt), R=["cntS"], W=["nsl"])
                for k in range(1, KMAX):
                    sc.op("dve", lambda e, k=k: e.scalar_tensor_tensor(out=nsl.ap(), in0=cntS.ap(), scalar=float(k * SL), in1=nsl.ap(), op0=ALU.is_gt, op1=ALU.add),
                          R=["cntS", "nsl"], W=["nsl"])
                sc.op("dve", lambda e: e.tensor_copy(out=offe.ap()[:, 0:1], in_=nsl.ap()[:, 0:1]), R=["nsl"], W=["offe"])
                for ei in range(1, NE):
                    sc.op("dve", lambda e, ei=ei: e.tensor_tensor(out=offe.ap()[:, ei:ei + 1], in0=offe.ap()[:, ei - 1:ei], in1=nsl.ap()[:, ei:ei + 1], op=ALU.add),
                          R=["nsl", "offe"], W=["offe"])
                sc.op("dve", lambda e: e.tensor_tensor(out=posb.ap(), in0=offe.ap(), in1=nsl.ap(), op=ALU.subtract), R=["offe", "nsl"], W=["posb"])
                sc.op("dve", lambda e: e.tensor_scalar(out=posb.ap(), in0=posb.ap(), scalar1=float(SL), scalar2=None, op0=ALU.mult), R=["posb"], W=["posb"])
                for hh in range(2):
                    sc.op("dve", lambda e, hh=hh: e.tensor_tensor(out=posfull.ap()[:, 16 * hh:16 * hh + 16, :], in0=bk(hh)[:, :].rearrange("p (a f) -> p a f", a=16),
                                                                  in1=posb.ap().unsqueeze(1).to_broadcast([128, 16, NE]), op=ALU.add),
                          R=[f"bank{hh}", "posb"], W=["posfull"])
                for ci, M_ in enumerate((M1, M2)):
                    mres = "M1" if ci == 0 else "M2"
                    sc.op("dve", lambda e, M_=M_: e.tensor_tensor(out=ptmp.ap(), in0=posfull.ap(), in1=M_.ap(), op=ALU.mult), R=["posfull", mres], W=["ptmp"])
                    sc.op("dve", lambda e, ci=ci: e.tensor_reduce(out=posf.ap()[:, ci, :], in_=ptmp.ap(), axis=AX.X, op=ALU.add), R=["ptmp"], W=["posf"])
                sc.op("dve", lambda e: e.tensor_scalar(out=posf.ap(), in0=posf.ap(), scalar1=float(NPOS - 1), scalar2=None, op0=ALU.min), R=["posf"], W=["posf"])
                sc.op("dve", lambda e: e.tensor_copy(out=posi.ap(), in_=posf.ap()), R=["posf"], W=["posi"])
                sc.op("dve", lambda e: e.tensor_tensor(out=cmp_.ap(), in0=offe.ap().unsqueeze(1).to_broadcast([128, NSLOT, NE]),
                                                       in1=sidx.ap().unsqueeze(2).to_broadcast([128, NSLOT, NE]), op=ALU.is_le), R=["offe", "sidx"], W=["cmp"])
                sc.op("dve", lambda e: e.tensor_reduce(out=eidf.ap()[:, 0, :], in_=cmp_.ap(), axis=AX.X, op=ALU.add), R=["cmp"], W=["eidf"])
                sc.op("dve", lambda e: e.tensor_scalar(out=eidf.ap()[:, 0, :], in0=eidf.ap()[:, 0, :], scalar1=float(NE - 1), scalar2=None, op0=ALU.min), R=["eidf"], W=["eidf"])
                sc.op("dve", lambda e: e.tensor_scalar(out=pidx.ap()[:, 1:2], in0=pidx.ap()[:, 0:1], scalar1=float(layer * NE * D), scalar2=None, op0=ALU.add), R=["pidx"], W=["pidx"])
                sc.op("dve", lambda e: e.tensor_scalar(out=pidx.ap()[:, 2:3], in0=pidx.ap()[:, 0:1], scalar1=float(layer * NE * HID), scalar2=None, op0=ALU.add), R=["pidx"], W=["pidx"])
                sc.op("dve", lambda e: e.tensor_scalar(out=eidf.ap()[:, 1, :], in0=eidf.ap()[:, 0, :], scalar1=128.0, scalar2=pidx.ap()[:, 0:1], op0=ALU.mult, op1=ALU.add),
                      R=["eidf", "pidx"], W=["eidf"])
                sc.op("dve", lambda e: e.tensor_scalar(out=eidf.ap()[:, 2, :], in0=eidf.ap()[:, 0, :], scalar1=128.0, scalar2=pidx.ap()[:, 0:1], op0=ALU.mult, op1=ALU.add),
                      R=["eidf", "pidx"], W=["eidf"])
                sc.op("dve", lambda e: e.tensor_copy(out=widx.ap(), in_=eidf.ap()[:, 1:3, :]), R=["eidf"], W=["widx"])
                for t in range(NT):
                    b = t % 4
                    b8 = t % 8
                    sc.dma("sp", lambda e, b=b, t=t: e.dma_start(out=xres[b].ap(), in_=src[t * 128:(t + 1) * 128, :]), W=[f"xres{b}"], pool="xr", n=4)
                    sc.op("act", lambda e, b=b, b8=b8: e.activation(out=xbf[b8].ap(), in_=xres[b].ap(), func=AF.Copy), R=[f"xres{b}"], W=[f"xbf{b8}"])
                    for ci in range(2):
                        sc.dma("pool", lambda e, b8=b8, t=t, ci=ci: e.indirect_dma_start(
                            out=Xs, out_offset=bass.IndirectOffsetOnAxis(ap=posi.ap()[:, ci, t:t + 1], axis=0), in_=xbf[b8].ap(), in_offset=None),
                            R=[f"xbf{b8}", "posi"], W=[("Xs", t, ci)], pool="sct", n=8)
                sc.barrier()

            with ExitStack() as es2:
                ent2 = es2.enter_context
                wA = [ent2(sbt(f"wA{i}", [128, 2, 8, HID], BF16)) for i in range(2)]
                wB = [ent2(sbt(f"wB{i}", [128, 4, D], BF16)) for i in range(2)]
                xs = [[ent2(sbt(f"xs{i}_{k}", [128, D], BF16)) for k in range(SL // 128)] for i in range(2)]
                xsT = [ent2(sbt(f"xsT{i}", [128, 8, SL], BF16)) for i in range(2)]
                sl = [ent2(sbt(f"sl{i}", [128, 4, SL], BF16)) for i in range(2)]
                gg = [ent2(sbt(f"gg{i}", [128, 4, SL], BF16)) for i in range(2)]
                ys = [ent2(sbt(f"ys{i}", [128, D], F32)) for i in range(2)]
                NS3 = SL // 128

                def load_slot(s_):
                    b = s_ % 2
                    for which in range(2):
                        sc.dma("pool", lambda e, which=which: e.indirect_dma_start(
                            out=wA[b].ap()[:, which, :, :].rearrange("p c h -> p (c h)"), out_offset=None, in_=Wc[which],
                            in_offset=bass.IndirectOffsetOnAxis(ap=widx.ap()[:, 0, s_:s_ + 1], axis=0)),
                            R=["widx"], W=[(f"wA{b}", which)], pool="wgt", n=12)
                    sc.dma("pool", lambda e: e.indirect_dma_start(
                        out=wB[b].ap().rearrange("p c d -> p (c d)"), out_offset=None, in_=Wc[2],
                        in_offset=bass.IndirectOffsetOnAxis(ap=widx.ap()[:, 0, s_:s_ + 1], axis=0)),
                        R=["widx"], W=[f"wB{b}"], pool="wgt", n=12)
                    for k in range(NS3):
                        r0 = s_ * SL + k * 128
                        sc.dma("sp", lambda e, k=k, r0=r0: e.dma_start(out=xs[b][k].ap(), in_=Xs[r0:r0 + 128, :]), R=["Xs"], W=[f"xs{b}_{k}"], pool="xs", n=6)

                load_slot(0)
                for s_ in range(NSLOT):
                    b = s_ % 2
                    if s_ + 1 < NSLOT:
                        load_slot(s_ + 1)
                    for k in range(NS3):
                        tb = 6 + (k % 2)
                        for c in range(8):
                            sc.op("pe", lambda e, c=c, k=k, tb=tb: e.transpose(out=bkh(tb)[:, c * 128:(c + 1) * 128], in_=xs[b][k].ap()[:, c * 128:(c + 1) * 128],
                                                                              identity=ident.ap()), R=[f"xs{b}_{k}", "ident"], W=[f"bank{tb}"])
                        sc.op("act" if k % 2 == 0 else "dve",
                              lambda e, k=k, tb=tb: (e.activation(out=xsT[b].ap()[:, :, k * 128:(k + 1) * 128], in_=bkh(tb)[:, :].rearrange("p (c f) -> p c f", c=8), func=AF.Copy)
                                                     if k % 2 == 0 else
                                                     e.tensor_copy(out=xsT[b].ap()[:, :, k * 128:(k + 1) * 128], in_=bkh(tb)[:, :].rearrange("p (c f) -> p c f", c=8))),
                              R=[f"bank{tb}"], W=[f"xsT{b}"])
                    for m in range(4):
                        for which in range(2):
                            pb = 2 * which + (m % 2)
                            for c in range(8):
                                sc.op("pe", lambda e, c=c, m=m, which=which, pb=pb: e.matmul(
                                    bk(pb)[:, 0:SL], lhsT=wA[b].ap()[:, which, c, m * 128:(m + 1) * 128], rhs=xsT[b].ap()[:, c, :],
                                    start=(c == 0), stop=(c == 7)), R=[(f"wA{b}", which), f"xsT{b}"], W=[f"bank{pb}"])
                        sc.op("act", lambda e, m=m: e.activation(out=sl[b].ap()[:, m, :], in_=bk(m % 2)[:, 0:SL], func=AF.Silu), R=[f"bank{m % 2}"], W=[f"sl{b}"])
                        sc.op("dve", lambda e, m=m: e.tensor_tensor(out=gg[b].ap()[:, m, :], in0=sl[b].ap()[:, m, :], in1=bk(2 + m % 2)[:, 0:SL], op=ALU.mult),
                              R=[f"sl{b}", f"bank{2 + m % 2}"], W=[f"gg{b}"])
                    for k in range(NS3):
                        yb_ = (s_ * NS3 + k) % 2
                        for hh in range(2):
                            pb = 4 + hh
                            for m in range(4):
                                sc.op("pe", lambda e, m=m, k=k, hh=hh, pb=pb: e.matmul(
                                    bk(pb)[:, :], lhsT=gg[b].ap()[:, m, k * 128:(k + 1) * 128], rhs=wB[b].ap()[:, m, hh * 512:(hh + 1) * 512],
                                    start=(m == 0), stop=(m == 3)), R=[f"gg{b}", f"wB{b}"], W=[f"bank{pb}"])
                            if hh == 0:
                                sc.op("act", lambda e, yb_=yb_, pb=pb: e.activation(out=ys[yb_].ap()[:, 0:512], in_=bk(pb)[:, :], func=AF.Copy), R=[f"bank{pb}"], W=[f"ys{yb_}"])
                            else:
                                sc.op("dve", lambda e, yb_=yb_, pb=pb: e.tensor_copy(out=ys[yb_].ap()[:, 512:1024], in_=bk(pb)[:, :]), R=[f"bank{pb}"], W=[f"ys{yb_}"])
                        r0 = s_ * SL + k * 128
                        sc.dma("sp", lambda e, yb_=yb_, r0=r0: e.dma_start(out=Ys[r0:r0 + 128, :], in_=ys[yb_].ap()), R=[f"ys{yb_}"], W=[("Ys", s_, k)], pool="ys", n=4)
                sc.barrier()

            with ExitStack() as es3:
                ent3 = es3.enter_context
                gb = ent3(sbt("gb", [128, 2, D], F32))
                NB3 = 4
                stat = [ent3(sbt(f"stat{i}", [128, 20], F32)) for i in range(NB3)]
                g1 = [ent3(sbt(f"g1_{i}", [128, D], F32)) for i in range(NB3)]
                g2 = [ent3(sbt(f"g2_{i}", [128, D], F32)) for i in range(NB3)]
                xres = [ent3(sbt(f"xres{i}", [128, D], F32)) for i in range(NB3)]
                yb = [ent3(sbt(f"yb{i}", [128, D], F32)) for i in range(NB3)]
                xbf = [ent3(sbt(f"xbf{i}", [128, D], BF16)) for i in range(NB3)]
                load_gb(gb, ln_ffn_g, ln_ffn_b, layer)
                def m3_fetch(t):
                    b = t % NB3
                    sc.dma("pool", lambda e: e.indirect_dma_start(out=g1[b].ap(), out_offset=None, in_=Ys,
                                                                  in_offset=bass.IndirectOffsetOnAxis(ap=posi.ap()[:, 0, t:t + 1], axis=0)),
                           R=["Ys", "posi"], W=[f"g1_{b}"], pool="gth", n=8)
                    sc.dma("pool", lambda e: e.indirect_dma_start(out=g2[b].ap(), out_offset=None, in_=Ys,
                                                                  in_offset=bass.IndirectOffsetOnAxis(ap=posi.ap()[:, 1, t:t + 1], axis=0)),
                           R=["Ys", "posi"], W=[f"g2_{b}"], pool="gth", n=8)
                    sc.dma("sp", lambda e: e.dma_start(out=xres[b].ap(), in_=src[t * 128:(t + 1) * 128, :]), W=[f"xres{b}"], pool="xr", n=4)

                def m3_produce_a(t):
                    b = t % NB3
                    sc.op("act", lambda e: e.activation(out=g1[b].ap(), in_=g1[b].ap(), func=AF.Copy, scale=WT.ap()[:, t, 0:1]), R=[f"g1_{b}", "WT"], W=[f"g1_{b}"])

                def m3_produce(t):
                    b = t % NB3
                    sc.op("dve", lambda e: e.scalar_tensor_tensor(out=g2[b].ap(), in0=g2[b].ap(), scalar=WT.ap()[:, t, 1:2], in1=g1[b].ap(), op0=ALU.mult, op1=ALU.add),
                          R=[f"g2_{b}", f"g1_{b}", "WT"], W=[f"g2_{b}"])
                    sc.op("dve", lambda e: e.scalar_tensor_tensor(out=yb[b].ap(), in0=xres[b].ap(), scalar=float(ALPHA), in1=g2[b].ap(), op0=ALU.mult, op1=ALU.add),
                          R=[f"xres{b}", f"g2_{b}"], W=[f"yb{b}"])

                ln_pipeline(m3_fetch, m3_produce_a, m3_produce, yb, (lambda b: f"yb{b}"), stat, gb, dst, None if last else xbf)
                sc.barrier()

    stages = []
    for layer in range(DEPTH):
        stages.append(("att", layer))
        stages.append(("moe", layer))
    for i, (kind, layer) in enumerate(stages):
        if stop_after is not None and i > stop_after:
            break
        if kind == "att":
            attention_layer(layer)
        elif SPARSE:
            moe_layer_sparse(layer)
        else:
            moe_layer(layer)

    sc.final_wait("sp")
    return nc


_CACHE = {}


def _prep_common(inputs):
    f = lambda a: np.ascontiguousarray(np.asarray(a), dtype=np.float32)
    m = {
        "rel_table": f(inputs["rel_table"]),
        "da_wq": f(inputs["da_wq"][0]), "da_wk": f(inputs["da_wk"][0]), "da_wv": f(inputs["da_wv"][0]), "da_wo": f(inputs["da_wo"][0]),
        "sb_wq": f(inputs["sb_wq"][0]), "sb_wk": f(inputs["sb_wk"][0]), "sb_wv": f(inputs["sb_wv"][0]), "sb_wo": f(inputs["sb_wo"][0]),
        "da_l": f(np.stack([inputs["da_lq1"][0], inputs["da_lk1"][0], inputs["da_lq2"][0], inputs["da_lk2"][0]], axis=0)),
        "da_subln_g": f(inputs["da_subln_g"]),
        "ln_mix_g": f(inputs["ln_mix_g"]), "ln_mix_b": f(inputs["ln_mix_b"]),
        "ln_ffn_g": f(inputs["ln_ffn_g"]), "ln_ffn_b": f(inputs["ln_ffn_b"]),
        "moe_w_group": f(inputs["moe_w_group"]), "moe_b_group": f(inputs["moe_b_group"]),
        "moe_w_expert": f(inputs["moe_w_expert"]), "moe_b_expert": f(inputs["moe_b_expert"]),
        "moe_w1": f(inputs["moe_w1"]), "moe_w3": f(inputs["moe_w3"]), "moe_w2": f(inputs["moe_w2"]),
    }
    m.update(_consts_host())
    return m


def kernel(**inputs):
    if "nc" not in _CACHE:
        _CACHE["nc"] = build_program()
    nc = _CACHE["nc"]
    common = _prep_common(inputs)
    x = np.asarray(inputs["x"], dtype=np.float32)
    in_maps = []
    for b in range(8):
        m = dict(common)
        m["x"] = np.ascontiguousarray(x[b])
        in_maps.append(m)
    res = run_bass_kernel_spmd(nc, in_maps, core_ids=list(range(8)))
    return np.stack([np.asarray(r["out"], dtype=np.float32) for r in res.results], axis=0)
```

```python
import math
from contextlib import ExitStack
import numpy as np
import ml_dtypes
import concourse.bass as bass
import concourse.mybir as mybir
from concourse.bass_utils import run_bass_kernel_spmd

F32 = mybir.dt.float32
BF16 = mybir.dt.bfloat16
AF = mybir.ActivationFunctionType
ALU = mybir.AluOpType
AX = mybir.AxisListType

S = 4096
D = 1024
NT = S // 128
DEPTH = 2
NE = 32
HID = 512
ALPHA = (2 * DEPTH) ** 0.25
LN_EPS = 1e-5
RMS_EPS = 1e-6
GW = 383
SL = 256
NSLOT = 63
NPOS = NSLOT * SL
KMAX = (S + SL - 1) // SL
I32 = mybir.dt.int32
SPARSE = True


class Sched:
    LIMIT = 30000
    NDMA = 24

    def __init__(self, nc):
        self.nc = nc
        self.eng = {"pe": nc.tensor, "act": nc.scalar, "dve": nc.vector, "pool": nc.gpsimd, "sp": nc.sync}
        self.esem = {}
        self.ecnt = {}
        self.nsem = 0
        for e in self.eng:
            self._newsem(e)
        self.seen = {e: {} for e in self.eng}
        self.lastw = {}
        self.readers = {}
        self.dpools = {}

    def _newsem(self, e):
        self.esem[e] = self.nc.alloc_semaphore(f"es_{e}_{self.nsem}")
        self.nsem += 1
        self.ecnt[e] = 0

    def _wait(self, e, dep):
        sem, val, weng = dep
        if weng == e and e == "pe":
            return
        k = sem.num
        if self.seen[e].get(k, 0) >= val:
            return
        self.eng[e].wait_ge(sem, val)
        self.seen[e][k] = val

    def _deps(self, e, R, W):
        for r in R:
            w = self.lastw.get(r)
            if w:
                for tok in w.values():
                    self._wait(e, tok)
        for w_ in W:
            lw = self.lastw.get(w_)
            if lw:
                for tok in lw.values():
                    self._wait(e, tok)
            rd = self.readers.get(w_)
            if rd:
                for tok in rd.values():
                    self._wait(e, tok)

    def _book(self, tok, R, W):
        for r in R:
            self.readers.setdefault(r, {})[tok[0].num] = tok
        for w_ in W:
            self.lastw.setdefault(w_, {})[tok[0].num] = tok
            self.readers[w_] = {}

    def op(self, e, fn, R=(), W=()):
        self._deps(e, R, W)
        if self.ecnt[e] >= self.LIMIT:
            self._newsem(e)
        inst = fn(self.eng[e])
        self.ecnt[e] += 1
        inst.then_inc(self.esem[e], 1)
        self._book((self.esem[e], self.ecnt[e], e), R, W)

    def dma(self, q, fn, R=(), W=(), pool="misc", n=6):
        pool = f"{pool}_{q}"
        if pool not in self.dpools:
            self.dpools[pool] = dict(sems=[self.nc.alloc_semaphore(f"dq_{pool}_{i}") for i in range(n)], val=[0] * n, nxt=0)
        P = self.dpools[pool]
        i = P["nxt"]
        P["nxt"] = (i + 1) % len(P["sems"])
        sem = P["sems"][i]
        if P["val"][i] > 0:
            self._wait(q, (sem, P["val"][i], "dma"))
        self._deps(q, R, W)
        inst = fn(self.eng[q])
        P["val"][i] += 16
        inst.then_inc(sem, 16)
        self._book((sem, P["val"][i], "dma"), R, W)

    def _dma_toks(self):
        toks = []
        for P in self.dpools.values():
            for sem, v in zip(P["sems"], P["val"]):
                if v > 0:
                    toks.append((sem, v, "dma"))
        return toks

    def barrier(self):
        toks = [(self.esem[e], self.ecnt[e], "x") for e in self.eng if self.ecnt[e] > 0]
        toks += self._dma_toks()
        for e in self.eng:
            for t in toks:
                self._wait(e, t)
        self.lastw = {}
        self.readers = {}

    def final_wait(self, e="sp"):
        for t in self._dma_toks():
            self._wait(e, t)
        for o in self.eng:
            if o != e and self.ecnt[o] > 0:
                self._wait(e, (self.esem[o], self.ecnt[o], o))


def _consts_host():
    ident = np.eye(128, dtype=np.float32)
    kk = np.arange(128)[:, None]
    qq = np.arange(128)[None, :]
    tri_incl = (kk >= np.arange(128)[None, :]).astype(np.float32)
    tri_low = (kk < np.arange(128)[None, :]).astype(np.float32)
    mask_le = (kk <= qq).astype(np.float32)
    mask_lt = (kk < qq).astype(np.float32)
    n = np.maximum(np.arange(GW) - 127, 0)
    nf = np.maximum(n, 1).astype(np.float32)
    large = 16 + (np.log(nf / np.float32(16)) / np.float32(math.log(128 / 16)) * np.float32(16)).astype(np.int32)
    large = np.minimum(large, 31)
    bucket = np.where(n < 16, n, large)
    oh = (bucket[None, :] == np.arange(32)[:, None]).astype(np.float32)
    bf = ml_dtypes.bfloat16
    return {
        "c_ident": ident.astype(bf), "c_tri_incl": tri_incl.astype(bf), "c_tri_low": tri_low.astype(bf),
        "c_mask_le": mask_le, "c_mask_lt": mask_lt.astype(bf), "c_oh": oh,
        "c_pidx": np.arange(128, dtype=np.float32).reshape(128, 1),
        "c_sidx": np.tile(np.arange(NSLOT, dtype=np.float32)[None, :], (128, 1)),
    }


def build_program(stop_after=None, debug=False):
    nc = bass.Bass("TRN2", target_bir_lowering=False)
    sc = Sched(nc)

    def din(name, shape, dt=F32):
        return nc.dram_tensor(name, list(shape), dt, kind="ExternalInput").ap()

    x_in = din("x", [S, D])
    rel_table = din("rel_table", [32, 8])
    attw = [
        dict(wq=din("da_wq", [D, D]), wk=din("da_wk", [D, D]), wv=din("da_wv", [D, D]), wo=din("da_wo", [D, D])),
        dict(wq=din("sb_wq", [D, D]), wk=din("sb_wk", [D, D]), wv=din("sb_wv", [D, D]), wo=din("sb_wo", [D, D])),
    ]
    da_l = din("da_l", [4, 64])
    da_g = din("da_subln_g", [1, 128])
    ln_mix_g = din("ln_mix_g", [DEPTH, D]); ln_mix_b = din("ln_mix_b", [DEPTH, D])
    ln_ffn_g = din("ln_ffn_g", [DEPTH, D]); ln_ffn_b = din("ln_ffn_b", [DEPTH, D])
    w_group = din("moe_w_group", [DEPTH, D, 4]); b_group = din("moe_b_group", [DEPTH, 4])
    w_expert = din("moe_w_expert", [DEPTH, D, NE]); b_expert = din("moe_b_expert", [DEPTH, NE])
    w1 = din("moe_w1", [DEPTH, NE, D, HID]); w3 = din("moe_w3", [DEPTH, NE, D, HID]); w2 = din("moe_w2", [DEPTH, NE, HID, D])
    c_ident = din("c_ident", [128, 128], BF16); c_tri_incl = din("c_tri_incl", [128, 128], BF16)
    c_tri_low = din("c_tri_low", [128, 128], BF16); c_mask_le = din("c_mask_le", [128, 128])
    c_mask_lt = din("c_mask_lt", [128, 128], BF16); c_oh = din("c_oh", [32, GW])
    c_pidx = din("c_pidx", [128, 1]); c_sidx = din("c_sidx", [128, NSLOT])
    Xs = nc.dram_tensor("Xs", [NPOS, D], BF16, kind="Internal").ap()
    Ys = nc.dram_tensor("Ys", [NPOS, D], F32, kind="Internal").ap()
    Wc = [nc.dram_tensor(f"Wc{i}", [NE * 128, 4096], BF16, kind="Internal").ap() for i in range(3)]

    out = nc.dram_tensor("out", [S, D], F32, kind="ExternalOutput").ap()
    skind = "ExternalOutput" if debug else "Internal"
    xr = [x_in] + [nc.dram_tensor(f"xr{i}", [S, D], F32, kind=skind).ap() for i in (1, 2, 3)] + [out]
    gd = nc.dram_tensor("gd", [8, 128, GW], F32, kind="Internal")

    PP = [nc.alloc_psum_tensor(f"pp{i}", [128, 1024], F32) for i in range(4)]

    _uid = [0]

    def sbt(name, shape, dt):
        _uid[0] += 1
        return nc.sbuf_tensor(f"{name}_u{_uid[0]}", shape, dt)

    def bk(i):
        return PP[i // 2].ap()[:, (i % 2) * 512:(i % 2) * 512 + 512]

    def bkh(i):
        return PP[i // 2].bitcast(BF16).ap()[:, (i % 2) * 1024:(i % 2) * 1024 + 1024]

    xT = nc.alloc_sbuf_tensor("xT", [128, 8, S], BF16)
    ident = nc.alloc_sbuf_tensor("ident", [128, 128], BF16)
    tri_incl = nc.alloc_sbuf_tensor("tri_incl", [128, 128], BF16)
    tri_low = nc.alloc_sbuf_tensor("tri_low", [128, 128], BF16)
    mask_le = nc.alloc_sbuf_tensor("mask_le", [128, 128], F32)
    mask_lt = nc.alloc_sbuf_tensor("mask_lt", [128, 128], BF16)
    cst = nc.alloc_sbuf_tensor("cst", [128, 8], F32)
    sc.dma("sp", lambda e: e.dma_start(out=ident.ap(), in_=c_ident), W=["ident"])
    sc.dma("sp", lambda e: e.dma_start(out=tri_incl.ap(), in_=c_tri_incl), W=["tri_incl"])
    sc.dma("sp", lambda e: e.dma_start(out=tri_low.ap(), in_=c_tri_low), W=["tri_low"])
    sc.dma("sp", lambda e: e.dma_start(out=mask_le.ap(), in_=c_mask_le), W=["mask_le"])
    sc.dma("sp", lambda e: e.dma_start(out=mask_lt.ap(), in_=c_mask_lt), W=["mask_lt"])
    sc.op("dve", lambda e: e.memset(cst.ap()[:, 0:1], LN_EPS), W=["cst"])
    sc.op("dve", lambda e: e.memset(cst.ap()[:, 1:2], RMS_EPS), W=["cst"])
    sc.op("dve", lambda e: e.memset(cst.ap()[:, 2:3], 1.0), W=["cst"])

    def make_xT_tile(src_bf, src_res, t, bank):
        for c in range(8):
            sc.op("pe", lambda e, c=c: e.transpose(out=bkh(bank)[:, c * 128:(c + 1) * 128], in_=src_bf[:, c * 128:(c + 1) * 128],
                                                    identity=ident.ap()),
                  R=[src_res, "ident"], W=[f"bank{bank}"])
        sc.op("act", lambda e: e.activation(out=xT.ap()[:, :, t * 128:(t + 1) * 128],
                                            in_=bkh(bank)[:, :].rearrange("p (c f) -> p c f", c=8), func=AF.Copy),
              R=[f"bank{bank}"], W=[("xT", t)])

    def ln_tile(y, yres, gb, stat, t, dst, xbf, lnbank):
        sres = ("stat", stat.name)
        st6 = stat.ap()[:, 0:12].rearrange("p (a b) -> p a b", a=2)
        for hh in range(2):
            sc.op("dve", lambda e, hh=hh: e.bn_stats(out=st6[:, hh, :], in_=y[:, hh * 512:(hh + 1) * 512]), R=[yres], W=[sres])
        sc.op("dve", lambda e: e.bn_aggr(out=stat.ap()[:, 12:14], in_=stat.ap()[:, 0:12]), R=[sres], W=[sres])
        sc.op("act", lambda e: e.activation(out=stat.ap()[:, 14:15], in_=stat.ap()[:, 13:14], func=AF.Ln, bias=float(LN_EPS), scale=1.0),
              R=[sres], W=[sres])
        sc.op("act", lambda e: e.activation(out=stat.ap()[:, 15:16], in_=stat.ap()[:, 14:15], func=AF.Exp, scale=-0.5), R=[sres], W=[sres])
        sc.op("dve", lambda e: e.tensor_scalar(out=stat.ap()[:, 16:17], in0=stat.ap()[:, 12:13], scalar1=stat.ap()[:, 15:16], scalar2=-1.0,
                                               op0=ALU.mult, op1=ALU.mult), R=[sres], W=[sres])
        sc.op("act", lambda e: e.activation(out=y, in_=y, func=AF.Identity, bias=stat.ap()[:, 16:17], scale=stat.ap()[:, 15:16]), R=[yres, sres], W=[yres])
        sc.op("dve", lambda e: e.tensor_tensor(out=y, in0=y, in1=gb.ap()[:, 0, :], op=ALU.mult), R=[yres, "gb0"], W=[yres])
        sc.op("dve", lambda e: e.tensor_tensor(out=y, in0=y, in1=gb.ap()[:, 1, :], op=ALU.add), R=[yres, "gb1"], W=[yres])
        sc.dma("sp", lambda e: e.dma_start(out=dst[t * 128:(t + 1) * 128, :], in_=y), R=[yres], W=[], pool="st", n=4)
        if xbf is not None:
            xb, xbres = xbf
            sc.op("act", lambda e: e.activation(out=xb, in_=y, func=AF.Copy), R=[yres], W=[xbres])
            make_xT_tile(xb, xbres, t, lnbank)

    def ln_s1(y, yres, stat):
        sres = ("stat", stat.name)
        st6 = stat.ap()[:, 0:12].rearrange("p (a b) -> p a b", a=2)
        for hh in range(2):
            sc.op("dve", lambda e, hh=hh: e.bn_stats(out=st6[:, hh, :], in_=y[:, hh * 512:(hh + 1) * 512]), R=[yres], W=[sres])
        sc.op("dve", lambda e: e.bn_aggr(out=stat.ap()[:, 12:14], in_=stat.ap()[:, 0:12]), R=[sres], W=[sres])

    def ln_s2(y, yres, stat):
        sres = ("stat", stat.name)
        sc.op("act", lambda e: e.activation(out=stat.ap()[:, 14:15], in_=stat.ap()[:, 13:14], func=AF.Ln, bias=float(LN_EPS), scale=1.0),
              R=[sres], W=[sres])
        sc.op("act", lambda e: e.activation(out=stat.ap()[:, 15:16], in_=stat.ap()[:, 14:15], func=AF.Exp, scale=-0.5), R=[sres], W=[sres])
        sc.op("dve", lambda e: e.tensor_scalar(out=stat.ap()[:, 16:17], in0=stat.ap()[:, 12:13], scalar1=stat.ap()[:, 15:16], scalar2=-1.0,
                                               op0=ALU.mult, op1=ALU.mult), R=[sres], W=[sres])
        sc.op("act", lambda e: e.activation(out=y, in_=y, func=AF.Identity, bias=stat.ap()[:, 16:17], scale=stat.ap()[:, 15:16]), R=[yres, sres], W=[yres])

    def ln_s3(y, yres, gb, t, dst, xbf):
        sc.op("dve", lambda e: e.tensor_tensor(out=y, in0=y, in1=gb.ap()[:, 0, :], op=ALU.mult), R=[yres, "gb0"], W=[yres])
        sc.op("dve", lambda e: e.tensor_tensor(out=y, in0=y, in1=gb.ap()[:, 1, :], op=ALU.add), R=[yres, "gb1"], W=[yres])
        sc.dma("sp", lambda e: e.dma_start(out=dst[t * 128:(t + 1) * 128, :], in_=y), R=[yres], W=[], pool="st", n=4)

    def ln_s3b(y, yres, xbf):
        xb, xbres = xbf
        sc.op("act", lambda e: e.activation(out=xb, in_=y, func=AF.Copy), R=[yres], W=[xbres])

    def ln_pipeline(pre, produce_a, produce, yb, ybres, stat, gb, dst, xbf):
        NB = len(yb)
        for i in range(-5, NT + 3):
            if pre is not None and 0 <= i + 5 < NT:
                pre(i + 5)
            if produce_a is not None and 0 <= i + 3 < NT:
                produce_a(i + 3)
            if 0 <= i + 1 < NT:
                t = i + 1
                ln_s2(yb[t % NB].ap(), ybres(t % NB), stat[t % NB])
            if xbf is not None and 0 <= i - 1 < NT:
                t = i - 1
                ln_s3b(yb[t % NB].ap(), ybres(t % NB), (xbf[t % NB].ap(), f"xbf{t % NB}"))
            if xbf is not None and 0 <= i - 2 < NT:
                t = i - 2
                make_xT_tile(xbf[t % NB].ap(), f"xbf{t % NB}", t, 6 + (t % 2))
            if 0 <= i + 2 < NT:
                t = i + 2
                produce(t)
                ln_s1(yb[t % NB].ap(), ybres(t % NB), stat[t % NB])
            if 0 <= i < NT:
                t = i
                ln_s3(yb[t % NB].ap(), ybres(t % NB), gb, t, dst, None)

    def load_gb(gb, g_ap, b_ap, layer):
        sc.dma("sp", lambda e: e.dma_start(out=gb.ap()[:, 0, :], in_=g_ap[layer:layer + 1, :].partition_broadcast(128)), W=["gb0"])
        sc.dma("sp", lambda e: e.dma_start(out=gb.ap()[:, 1, :], in_=b_ap[layer:layer + 1, :].partition_broadcast(128)), W=["gb1"])

    with ExitStack() as es:
        xld = [es.enter_context(sbt(f"xld{i}", [128, D], BF16)) for i in range(2)]
        for t in range(NT):
            b = t % 2
            sc.dma("pool", lambda e, b=b, t=t: e.dma_start(out=xld[b].ap(), in_=x_in[t * 128:(t + 1) * 128, :]), W=[f"xld{b}"])
            make_xT_tile(xld[b].ap(), f"xld{b}", t, 6 + b)
        sc.barrier()

    def attention_layer(layer):
        kind = "diff" if layer % 2 == 0 else "sb"
        W_ = attw[layer % 2]
        VW = 130 if kind == "diff" else 128
        with ExitStack() as es:
            oT = es.enter_context(sbt("oT", [128, 8, S], BF16))
            with ExitStack() as es2:
                ent = es2.enter_context
                qT = ent(sbt("qT", [128, 2, S], BF16))
                kT = ent(sbt("kT", [128, S], BF16))
                V = ent(sbt("V", [128, NT, VW], BF16))
                wqkv = [ent(sbt(f"wqkv{i}", [128, 3, 8, 128], BF16)) for i in range(2)]
                NB = 4
                if kind == "diff":
                    Pt = [ent(sbt(f"Pt{i}", [128, 512], BF16)) for i in range(NB)]
                    Ep = ent(sbt("Ep", [128, 8, 2, 128], F32))
                    gtmp = ent(sbt("gtmp", [128, GW], F32))
                    rb = ent(sbt("rb", [32, 128], F32))
                    rtab = ent(sbt("rtab", [32, 8], F32))
                    oh = ent(sbt("oh", [32, GW], F32))
                    ones32 = ent(sbt("ones32", [32, 128], F32))
                    cfar = ent(sbt("cfar", [128, 16], F32))
                    lam = ent(sbt("lam", [128, 8], F32))
                    lvec = ent(sbt("lvec", [128, 4, 64], F32))
                    gsub = ent(sbt("gsub", [128, 128], F32))
                    R1 = ent(sbt("R1", [128, 4, 128], F32))
                    ot = [ent(sbt(f"ot{i}", [128, 128], F32)) for i in range(4)]
                    sm = [ent(sbt(f"sm{i}", [128, 8], F32)) for i in range(4)]
                    junk = ent(sbt("junk", [128, 128], F32))
                else:
                    eb = [ent(sbt(f"eb{i}", [128, 512], BF16)) for i in range(1)]
                    spb = [ent(sbt(f"spb{i}", [128, 512], BF16)) for i in range(1)]
                    gb_ = [ent(sbt(f"gb_{i}", [128, 512], BF16)) for i in range(1)]
                    wb = [ent(sbt(f"wb{i}", [128, 512], BF16)) for i in range(1)]
                    eb2 = [ent(sbt(f"eb2_{i}", [128, 2, 512], BF16)) for i in range(3)]
                    sp2 = [ent(sbt(f"sp2_{i}", [128, 2, 512], BF16)) for i in range(3)]
                    g2b = [ent(sbt(f"g2b_{i}", [128, 2, 512], BF16)) for i in range(2)]
                    w2b = [ent(sbt(f"w2b_{i}", [128, 2, 512], BF16)) for i in range(2)]
                onb = [ent(sbt(f"onb{i}", [128, 128], BF16)) for i in range(4)]

                sc.op("pool", lambda e: e.memset(qT.ap()[:, 0, :], 0.0), W=["qT"])
                sc.op("dve", lambda e: e.memset(qT.ap()[:, 1, :], 0.0), W=["qT"])
                lam_init = 0.8 - 0.6 * math.exp(-0.3 * layer)
                if kind == "diff":
                    sc.dma("sp", lambda e: e.dma_start(out=lvec.ap(), in_=da_l.partition_broadcast(128)), W=["lvec"])
                    sc.dma("sp", lambda e: e.dma_start(out=gsub.ap(), in_=da_g[0:1, :].partition_broadcast(128)), W=["gsub"])
                    sc.op("dve", lambda e: e.tensor_scalar(out=gsub.ap(), in0=gsub.ap(), scalar1=float(1.0 - lam_init), scalar2=None, op0=ALU.mult),
                          R=["gsub"], W=["gsub"])
                    sc.op("dve", lambda e: e.tensor_tensor(out=lvec.ap()[:, 0, :], in0=lvec.ap()[:, 0, :], in1=lvec.ap()[:, 1, :], op=ALU.mult), R=["lvec"], W=["lvec"])
                    sc.op("dve", lambda e: e.tensor_tensor(out=lvec.ap()[:, 2, :], in0=lvec.ap()[:, 2, :], in1=lvec.ap()[:, 3, :], op=ALU.mult), R=["lvec"], W=["lvec"])
                    sc.op("dve", lambda e: e.tensor_reduce(out=lam.ap()[:, 0:1], in_=lvec.ap()[:, 0, :], axis=AX.X, op=ALU.add), R=["lvec"], W=["lam"])
                    sc.op("dve", lambda e: e.tensor_reduce(out=lam.ap()[:, 1:2], in_=lvec.ap()[:, 2, :], axis=AX.X, op=ALU.add), R=["lvec"], W=["lam"])
                    sc.op("act", lambda e: e.activation(out=lam.ap()[:, 2:4], in_=lam.ap()[:, 0:2], func=AF.Exp), R=["lam"], W=["lam"])
                    sc.op("dve", lambda e: e.scalar_tensor_tensor(out=lam.ap()[:, 4:5], in0=lam.ap()[:, 3:4], scalar=float(-lam_init), in1=lam.ap()[:, 2:3],
                                                                  op0=ALU.add, op1=ALU.subtract), R=["lam"], W=["lam"])
                    sc.dma("sp", lambda e: e.dma_start(out=rtab.ap(), in_=rel_table), W=["rtab"])
                    sc.dma("sp", lambda e: e.dma_start(out=oh.ap(), in_=c_oh), W=["oh"])
                    sc.dma("sp", lambda e: e.dma_start(out=cfar.ap()[:, 0:8], in_=rel_table[31:32, :].partition_broadcast(128)), W=["cfar"])
                    sc.op("dve", lambda e: e.tensor_scalar(out=cfar.ap()[:, 8:16], in0=cfar.ap()[:, 0:8], scalar1=-1.0, scalar2=None, op0=ALU.mult), R=["cfar"], W=["cfar"])
                    sc.op("dve", lambda e: e.memset(ones32.ap(), 1.0), W=["ones32"])
                    for h in range(8):
                        sc.op("dve", lambda e, h=h: e.tensor_scalar(out=rb.ap(), in0=ones32.ap(), scalar1=rtab.ap()[:, h:h + 1], scalar2=None, op0=ALU.mult),
                              R=["ones32", "rtab"], W=["rb"])
                        sc.op("pe", lambda e: e.matmul(bk(6)[:, 0:GW], lhsT=rb.ap(), rhs=oh.ap(), start=True, stop=True), R=["rb", "oh"], W=["bank6"])
                        sc.op("act", lambda e: e.activation(out=gtmp.ap(), in_=bk(6)[:, 0:GW], func=AF.Copy), R=["bank6"], W=["gtmp"])
                        sc.dma("sp", lambda e, h=h: e.dma_start(out=gd.ap()[h], in_=gtmp.ap()), R=["gtmp"], W=["gd"])
                        for Dd in range(2):
                            src = bass.AP(tensor=gd.ap().tensor, offset=h * 128 * GW + Dd * 128 + 127, ap=[[GW - 1, 128], [1, 128]])
                            sc.dma("sp", lambda e, h=h, Dd=Dd, src=src: e.dma_start(out=Ep.ap()[:, h, Dd, :], in_=src), R=["gd"], W=["Ep"])
                            sc.op("act", lambda e, h=h, Dd=Dd: e.activation(out=Ep.ap()[:, h, Dd, :], in_=Ep.ap()[:, h, Dd, :], func=AF.Exp,
                                                                             bias=cfar.ap()[:, 8 + h:9 + h], scale=1.0), R=["Ep", "cfar"], W=["Ep"])
                        sc.op("dve", lambda e, h=h: e.tensor_tensor(out=Ep.ap()[:, h, 0, :], in0=Ep.ap()[:, h, 0, :], in1=mask_le.ap(), op=ALU.mult),
                              R=["Ep", "mask_le"], W=["Ep"])
                    sc.op("dve", lambda e: e.memset(V.ap()[:, :, 128:130], 1.0), W=["Vones"])

                def load_w(j):
                    b = j % 2
                    for i, nm in enumerate(("wq", "wk", "wv")):
                        src = W_[nm].rearrange("(c p) f -> p c f", p=128)[:, :, j * 128:(j + 1) * 128]
                        sc.dma("pool", lambda e, i=i, src=src, b=b: e.dma_start(out=wqkv[b].ap()[:, i, :, :], in_=src), W=[(f"wqkv{b}", i)], pool="wqkv", n=6)

                def project(j):
                    b = j % 2
                    wres = f"wqkv{b}"
                    for which, dstT, scale in ((0, qT, 0.125), (1, kT, None)):
                        for tc in range(8):
                            pb = 6 + (tc % 2)
                            for c in range(8):
                                sc.op("pe", lambda e, c=c, tc=tc, pb=pb, which=which: e.matmul(
                                    bk(pb)[:, :], lhsT=wqkv[b].ap()[:, which, c, :], rhs=xT.ap()[:, c, tc * 512:(tc + 1) * 512],
                                    start=(c == 0), stop=(c == 7)),
                                    R=[(wres, which)] + [("xT", t) for t in range(4 * tc, 4 * tc + 4)], W=[f"bank{pb}"])
                            if scale is not None:
                                if kind == "sb":
                                    sc.op("dve", lambda e, tc=tc, pb=pb: e.tensor_scalar(out=qT.ap()[0:64, 0, tc * 512:(tc + 1) * 512], in0=bk(pb)[0:64, :], scalar1=float(scale), scalar2=None,
                                                                                      op0=ALU.mult), R=[f"bank{pb}"], W=["qT"])
                                else:
                                    sc.op("act", lambda e, tc=tc, pb=pb: e.activation(out=qT.ap()[0:64, 0, tc * 512:(tc + 1) * 512], in_=bk(pb)[0:64, :], func=AF.Copy, scale=scale),
                                          R=[f"bank{pb}"], W=["qT"])
                                sc.op("dve", lambda e, tc=tc, pb=pb: e.tensor_scalar(out=qT.ap()[64:128, 1, tc * 512:(tc + 1) * 512], in0=bk(pb)[64:128, :], scalar1=float(scale), scalar2=None,
                                                                                  op0=ALU.mult), R=[f"bank{pb}"], W=["qT"])
                            else:
                                sc.op("dve", lambda e, tc=tc, pb=pb: e.tensor_copy(out=dstT.ap()[:, tc * 512:(tc + 1) * 512], in_=bk(pb)[:, :]),
                                      R=[f"bank{pb}"], W=["kT"])
                    for tg in range(8):
                        pb = 6 + (tg % 2)
                        for tt in range(4):
                            t = 4 * tg + tt
                            for c in range(8):
                                sc.op("pe", lambda e, c=c, t=t, tt=tt, pb=pb: e.matmul(
                                    bk(pb)[:, tt * 128:(tt + 1) * 128], lhsT=xT.ap()[:, c, t * 128:(t + 1) * 128], rhs=wqkv[b].ap()[:, 2, c, :],
                                    start=(c == 0), stop=(c == 7), skip_group_check=True),
                                    R=[(wres, 2), ("xT", t)], W=[f"bank{pb}"])
                        v_on_act = (tg % 2 == 0) and kind != "sb"
                        sc.op("act" if v_on_act else "dve",
                              lambda e, tg=tg, pb=pb, v_on_act=v_on_act: (e.activation(out=V.ap()[:, 4 * tg:4 * tg + 4, 0:128], in_=bk(pb)[:, :].rearrange("p (a f) -> p a f", a=4), func=AF.Copy)
                                                       if v_on_act else
                                                       e.tensor_copy(out=V.ap()[:, 4 * tg:4 * tg + 4, 0:128], in_=bk(pb)[:, :].rearrange("p (a f) -> p a f", a=4))),
                              R=[f"bank{pb}"], W=["V"])

                def finish_tile(j, t, src_bf, src_res):
                    sc.op("pe", lambda e: e.transpose(out=bkh(7)[:, 0:128], in_=src_bf, identity=ident.ap()), R=[src_res, "ident"], W=["bank7"])
                    sc.op("dve", lambda e: e.tensor_copy(out=oT.ap()[:, j, t * 128:(t + 1) * 128], in_=bkh(7)[:, 0:128]), R=["bank7"], W=[("oT", t)])

                def attn_diff(j):
                    units = []
                    for qc in range(8):
                        for c in range(2):
                            for kt in range(4 * qc + 4):
                                units.append((qc, c, kt))
                    n = len(units)
                    gidx = {}
                    for (qc, c, kt) in units:
                        gidx.setdefault((qc, c), len(gidx))

                    def stage_A(u):
                        qc, c, kt = units[u]
                        qlo = max(0, kt - 4 * qc)
                        zb = (0, 1, 6)[u % 3]
                        sc.op("pe", lambda e: e.matmul(bk(zb)[:, qlo * 128:512], lhsT=kT.ap()[:, kt * 128:(kt + 1) * 128],
                                                       rhs=qT.ap()[:, c, qc * 512 + qlo * 128:(qc + 1) * 512], start=True, stop=True),
                              R=["qT", "kT"], W=[f"bank{zb}"])

                    def stage_B(u):
                        qc, c, kt = units[u]
                        qlo = max(0, kt - 4 * qc)
                        zb = (0, 1, 6)[u % 3]
                        pb = u % NB
                        sc.op("act", lambda e: e.activation(out=Pt[pb].ap()[:, qlo * 128:512], in_=bk(zb)[:, qlo * 128:512], func=AF.Exp),
                              R=[f"bank{zb}"], W=[f"Pt{pb}"])
                        for Dd in range(2):
                            ql = kt + Dd - 4 * qc
                            if 0 <= ql <= 3:
                                sc.op("dve", lambda e, ql=ql, Dd=Dd: e.tensor_tensor(out=Pt[pb].ap()[:, ql * 128:(ql + 1) * 128], in0=Pt[pb].ap()[:, ql * 128:(ql + 1) * 128],
                                                                                    in1=Ep.ap()[:, j, Dd, :], op=ALU.mult),
                                      R=[f"Pt{pb}", "Ep"], W=[f"Pt{pb}"])

                    def stage_H(u):
                        qc, c, kt = units[u]
                        qlo = max(0, kt - 4 * qc)
                        pb = u % NB
                        g = gidx[(qc, c)]
                        ob = 2 + 2 * (g % 2)
                        for ql in range(qlo, 4):
                            bank = ob + ql // 2
                            col = (ql % 2) * 256
                            sc.op("pe", lambda e, ql=ql, bank=bank, col=col: e.matmul(
                                bk(bank)[:, col:col + 129], lhsT=Pt[pb].ap()[:, ql * 128:(ql + 1) * 128], rhs=V.ap()[:, kt, 0:129],
                                start=(kt == 0 and ql % 2 == 0), stop=(kt == 4 * qc + ql), skip_group_check=True),
                                R=[f"Pt{pb}", "V", "Vones"], W=[f"bank{bank}"])
                        if kt == 4 * qc + 3:
                            for ql in range(4):
                                pending.append((u + 1 + ql, (lambda ql=ql, c=c, qc=qc, ob=ob: evac_chain(ql, c, qc, ob))))

                    def evac_chain(ql, c, qc, ob):
                        bank = ob + ql // 2
                        col = (ql % 2) * 256
                        s_ = sm[ql]
                        sres = f"sm{ql}"
                        sc.op("dve", lambda e: e.reciprocal(out=s_.ap()[:, 0:1], in_=bk(bank)[:, col + 128:col + 129]), R=[f"bank{bank}"], W=[sres])
                        if c == 0:
                            sc.op("dve", lambda e: e.tensor_scalar(out=R1.ap()[:, ql, :], in0=bk(bank)[:, col:col + 128], scalar1=s_.ap()[:, 0:1], scalar2=None, op0=ALU.mult),
                                  R=[f"bank{bank}", sres], W=[("R1", ql)])
                            return
                        o_ = ot[ql]
                        ores = f"ot{ql}"
                        nb_ = onb[ql]
                        nres = f"onb{ql}"
                        sc.op("dve", lambda e: e.tensor_tensor(out=s_.ap()[:, 1:2], in0=s_.ap()[:, 0:1], in1=lam.ap()[:, 4:5], op=ALU.mult), R=[sres, "lam"], W=[sres])
                        sc.op("dve", lambda e: e.scalar_tensor_tensor(out=o_.ap(), in0=bk(bank)[:, col:col + 128], scalar=s_.ap()[:, 1:2], in1=R1.ap()[:, ql, :],
                                                                      op0=ALU.mult, op1=ALU.add), R=[f"bank{bank}", sres, ("R1", ql)], W=[ores])
                        sc.op("act", lambda e: e.activation(out=junk.ap(), in_=o_.ap(), func=AF.Square, accum_out=s_.ap()[:, 2:3]), R=[ores], W=[sres, "junk"])
                        sc.op("act", lambda e: e.activation(out=s_.ap()[:, 3:4], in_=s_.ap()[:, 2:3], func=AF.Ln, bias=float(RMS_EPS), scale=1.0 / 128.0),
                              R=[sres, "cst"], W=[sres])
                        sc.op("act", lambda e: e.activation(out=s_.ap()[:, 4:5], in_=s_.ap()[:, 3:4], func=AF.Exp, scale=-0.5), R=[sres], W=[sres])
                        def part2():
                            sc.op("dve", lambda e: e.scalar_tensor_tensor(out=nb_.ap(), in0=o_.ap(), scalar=s_.ap()[:, 4:5], in1=gsub.ap(), op0=ALU.mult, op1=ALU.mult),
                                  R=[ores, sres, "gsub"], W=[nres])
                            pending.append((cur[0] + 2, (lambda: finish_tile(j, 4 * qc + ql, nb_.ap(), nres))))
                        pending.append((cur[0] + 2, part2))

                    pending = []
                    cur = [0]

                    def run_pending(i, flush=False):
                        k = 0
                        while k < len(pending):
                            if flush or pending[k][0] <= i:
                                fn = pending.pop(k)[1]
                                fn()
                            else:
                                k += 1

                    for i in range(-3, n):
                        cur[0] = i
                        if 0 <= i + 3 < n:
                            stage_A(i + 3)
                        if 0 <= i + 2 < n:
                            stage_B(i + 2)
                        if 0 <= i < n:
                            stage_H(i)
                        run_pending(i)
                    cur[0] = n
                    while pending:
                        run_pending(n, flush=True)

                def attn_sb2(j):
                    pairs = [(qc, kt) for qc in range(8) for kt in range(4 * qc + 3, -1, -1)]
                    n = len(pairs)

                    def geo(p):
                        qc, kt = pairs[p]
                        return qc, kt, max(0, kt - 4 * qc)

                    def zb(p, c):
                        return (0, 1)[c] if p % 2 == 0 else (6, 7)[c]

                    def pair_ps(ti, lo):
                        return PP[ti].ap().rearrange("p (b f) -> p b f", b=2)[:, :, lo:512]

                    def st_A(p):
                        qc, kt, qlo = geo(p)
                        lo = qlo * 128
                        for c in range(2):
                            z = zb(p, c)
                            sc.op("pe", lambda e, c=c, z=z: e.matmul(bk(z)[:, lo:512], lhsT=kT.ap()[:, kt * 128:(kt + 1) * 128],
                                                                     rhs=qT.ap()[:, c, qc * 512 + lo:(qc + 1) * 512], start=True, stop=True),
                                  R=["qT", "kT"], W=[f"bank{z}"])

                    def st_B(p):
                        qc, kt, qlo = geo(p)
                        lo = qlo * 128
                        b = p % 3
                        sc.op("act", lambda e: e.activation(out=eb2[b].ap()[:, :, lo:512], in_=pair_ps(0 if p % 2 == 0 else 3, lo), func=AF.Exp),
                              R=[f"bank{zb(p, 0)}", f"bank{zb(p, 1)}"], W=[f"eb{b}"])
                        if kt >= 4 * qc:
                            sc.op("dve", lambda e: e.tensor_tensor(out=eb2[b].ap()[:, :, lo:lo + 128], in0=eb2[b].ap()[:, :, lo:lo + 128],
                                                                  in1=mask_lt.ap().unsqueeze(1).to_broadcast([128, 2, 128]), op=ALU.mult),
                                  R=[f"eb{b}", "mask_lt"], W=[f"eb{b}"])

                    def st_C(p):
                        qc, kt, qlo = geo(p)
                        lo = qlo * 128
                        b = p % 3
                        sc.op("act", lambda e: e.activation(out=sp2[b].ap()[:, :, lo:512], in_=eb2[b].ap()[:, :, lo:512], func=AF.Ln, bias=1.0, scale=1.0),
                              R=[f"eb{b}"], W=[f"spb{b}"])

                    def st_D(p):
                        qc, kt, qlo = geo(p)
                        lo = qlo * 128
                        b = p % 3
                        for c in range(2):
                            sc.op("pe", lambda e, c=c: e.matmul(bk(2 + c)[:, lo:512], lhsT=tri_incl.ap(), rhs=sp2[b].ap()[:, c, lo:512],
                                                                start=(kt == 4 * qc + 3), stop=False, skip_group_check=True),
                                  R=[f"spb{b}", "tri_incl"], W=[f"bank{2 + c}"])

                    def st_E(p):
                        qc, kt, qlo = geo(p)
                        lo = qlo * 128
                        b2 = p % 2
                        sc.op("act", lambda e: e.activation(out=g2b[b2].ap()[:, :, lo:512], in_=pair_ps(1, lo), func=AF.Exp, scale=-1.0),
                              R=["bank2", "bank3"], W=[f"gb_{b2}"])

                    def st_F(p):
                        qc, kt, qlo = geo(p)
                        lo = qlo * 128
                        b = p % 3
                        if kt == 0:
                            return
                        for c in range(2):
                            sc.op("pe", lambda e, c=c: e.matmul(bk(2 + c)[:, lo:512], lhsT=tri_low.ap(), rhs=sp2[b].ap()[:, c, lo:512],
                                                                start=False, stop=False, skip_group_check=True),
                                  R=[f"spb{b}", "tri_low"], W=[f"bank{2 + c}"])

                    def st_G(p):
                        qc, kt, qlo = geo(p)
                        lo = qlo * 128
                        b = p % 3
                        b2 = p % 2
                        sc.op("dve", lambda e: e.tensor_tensor(out=w2b[b2].ap()[:, :, lo:512], in0=eb2[b].ap()[:, :, lo:512], in1=g2b[b2].ap()[:, :, lo:512], op=ALU.mult),
                              R=[f"eb{b}", f"gb_{b2}"], W=[f"wb{b2}"])

                    def st_H(p):
                        qc, kt, qlo = geo(p)
                        b2 = p % 2
                        ob = 4 + (qc % 2)
                        for c in range(2):
                            for ql in range(qlo, 4):
                                col = (c * 4 + ql) * 64
                                sc.op("pe", lambda e, c=c, ql=ql, col=col: e.matmul(
                                    bk(ob)[:, col:col + 64], lhsT=w2b[b2].ap()[:, c, ql * 128:(ql + 1) * 128], rhs=V.ap()[:, kt, c * 64:(c + 1) * 64],
                                    start=(kt == 4 * qc + 3 and c == 0), stop=(kt == 0), skip_group_check=True),
                                    R=[f"wb{b2}", "V"], W=[f"bank{ob}"])
                        if kt == 0:
                            for ql in range(4):
                                pending.append((cur[0] + 1 + ql, (lambda ql=ql, qc=qc, ob=ob: evac_sb(ql, qc, ob))))

                    def evac_sb(ql, qc, ob):
                        nb_ = onb[ql]
                        nres = f"onb{ql}"
                        src = bass.AP(tensor=bk(ob).tensor, offset=bk(ob).offset + ql * 64, ap=[list(bk(ob).ap[0]), [256, 2], [1, 64]])
                        sc.op("dve", lambda e: e.tensor_copy(out=nb_.ap().rearrange("p (a f) -> p a f", a=2), in_=src), R=[f"bank{ob}"], W=[nres])
                        pending.append((cur[0] + 2, (lambda: finish_tile(j, 4 * qc + ql, nb_.ap(), nres))))

                    pending = []
                    cur = [0]

                    def run_pending(i, flush=False):
                        k = 0
                        while k < len(pending):
                            if flush or pending[k][0] <= i:
                                fn = pending.pop(k)[1]
                                fn()
                            else:
                                k += 1

                    for i in range(-2, n + 1):
                        cur[0] = i
                        if 0 <= i + 2 < n:
                            st_A(i + 2)
                        if 0 <= i + 1 < n:
                            st_B(i + 1)
                        if 0 <= i - 1 < n:
                            st_F(i - 1)
                        if 0 <= i < n:
                            st_D(i)
                            st_E(i)
                        if 0 <= i + 1 < n:
                            st_C(i + 1)
                        if 0 <= i < n:
                            st_G(i)
                        if 0 <= i - 1 < n:
                            st_H(i - 1)
                        run_pending(i)
                    cur[0] = n + 1
                    while pending:
                        run_pending(n + 1, flush=True)

                def attn_sb(j):
                    units = []
                    for qc in range(8):
                        for kt in range(4 * qc + 3, -1, -1):
                            for c in range(2):
                                units.append((qc, c, kt))
                    n = len(units)

                    def geo(u):
                        qc, c, kt = units[u]
                        return qc, c, kt, max(0, kt - 4 * qc)

                    def stage_A(u):
                        qc, c, kt, qlo = geo(u)
                        zb = (0, 1, 6)[u % 3]
                        sc.op("pe", lambda e: e.matmul(bk(zb)[:, qlo * 128:512], lhsT=kT.ap()[:, kt * 128:(kt + 1) * 128],
                                                       rhs=qT.ap()[:, c, qc * 512 + qlo * 128:(qc + 1) * 512], start=True, stop=True),
                              R=["qT", "kT"], W=[f"bank{zb}"])

                    def stage_B(u):
                        qc, c, kt, qlo = geo(u)
                        zb = (0, 1, 6)[u % 3]
                        b = u % NB
                        sc.op("act", lambda e: e.activation(out=eb[b].ap()[:, qlo * 128:512], in_=bk(zb)[:, qlo * 128:512], func=AF.Exp),
                              R=[f"bank{zb}"], W=[f"eb{b}"])
                        if kt >= 4 * qc:
                            sc.op("dve", lambda e: e.tensor_tensor(out=eb[b].ap()[:, qlo * 128:(qlo + 1) * 128], in0=eb[b].ap()[:, qlo * 128:(qlo + 1) * 128],
                                                                  in1=mask_lt.ap(), op=ALU.mult), R=[f"eb{b}", "mask_lt"], W=[f"eb{b}"])

                    def stage_C(u):
                        qc, c, kt, qlo = geo(u)
                        b = u % NB
                        sc.op("act", lambda e: e.activation(out=spb[b].ap()[:, qlo * 128:512], in_=eb[b].ap()[:, qlo * 128:512], func=AF.Ln, bias=1.0, scale=1.0),
                              R=[f"eb{b}"], W=[f"spb{b}"])

                    def stage_D(u):
                        qc, c, kt, qlo = geo(u)
                        b = u % NB
                        cb = 2 + c
                        sc.op("pe", lambda e: e.matmul(bk(cb)[:, qlo * 128:512], lhsT=tri_incl.ap(), rhs=spb[b].ap()[:, qlo * 128:512],
                                                       start=(kt == 4 * qc + 3), stop=False, skip_group_check=True),
                              R=[f"spb{b}", "tri_incl"], W=[f"bank{cb}"])

                    def stage_E(u):
                        qc, c, kt, qlo = geo(u)
                        b = u % NB
                        cb = 2 + c
                        sc.op("act", lambda e: e.activation(out=gb_[b].ap()[:, qlo * 128:512], in_=bk(cb)[:, qlo * 128:512], func=AF.Exp, scale=-1.0),
                              R=[f"bank{cb}"], W=[f"gb_{b}"])

                    def stage_F(u):
                        qc, c, kt, qlo = geo(u)
                        b = u % NB
                        cb = 2 + c
                        if kt == 0:
                            return
                        sc.op("pe", lambda e: e.matmul(bk(cb)[:, qlo * 128:512], lhsT=tri_low.ap(), rhs=spb[b].ap()[:, qlo * 128:512],
                                                       start=False, stop=False, skip_group_check=True),
                              R=[f"spb{b}", "tri_low"], W=[f"bank{cb}"])

                    def stage_G(u):
                        qc, c, kt, qlo = geo(u)
                        b = u % NB
                        sc.op("dve", lambda e: e.tensor_tensor(out=wb[b].ap()[:, qlo * 128:512], in0=eb[b].ap()[:, qlo * 128:512], in1=gb_[b].ap()[:, qlo * 128:512], op=ALU.mult),
                              R=[f"eb{b}", f"gb_{b}"], W=[f"wb{b}"])

                    def stage_H(u):
                        qc, c, kt, qlo = geo(u)
                        b = u % NB
                        ob = 4 + (qc % 2)
                        for ql in range(qlo, 4):
                            col = (c * 4 + ql) * 64
                            sc.op("pe", lambda e, ql=ql, col=col: e.matmul(
                                bk(ob)[:, col:col + 64], lhsT=wb[b].ap()[:, ql * 128:(ql + 1) * 128], rhs=V.ap()[:, kt, c * 64:(c + 1) * 64],
                                start=(kt == 4 * qc + 3 and c == 0), stop=(kt == 0), skip_group_check=True),
                                R=[f"wb{b}", "V"], W=[f"bank{ob}"])
                        if kt == 0 and c == 1:
                            for ql in range(4):
                                pending.append((cur[0] + 1 + ql, (lambda ql=ql, qc=qc, ob=ob: evac_sb(ql, qc, ob))))

                    def evac_sb(ql, qc, ob):
                        nb_ = onb[ql]
                        nres = f"onb{ql}"
                        src = bass.AP(tensor=bk(ob).tensor, offset=bk(ob).offset + ql * 64, ap=[list(bk(ob).ap[0]), [256, 2], [1, 64]])
                        sc.op("dve", lambda e: e.tensor_copy(out=nb_.ap().rearrange("p (a f) -> p a f", a=2), in_=src), R=[f"bank{ob}"], W=[nres])
                        pending.append((cur[0] + 2, (lambda: finish_tile(j, 4 * qc + ql, nb_.ap(), nres))))

                    pending = []
                    cur = [0]

                    def run_pending(i, flush=False):
                        k = 0
                        while k < len(pending):
                            if flush or pending[k][0] <= i:
                                fn = pending.pop(k)[1]
                                fn()
                            else:
                                k += 1

                    for i in range(-4, n + 1):
                        cur[0] = i
                        if 0 <= i + 3 < n:
                            stage_A(i + 3)
                        if 0 <= i + 2 < n:
                            stage_B(i + 2)
                        if 0 <= i + 1 < n:
                            stage_D(i + 1)
                            stage_E(i + 1)
                        if 0 <= i + 2 < n:
                            stage_C(i + 2)
                        if 0 <= i < n:
                            stage_F(i)
                            stage_G(i)
                        if 0 <= i - 1 < n:
                            stage_H(i - 1)
                        run_pending(i)
                    cur[0] = n + 1
                    while pending:
                        run_pending(n + 1, flush=True)

                def convert_experts(j):
                    for ei in range(4 * j, 4 * j + 4):
                        for wi, (wsrc, nch) in enumerate(((w1, 8), (w3, 8), (w2, 4))):
                            dstv = Wc[wi][ei * 128:(ei + 1) * 128, :].rearrange("p (c h) -> p c h", c=nch)
                            srcv = wsrc[layer, ei].rearrange("(c p) h -> p c h", p=128)
                            sc.dma("pool", lambda e, dstv=dstv, srcv=srcv: e.dma_start(out=dstv, in_=srcv), R=[("oT", NT - 1)], W=[("Wc", wi, ei)], pool="cv", n=8)

                load_w(0)
                for j in range(8):
                    if j + 1 < 8:
                        load_w(j + 1)
                    project(j)
                    if kind == "diff":
                        attn_diff(j)
                    else:
                        attn_sb2(j)
                    if j < 7:
                        convert_experts(j)
                    if j == 6:
                        convert_experts(7)
                sc.barrier()

            with ExitStack() as es3:
                ent = es3.enter_context
                wo = ent(sbt("wo", [128, 8, D], BF16))
                gb = ent(sbt("gb", [128, 2, D], F32))
                NBW = 4
                stat = [ent(sbt(f"stat{i}", [128, 20], F32)) for i in range(NBW)]
                xres = [ent(sbt(f"xres{i}", [128, D], F32)) for i in range(NBW)]
                yb = [ent(sbt(f"yb{i}", [128, D], F32)) for i in range(NBW)]
                xbf = [ent(sbt(f"xbf{i}", [128, D], BF16)) for i in range(NBW)]
                for c in range(8):
                    sc.dma("pool", lambda e, c=c: e.dma_start(out=wo.ap()[:, c, :], in_=W_["wo"][c * 128:(c + 1) * 128, :]), W=[("wo", c)], pool="wo", n=8)
                load_gb(gb, ln_mix_g, ln_mix_b, layer)
                src = xr[2 * layer]
                dst = xr[2 * layer + 1]
                def wo_produce_a(t):
                    b = t % NBW
                    sc.dma("sp", lambda e: e.dma_start(out=xres[b].ap(), in_=src[t * 128:(t + 1) * 128, :]), W=[f"xres{b}"], pool="xr", n=4)
                    for hh in range(2):
                        pb = 2 * (t % 2) + hh
                        for jj in range(8):
                            sc.op("pe", lambda e, jj=jj, hh=hh, pb=pb: e.matmul(
                                bk(pb)[:, :], lhsT=oT.ap()[:, jj, t * 128:(t + 1) * 128], rhs=wo.ap()[:, jj, hh * 512:(hh + 1) * 512],
                                start=(jj == 0), stop=(jj == 7)), R=[("oT", t), ("wo", jj)], W=[f"bank{pb}"])

                def wo_produce(t):
                    b = t % NBW
                    for hh in range(2):
                        pb = 2 * (t % 2) + hh
                        sc.op("dve", lambda e, hh=hh, pb=pb: e.scalar_tensor_tensor(
                            out=yb[b].ap()[:, hh * 512:(hh + 1) * 512], in0=xres[b].ap()[:, hh * 512:(hh + 1) * 512], scalar=float(ALPHA),
                            in1=bk(pb)[:, :], op0=ALU.mult, op1=ALU.add), R=[f"xres{b}", f"bank{pb}"], W=[f"yb{b}"])

                ln_pipeline(None, wo_produce_a, wo_produce, yb, (lambda b: f"yb{b}"), stat, gb, dst, xbf)
                sc.barrier()

    def moe_layer(layer):
        with ExitStack() as es:
            ent = es.enter_context
            TG = 8
            acc = ent(sbt("acc", [128, TG, D], F32))
            wA = [ent(sbt(f"wA{i}", [128, 2, 8, HID], BF16)) for i in range(2)]
            wB = [ent(sbt(f"wB{i}", [128, 4, D], BF16)) for i in range(2)]
            sl = [ent(sbt(f"sl{i}", [128, 4, 512], BF16)) for i in range(2)]
            gg = [ent(sbt(f"gg{i}", [128, 4, 512], BF16)) for i in range(2)]
            wr = ent(sbt("wr", [128, 8, 36], BF16))
            br = ent(sbt("br", [128, 36], F32))
            lg = ent(sbt("lg", [128, NT, 36], F32))
            gates = ent(sbt("gates", [128, NT, NE], F32))
            rt = ent(sbt("rt", [128, 8, 64], F32))
            gb = ent(sbt("gb", [128, 2, D], F32))
            stat = ent(sbt("stat", [128, 20], F32))
            xres = [ent(sbt(f"xres{i}", [128, D], F32)) for i in range(2)]
            xbf = [ent(sbt(f"xbf{i}", [128, D], BF16)) for i in range(2)]

            sc.dma("pool", lambda e: e.dma_start(out=wr.ap()[:, :, 0:4], in_=w_group[layer].rearrange("(c p) g -> p c g", p=128)), W=["wr"])
            sc.dma("pool", lambda e: e.dma_start(out=wr.ap()[:, :, 4:36], in_=w_expert[layer].rearrange("(c p) g -> p c g", p=128)), W=["wr"])
            sc.dma("sp", lambda e: e.dma_start(out=br.ap()[:, 0:4], in_=b_group[layer:layer + 1, :].partition_broadcast(128)), W=["br"])
            sc.dma("sp", lambda e: e.dma_start(out=br.ap()[:, 4:36], in_=b_expert[layer:layer + 1, :].partition_broadcast(128)), W=["br"])
            load_gb(gb, ln_ffn_g, ln_ffn_b, layer)
            for t in range(NT):
                pb = 6 + (t % 2)
                for c in range(8):
                    sc.op("pe", lambda e, c=c, t=t, pb=pb: e.matmul(bk(pb)[:, 0:36], lhsT=xT.ap()[:, c, t * 128:(t + 1) * 128], rhs=wr.ap()[:, c, :],
                                                                    start=(c == 0), stop=(c == 7)), R=[("xT", t), "wr"], W=[f"bank{pb}"])
                sc.op("dve", lambda e, t=t, pb=pb: e.tensor_tensor(out=lg.ap()[:, t, :], in0=bk(pb)[:, 0:36], in1=br.ap(), op=ALU.add),
                      R=[f"bank{pb}", "br"], W=["lg"])
            for t in range(NT):
                r_ = rt.ap()
                L = lg.ap()[:, t, :]
                ops = []
                sc.op("dve", lambda e, L=L: e.tensor_reduce(out=r_[:, 0, 0:1], in_=L[:, 0:4], axis=AX.X, op=ALU.max), R=["lg"], W=["rt"])
                sc.op("dve", lambda e, L=L: e.tensor_scalar(out=r_[:, 0, 4:8], in0=L[:, 0:4], scalar1=r_[:, 0, 0:1], scalar2=None, op0=ALU.is_equal), R=["lg", "rt"], W=["rt"])
                sc.op("dve", lambda e, L=L: e.tensor_scalar(out=r_[:, 0, 8:12], in0=L[:, 0:4], scalar1=r_[:, 0, 0:1], scalar2=None, op0=ALU.subtract), R=["lg", "rt"], W=["rt"])
                sc.op("act", lambda e: e.activation(out=r_[:, 0, 8:12], in_=r_[:, 0, 8:12], func=AF.Exp, accum_out=r_[:, 0, 1:2]), R=["rt"], W=["rt"])
                sc.op("dve", lambda e: e.reciprocal(out=r_[:, 0, 2:3], in_=r_[:, 0, 1:2]), R=["rt"], W=["rt"])
                sc.op("dve", lambda e: e.tensor_scalar(out=r_[:, 0, 12:16], in0=r_[:, 0, 4:8], scalar1=-1.0, scalar2=1e30, op0=ALU.add, op1=ALU.mult), R=["rt"], W=["rt"])
                sc.op("dve", lambda e, L=L: e.tensor_tensor(out=r_[:, 1, 0:32].rearrange("p (g e) -> p g e", g=4), in0=L[:, 4:36].rearrange("p (g e) -> p g e", g=4),
                                                            in1=r_[:, 0, 12:16].unsqueeze(2).to_broadcast([128, 4, 8]), op=ALU.add), R=["lg", "rt"], W=["rt"])
                sc.op("dve", lambda e: e.max(out=r_[:, 2, 0:8], in_=r_[:, 1, 0:32]), R=["rt"], W=["rt"])
                sc.op("dve", lambda e: e.tensor_scalar(out=r_[:, 3, 0:32], in0=r_[:, 1, 0:32], scalar1=r_[:, 2, 0:1], scalar2=None, op0=ALU.is_equal), R=["rt"], W=["rt"])
                sc.op("dve", lambda e: e.tensor_scalar(out=r_[:, 4, 0:32], in0=r_[:, 1, 0:32], scalar1=r_[:, 2, 1:2], scalar2=None, op0=ALU.is_equal), R=["rt"], W=["rt"])
                sc.op("dve", lambda e: e.tensor_tensor(out=r_[:, 2, 8:9], in0=r_[:, 2, 1:2], in1=r_[:, 2, 0:1], op=ALU.subtract), R=["rt"], W=["rt"])
                sc.op("act", lambda e: e.activation(out=r_[:, 2, 9:10], in_=r_[:, 2, 8:9], func=AF.Exp), R=["rt"], W=["rt"])
                sc.op("dve", lambda e: e.tensor_scalar(out=r_[:, 2, 10:11], in0=r_[:, 2, 9:10], scalar1=1.0, scalar2=None, op0=ALU.add), R=["rt"], W=["rt"])
                sc.op("dve", lambda e: e.reciprocal(out=r_[:, 2, 11:12], in_=r_[:, 2, 10:11]), R=["rt"], W=["rt"])
                sc.op("dve", lambda e: e.tensor_tensor(out=r_[:, 2, 12:13], in0=r_[:, 2, 11:12], in1=r_[:, 0, 2:3], op=ALU.mult), R=["rt"], W=["rt"])
                sc.op("dve", lambda e: e.tensor_tensor(out=r_[:, 2, 13:14], in0=r_[:, 0, 2:3], in1=r_[:, 2, 12:13], op=ALU.subtract), R=["rt"], W=["rt"])
                sc.op("dve", lambda e: e.tensor_scalar(out=r_[:, 3, 0:32], in0=r_[:, 3, 0:32], scalar1=r_[:, 2, 12:13], scalar2=None, op0=ALU.mult), R=["rt"], W=["rt"])
                sc.op("dve", lambda e, t=t: e.scalar_tensor_tensor(out=gates.ap()[:, t, :], in0=r_[:, 4, 0:32], scalar=r_[:, 2, 13:14], in1=r_[:, 3, 0:32],
                                                                   op0=ALU.mult, op1=ALU.add), R=["rt"], W=[("gates", t)])

            def load_expert(ei, b):
                sc.dma("pool", lambda e: e.dma_start(out=wA[b].ap()[:, 0, :, :], in_=w1[layer, ei].rearrange("(c p) h -> p c h", p=128)), W=[f"wA{b}"])
                sc.dma("pool", lambda e: e.dma_start(out=wA[b].ap()[:, 1, :, :], in_=w3[layer, ei].rearrange("(c p) h -> p c h", p=128)), W=[f"wA{b}"])
                sc.dma("pool", lambda e: e.dma_start(out=wB[b].ap(), in_=w2[layer, ei].rearrange("(c p) d -> p c d", p=128)), W=[f"wB{b}"])

            src = xr[2 * layer + 1]
            dst = xr[2 * layer + 2]
            last = (layer == DEPTH - 1)
            ngroups = NT // TG
            it = 0
            load_expert(0, 0)
            for G in range(ngroups):
                for ei in range(NE):
                    b = it % 2
                    nxt = it + 1
                    if nxt < ngroups * NE:
                        load_expert(nxt % NE, nxt % 2)
                    for half in range(TG // 4):
                        tok0 = (G * TG + half * 4) * 128
                        hb = (it * (TG // 4) + half) % 2
                        for m in range(4):
                            for which in range(2):
                                pb = 2 * which + (m % 2)
                                for c in range(8):
                                    sc.op("pe", lambda e, c=c, m=m, which=which, pb=pb: e.matmul(
                                        bk(pb)[:, :], lhsT=wA[b].ap()[:, which, c, m * 128:(m + 1) * 128], rhs=xT.ap()[:, c, tok0:tok0 + 512],
                                        start=(c == 0), stop=(c == 7)),
                                        R=[f"wA{b}"] + [("xT", tok0 // 128 + q) for q in range(4)], W=[f"bank{pb}"])
                            sc.op("act", lambda e, m=m, hb=hb: e.activation(out=sl[hb].ap()[:, m, :], in_=bk(m % 2)[:, :], func=AF.Silu),
                                  R=[f"bank{m % 2}"], W=[f"sl{hb}"])
                            sc.op("dve", lambda e, m=m, hb=hb: e.tensor_tensor(out=gg[hb].ap()[:, m, :], in0=sl[hb].ap()[:, m, :], in1=bk(2 + m % 2)[:, :], op=ALU.mult),
                                  R=[f"sl{hb}", f"bank{2 + m % 2}"], W=[f"gg{hb}"])
                        for tt in range(4):
                            tl = half * 4 + tt
                            tglob = G * TG + tl
                            for hh in range(2):
                                pb = 4 + hh
                                for m in range(4):
                                    sc.op("pe", lambda e, m=m, tt=tt, hh=hh, pb=pb: e.matmul(
                                        bk(pb)[:, :], lhsT=gg[hb].ap()[:, m, tt * 128:(tt + 1) * 128], rhs=wB[b].ap()[:, m, hh * 512:(hh + 1) * 512],
                                        start=(m == 0), stop=(m == 3)), R=[f"gg{hb}", f"wB{b}"], W=[f"bank{pb}"])
                                if ei == 0:
                                    sc.op("dve", lambda e, tl=tl, hh=hh, pb=pb, tglob=tglob: e.tensor_scalar(
                                        out=acc.ap()[:, tl, hh * 512:(hh + 1) * 512], in0=bk(pb)[:, :], scalar1=gates.ap()[:, tglob, ei:ei + 1], scalar2=None, op0=ALU.mult),
                                        R=[f"bank{pb}", ("gates", tglob)], W=[("acc", tl)])
                                else:
                                    sc.op("dve", lambda e, tl=tl, hh=hh, pb=pb, tglob=tglob, ei=ei: e.scalar_tensor_tensor(
                                        out=acc.ap()[:, tl, hh * 512:(hh + 1) * 512], in0=bk(pb)[:, :], scalar=gates.ap()[:, tglob, ei:ei + 1],
                                        in1=acc.ap()[:, tl, hh * 512:(hh + 1) * 512], op0=ALU.mult, op1=ALU.add),
                                        R=[f"bank{pb}", ("gates", tglob), ("acc", tl)], W=[("acc", tl)])
                    it += 1
                for tl in range(TG):
                    t = G * TG + tl
                    b2 = t % 2
                    sc.dma("sp", lambda e, b2=b2, t=t: e.dma_start(out=xres[b2].ap(), in_=src[t * 128:(t + 1) * 128, :]), W=[f"xres{b2}"], pool="xr", n=4)
                    sc.op("dve", lambda e, b2=b2, tl=tl: e.scalar_tensor_tensor(out=acc.ap()[:, tl, :], in0=xres[b2].ap(), scalar=float(ALPHA), in1=acc.ap()[:, tl, :],
                                                                                op0=ALU.mult, op1=ALU.add), R=[f"xres{b2}", ("acc", tl)], W=[("acc", tl)])
                    ln_tile(acc.ap()[:, tl, :], ("acc", tl), gb, stat, t, dst, None if last else (xbf[b2].ap(), f"xbf{b2}"), 6 + b2)
            sc.barrier()

    def moe_layer_sparse(layer):
        src = xr[2 * layer + 1]
        dst = xr[2 * layer + 2]
        last = (layer == DEPTH - 1)
        w13rows = [w1.rearrange("l e d h -> (l e d) h"), w3.rearrange("l e d h -> (l e d) h")]
        w2rows = w2.rearrange("l e h d -> (l e h) d")
        with ExitStack() as es:
            ent = es.enter_context
            WT = ent(sbt("WT", [128, NT, 2], F32))
            posi = ent(sbt("posi", [128, 2, NT], I32))
            widx = ent(sbt("widx", [128, 2, NSLOT], I32))
            with ExitStack() as es1:
                ent1 = es1.enter_context
                M1 = ent1(sbt("M1", [128, NT, NE], F32))
                M2 = ent1(sbt("M2", [128, NT, NE], F32))
                wr = ent1(sbt("wr", [128, 8, 36], BF16))
                br = ent1(sbt("br", [128, 36], F32))
                lg = ent1(sbt("lg", [128, NT, 36], F32))
                rs = ent1(sbt("rs", [128, 6, NT], F32))
                r4 = ent1(sbt("r4", [128, 3, NT, 4], F32))
                elm = ent1(sbt("elm", [128, NT, NE], F32))
                Abf = ent1(sbt("Abf", [128, NT, NE], BF16))
                ones_bf = ent1(sbt("ones_bf", [128, 128], BF16))
                cntS = ent1(sbt("cntS", [128, NE], F32))
                nsl = ent1(sbt("nsl", [128, NE], F32))
                offe = ent1(sbt("offe", [128, NE], F32))
                posb = ent1(sbt("posb", [128, NE], F32))
                posfull = ent1(sbt("posfull", [128, NT, NE], F32))
                ptmp = ent1(sbt("ptmp", [128, NT, NE], F32))
                posf = ent1(sbt("posf", [128, 2, NT], F32))
                sidx = ent1(sbt("sidx", [128, NSLOT], F32))
                pidx = ent1(sbt("pidx", [128, 4], F32))
                cmp_ = ent1(sbt("cmp", [128, NSLOT, NE], F32))
                eidf = ent1(sbt("eidf", [128, 3, NSLOT], F32))
                xres = [ent1(sbt(f"xres{i}", [128, D], F32)) for i in range(4)]
                xbf = [ent1(sbt(f"xbf{i}", [128, D], BF16)) for i in range(8)]

                sc.dma("pool", lambda e: e.dma_start(out=wr.ap()[:, :, 0:4], in_=w_group[layer].rearrange("(c p) g -> p c g", p=128)), W=["wr"])
                sc.dma("pool", lambda e: e.dma_start(out=wr.ap()[:, :, 4:36], in_=w_expert[layer].rearrange("(c p) g -> p c g", p=128)), W=["wr"])
                sc.dma("sp", lambda e: e.dma_start(out=br.ap()[:, 0:4], in_=b_group[layer:layer + 1, :].partition_broadcast(128)), W=["br"])
                sc.dma("sp", lambda e: e.dma_start(out=br.ap()[:, 4:36], in_=b_expert[layer:layer + 1, :].partition_broadcast(128)), W=["br"])
                sc.dma("sp", lambda e: e.dma_start(out=sidx.ap(), in_=c_sidx), W=["sidx"])
                sc.dma("sp", lambda e: e.dma_start(out=pidx.ap()[:, 0:1], in_=c_pidx), W=["pidx"])
                sc.op("dve", lambda e: e.memset(ones_bf.ap(), 1.0), W=["ones_bf"])
                for t in range(NT):
                    pb = 6 + (t % 2)
                    for c in range(8):
                        sc.op("pe", lambda e, c=c, t=t, pb=pb: e.matmul(bk(pb)[:, 0:36], lhsT=xT.ap()[:, c, t * 128:(t + 1) * 128], rhs=wr.ap()[:, c, :],
                                                                        start=(c == 0), stop=(c == 7)), R=[("xT", t), "wr"], W=[f"bank{pb}"])
                    sc.op("dve", lambda e, t=t, pb=pb: e.tensor_tensor(out=lg.ap()[:, t, :], in0=bk(pb)[:, 0:36], in1=br.ap(), op=ALU.add),
                          R=[f"bank{pb}", "br"], W=["lg"])
                LG = lg.ap()
                gmax = rs.ap()[:, 0, :]
                gsum = rs.ap()[:, 1, :]
                ggate = rs.ap()[:, 2, :]
                m1 = rs.ap()[:, 3, :]
                m2 = rs.ap()[:, 4, :]
                rr = rs.ap()[:, 5, :]
                gm = r4.ap()[:, 0, :, :]
                dd = r4.ap()[:, 1, :, :]
                pen = r4.ap()[:, 2, :, :]
                sc.op("dve", lambda e: e.tensor_reduce(out=gmax, in_=LG[:, :, 0:4], axis=AX.X, op=ALU.max), R=["lg"], W=["rs"])
                sc.op("dve", lambda e: e.tensor_tensor(out=gm, in0=LG[:, :, 0:4], in1=gmax.unsqueeze(2).to_broadcast([128, NT, 4]), op=ALU.is_equal), R=["lg", "rs"], W=["r4"])
                sc.op("dve", lambda e: e.tensor_tensor(out=dd, in0=LG[:, :, 0:4], in1=gmax.unsqueeze(2).to_broadcast([128, NT, 4]), op=ALU.subtract), R=["lg", "rs"], W=["r4"])
                sc.op("act", lambda e: e.activation(out=dd, in_=dd, func=AF.Exp), R=["r4"], W=["r4"])
                sc.op("dve", lambda e: e.tensor_reduce(out=gsum, in_=dd, axis=AX.X, op=ALU.add), R=["r4"], W=["rs"])
                sc.op("dve", lambda e: e.reciprocal(out=ggate, in_=gsum), R=["rs"], W=["rs"])
                sc.op("dve", lambda e: e.tensor_scalar(out=pen, in0=gm, scalar1=-1.0, scalar2=1e30, op0=ALU.add, op1=ALU.mult), R=["r4"], W=["r4"])
                elm4 = elm.ap().rearrange("p t (g e) -> p t g e", g=4)
                sc.op("dve", lambda e: e.tensor_tensor(out=elm4, in0=LG[:, :, 4:36].rearrange("p t (g e) -> p t g e", g=4),
                                                       in1=pen.unsqueeze(3).to_broadcast([128, NT, 4, 8]), op=ALU.add), R=["lg", "r4"], W=["elm"])
                sc.op("dve", lambda e: e.tensor_reduce(out=m1, in_=elm.ap(), axis=AX.X, op=ALU.max), R=["elm"], W=["rs"])
                sc.op("dve", lambda e: e.tensor_tensor(out=M1.ap(), in0=elm.ap(), in1=m1.unsqueeze(2).to_broadcast([128, NT, NE]), op=ALU.is_equal), R=["elm", "rs"], W=["M1"])
                sc.op("dve", lambda e: e.scalar_tensor_tensor(out=elm.ap(), in0=M1.ap(), scalar=-1e30, in1=elm.ap(), op0=ALU.mult, op1=ALU.add), R=["elm", "M1"], W=["elm"])
                sc.op("dve", lambda e: e.tensor_reduce(out=m2, in_=elm.ap(), axis=AX.X, op=ALU.max), R=["elm"], W=["rs"])
                sc.op("dve", lambda e: e.tensor_tensor(out=M2.ap(), in0=elm.ap(), in1=m2.unsqueeze(2).to_broadcast([128, NT, NE]), op=ALU.is_equal), R=["elm", "rs"], W=["M2"])
                sc.op("dve", lambda e: e.tensor_tensor(out=rr, in0=m2, in1=m1, op=ALU.subtract), R=["rs"], W=["rs"])
                sc.op("act", lambda e: e.activation(out=rr, in_=rr, func=AF.Exp), R=["rs"], W=["rs"])
                sc.op("dve", lambda e: e.tensor_scalar(out=rr, in0=rr, scalar1=1.0, scalar2=None, op0=ALU.add), R=["rs"], W=["rs"])
                sc.op("dve", lambda e: e.reciprocal(out=rr, in_=rr), R=["rs"], W=["rs"])
                sc.op("dve", lambda e: e.tensor_tensor(out=WT.ap()[:, :, 0], in0=rr, in1=ggate, op=ALU.mult), R=["rs"], W=["WT"])
                sc.op("dve", lambda e: e.tensor_tensor(out=WT.ap()[:, :, 1], in0=ggate, in1=WT.ap()[:, :, 0], op=ALU.subtract), R=["rs", "WT"], W=["WT"])
                sc.op("dve", lambda e: e.tensor_tensor(out=Abf.ap(), in0=M1.ap(), in1=M2.ap(), op=ALU.add), R=["M1", "M2"], W=["Abf"])
                for t in range(NT):
                    rb_ = t // 16
                    col = (t % 16) * 32
                    for tp in range(t):
                        sc.op("pe", lambda e, tp=tp, rb_=rb_, col=col: e.matmul(bk(rb_)[:, col:col + 32], lhsT=ones_bf.ap(), rhs=Abf.ap()[:, tp, :],
                                                                                  start=(tp == 0), stop=False, skip_group_check=True),
                              R=["Abf", "ones_bf"], W=[f"bank{rb_}"])
                    sc.op("pe", lambda e, t=t, rb_=rb_, col=col: e.matmul(bk(rb_)[:, col:col + 32], lhsT=tri_low.ap(), rhs=Abf.ap()[:, t, :],
                                                                          start=(t == 0), stop=True, skip_group_check=True),
                          R=["Abf", "tri_low"], W=[f"bank{rb_}"])
                for t in range(NT):
                    sc.op("pe", lambda e, t=t: e.matmul(bk(2)[:, 0:32], lhsT=ones_bf.ap(), rhs=Abf.ap()[:, t, :], start=(t == 0), stop=(t == NT - 1)),
                          R=["Abf", "ones_bf"], W=["bank2"])
                sc.op("dve", lambda e: e.tensor_copy(out=cntS.ap(), in_=bk(2)[:, 0:32]), R=["bank2"], W=["cntS"])
                sc.op("dve", lambda e: e.tensor_scalar(out=nsl.ap(), in0=cntS.ap(), scalar1=0.0, scalar2=None, op0=ALU.is_gt), R=["cntS"], W=["nsl"])
                for k in range(1, KMAX):
                    sc.op("dve", lambda e, k=k: e.scalar_tensor_tensor(out=nsl.ap(), in0=cntS.ap(), scalar=float(k * SL), in1=nsl.ap(), op0=ALU.is_gt, op1=ALU.add),
                          R=["cntS", "nsl"], W=["nsl"])
                sc.op("dve", lambda e: e.tensor_copy(out=offe.ap()[:, 0:1], in_=nsl.ap()[:, 0:1]), R=["nsl"], W=["offe"])
                for ei in range(1, NE):
                    sc.op("dve", lambda e, ei=ei: e.tensor_tensor(out=offe.ap()[:, ei:ei + 1], in0=offe.ap()[:, ei - 1:ei], in1=nsl.ap()[:, ei:ei + 1], op=ALU.add),
                          R=["nsl", "offe"], W=["offe"])
                sc.op("dve", lambda e: e.tensor_tensor(out=posb.ap(), in0=offe.ap(), in1=nsl.ap(), op=ALU.subtract), R=["offe", "nsl"], W=["posb"])
                sc.op("dve", lambda e: e.tensor_scalar(out=posb.ap(), in0=posb.ap(), scalar1=float(SL), scalar2=None, op0=ALU.mult), R=["posb"], W=["posb"])
                for hh in range(2):
                    sc.op("dve", lambda e, hh=hh: e.tensor_tensor(out=posfull.ap()[:, 16 * hh:16 * hh + 16, :], in0=bk(hh)[:, :].rearrange("p (a f) -> p a f", a=16),
                                                                  in1=posb.ap().unsqueeze(1).to_broadcast([128, 16, NE]), op=ALU.add),
                          R=[f"bank{hh}", "posb"], W=["posfull"])
                for ci, M_ in enumerate((M1, M2)):
                    mres = "M1" if ci == 0 else "M2"
                    sc.op("dve", lambda e, M_=M_: e.tensor_tensor(out=ptmp.ap(), in0=posfull.ap(), in1=M_.ap(), op=ALU.mult), R=["posfull", mres], W=["ptmp"])
                    sc.op("dve", lambda e, ci=ci: e.tensor_reduce(out=posf.ap()[:, ci, :], in_=ptmp.ap(), axis=AX.X, op=ALU.add), R=["ptmp"], W=["posf"])
                sc.op("dve", lambda e: e.tensor_scalar(out=posf.ap(), in0=posf.ap(), scalar1=float(NPOS - 1), scalar2=None, op0=ALU.min), R=["posf"], W=["posf"])
                sc.op("dve", lambda e: e.tensor_copy(out=posi.ap(), in_=posf.ap()), R=["posf"], W=["posi"])
                sc.op("dve", lambda e: e.tensor_tensor(out=cmp_.ap(), in0=offe.ap().unsqueeze(1).to_broadcast([128, NSLOT, NE]),
                                                       in1=sidx.ap().unsqueeze(2).to_broadcast([128, NSLOT, NE]), op=ALU.is_le), R=["offe", "sidx"], W=["cmp"])
                sc.op("dve", lambda e: e.tensor_reduce(out=eidf.ap()[:, 0, :], in_=cmp_.ap(), axis=AX.X, op=ALU.add), R=["cmp"], W=["eidf"])
                sc.op("dve", lambda e: e.tensor_scalar(out=eidf.ap()[:, 0, :], in0=eidf.ap()[:, 0, :], scalar1=float(NE - 1), scalar2=None, op0=ALU.min), R=["eidf"], W=["eidf"])
                sc.op("dve", lambda e: e.tensor_scalar(out=pidx.ap()[:, 1:2], in0=pidx.ap()[:, 0:1], scalar1=float(layer * NE * D), scalar2=None, op0=ALU.add), R=["pidx"], W=["pidx"])
                sc.op("dve", lambda e: e.tensor_scalar(out=pidx.ap()[:, 2:3], in0=pidx.ap()[:, 0:1], scalar1=float(layer * NE * HID), scalar2=None, op0=ALU.add), R=["pidx"], W=["pidx"])
                sc.op("dve", lambda e: e.tensor_scalar(out=eidf.ap()[:, 1, :], in0=eidf.ap()[:, 0, :], scalar1=128.0, scalar2=pidx.ap()[:, 0:1], op0=ALU.mult, op1=ALU.add),
                      R=["eidf", "pidx"], W=["eidf"])
                sc.op("dve", lambda e: e.tensor_scalar(out=eidf.ap()[:, 2, :], in0=eidf.ap()[:, 0, :], scalar1=128.0, scalar2=pidx.ap()[:, 0:1], op0=ALU.mult, op1=ALU.add),
                      R=["eidf", "pidx"], W=["eidf"])
                sc.op("dve", lambda e: e.tensor_copy(out=widx.ap(), in_=eidf.ap()[:, 1:3, :]), R=["eidf"], W=["widx"])
                for t in range(NT):
                    b = t % 4
                    b8 = t % 8
                    sc.dma("sp", lambda e, b=b, t=t: e.dma_start(out=xres[b].ap(), in_=src[t * 128:(t + 1) * 128, :]), W=[f"xres{b}"], pool="xr", n=4)
                    sc.op("act", lambda e, b=b, b8=b8: e.activation(out=xbf[b8].ap(), in_=xres[b].ap(), func=AF.Copy), R=[f"xres{b}"], W=[f"xbf{b8}"])
                    for ci in range(2):
                        sc.dma("pool", lambda e, b8=b8, t=t, ci=ci: e.indirect_dma_start(
                            out=Xs, out_offset=bass.IndirectOffsetOnAxis(ap=posi.ap()[:, ci, t:t + 1], axis=0), in_=xbf[b8].ap(), in_offset=None),
                            R=[f"xbf{b8}", "posi"], W=[("Xs", t, ci)], pool="sct", n=8)
                sc.barrier()

            with ExitStack() as es2:
                ent2 = es2.enter_context
                wA = [ent2(sbt(f"wA{i}", [128, 2, 8, HID], BF16)) for i in range(2)]
                wB = [ent2(sbt(f"wB{i}", [128, 4, D], BF16)) for i in range(2)]
                xs = [[ent2(sbt(f"xs{i}_{k}", [128, D], BF16)) for k in range(SL // 128)] for i in range(2)]
                xsT = [ent2(sbt(f"xsT{i}", [128, 8, SL], BF16)) for i in range(2)]
                sl = [ent2(sbt(f"sl{i}", [128, 4, SL], BF16)) for i in range(2)]
                gg = [ent2(sbt(f"gg{i}", [128, 4, SL], BF16)) for i in range(2)]
                ys = [ent2(sbt(f"ys{i}", [128, D], F32)) for i in range(2)]
                NS3 = SL // 128

                def load_slot(s_):
                    b = s_ % 2
                    for which in range(2):
                        sc.dma("pool", lambda e, which=which: e.indirect_dma_start(
                            out=wA[b].ap()[:, which, :, :].rearrange("p c h -> p (c h)"), out_offset=None, in_=Wc[which],
                            in_offset=bass.IndirectOffsetOnAxis(ap=widx.ap()[:, 0, s_:s_ + 1], axis=0)),
                            R=["widx"], W=[(f"wA{b}", which)], pool="wgt", n=12)
                    sc.dma("pool", lambda e: e.indirect_dma_start(
                        out=wB[b].ap().rearrange("p c d -> p (c d)"), out_offset=None, in_=Wc[2],
                        in_offset=bass.IndirectOffsetOnAxis(ap=widx.ap()[:, 0, s_:s_ + 1], axis=0)),
                        R=["widx"], W=[f"wB{b}"], pool="wgt", n=12)
                    for k in range(NS3):
                        r0 = s_ * SL + k * 128
                        sc.dma("sp", lambda e, k=k, r0=r0: e.dma_start(out=xs[b][k].ap(), in_=Xs[r0:r0 + 128, :]), R=["Xs"], W=[f"xs{b}_{k}"], pool="xs", n=6)

                load_slot(0)
                for s_ in range(NSLOT):
                    b = s_ % 2
                    if s_ + 1 < NSLOT:
                        load_slot(s_ + 1)
                    for k in range(NS3):
                        tb = 6 + (k % 2)
                        for c in range(8):
                            sc.op("pe", lambda e, c=c, k=k, tb=tb: e.transpose(out=bkh(tb)[:, c * 128:(c + 1) * 128], in_=xs[b][k].ap()[:, c * 128:(c + 1) * 128],
                                                                              identity=ident.ap()), R=[f"xs{b}_{k}", "ident"], W=[f"bank{tb}"])
                        sc.op("act" if k % 2 == 0 else "dve",
                              lambda e, k=k, tb=tb: (e.activation(out=xsT[b].ap()[:, :, k * 128:(k + 1) * 128], in_=bkh(tb)[:, :].rearrange("p (c f) -> p c f", c=8), func=AF.Copy)
                                                     if k % 2 == 0 else
                                                     e.tensor_copy(out=xsT[b].ap()[:, :, k * 128:(k + 1) * 128], in_=bkh(tb)[:, :].rearrange("p (c f) -> p c f", c=8))),
                              R=[f"bank{tb}"], W=[f"xsT{b}"])
                    for m in range(4):
                        for which in range(2):
                            pb = 2 * which + (m % 2)
                            for c in range(8):
                                sc.op("pe", lambda e, c=c, m=m, which=which, pb=pb: e.matmul(
                                    bk(pb)[:, 0:SL], lhsT=wA[b].ap()[:, which, c, m * 128:(m + 1) * 128], rhs=xsT[b].ap()[:, c, :],
                                    start=(c == 0), stop=(c == 7)), R=[(f"wA{b}", which), f"xsT{b}"], W=[f"bank{pb}"])
                        sc.op("act", lambda e, m=m: e.activation(out=sl[b].ap()[:, m, :], in_=bk(m % 2)[:, 0:SL], func=AF.Silu), R=[f"bank{m % 2}"], W=[f"sl{b}"])
                        sc.op("dve", lambda e, m=m: e.tensor_tensor(out=gg[b].ap()[:, m, :], in0=sl[b].ap()[:, m, :], in1=bk(2 + m % 2)[:, 0:SL], op=ALU.mult),
                              R=[f"sl{b}", f"bank{2 + m % 2}"], W=[f"gg{b}"])
                    for k in range(NS3):
                        yb_ = (s_ * NS3 + k) % 2
                        for hh in range(2):
                            pb = 4 + hh
                            for m in range(4):
                                sc.op("pe", lambda e, m=m, k=k, hh=hh, pb=pb: e.matmul(
                                    bk(pb)[:, :], lhsT=gg[b].ap()[:, m, k * 128:(k + 1) * 128], rhs=wB[b].ap()[:, m, hh * 512:(hh + 1) * 512],
                                    start=(m == 0), stop=(m == 3)), R=[f"gg{b}", f"wB{b}"], W=[f"bank{pb}"])
                            if hh == 0:
                                sc.op("act", lambda e, yb_=yb_, pb=pb: e.activation(out=ys[yb_].ap()[:, 0:512], in_=bk(pb)[:, :], func=AF.Copy), R=[f"bank{pb}"], W=[f"ys{yb_}"])
                            else:
                                sc.op("dve", lambda e, yb_=yb_, pb=pb: e.tensor_copy(out=ys[yb_].ap()[:, 512:1024], in_=bk(pb)[:, :]), R=[f"bank{pb}"], W=[f"ys{yb_}"])
                        r0 = s_ * SL + k * 128
                        sc.dma("sp", lambda e, yb_=yb_, r0=r0: e.dma_start(out=Ys[r0:r0 + 128, :], in_=ys[yb_].ap()), R=[f"ys{yb_}"], W=[("Ys", s_, k)], pool="ys", n=4)
                sc.barrier()

            with ExitStack() as es3:
                ent3 = es3.enter_context
                gb = ent3(sbt("gb", [128, 2, D], F32))
                NB3 = 4
                stat = [ent3(sbt(f"stat{i}", [128, 20], F32)) for i in range(NB3)]
                g1 = [ent3(sbt(f"g1_{i}", [128, D], F32)) for i in range(NB3)]
                g2 = [ent3(sbt(f"g2_{i}", [128, D], F32)) for i in range(NB3)]
                xres = [ent3(sbt(f"xres{i}", [128, D], F32)) for i in range(NB3)]
                yb = [ent3(sbt(f"yb{i}", [128, D], F32)) for i in range(NB3)]
                xbf = [ent3(sbt(f"xbf{i}", [128, D], BF16)) for i in range(NB3)]
                load_gb(gb, ln_ffn_g, ln_ffn_b, layer)
                def m3_fetch(t):
                    b = t % NB3
                    sc.dma("pool", lambda e: e.indirect_dma_start(out=g1[b].ap(), out_offset=None, in_=Ys,
                                                                  in_offset=bass.IndirectOffsetOnAxis(ap=posi.ap()[:, 0, t:t + 1], axis=0)),
                           R=["Ys", "posi"], W=[f"g1_{b}"], pool="gth", n=8)
                    sc.dma("pool", lambda e: e.indirect_dma_start(out=g2[b].ap(), out_offset=None, in_=Ys,
                                                                  in_offset=bass.IndirectOffsetOnAxis(ap=posi.ap()[:, 1, t:t + 1], axis=0)),
                           R=["Ys", "posi"], W=[f"g2_{b}"], pool="gth", n=8)
                    sc.dma("sp", lambda e: e.dma_start(out=xres[b].ap(), in_=src[t * 128:(t + 1) * 128, :]), W=[f"xres{b}"], pool="xr", n=4)

                def m3_produce_a(t):
                    b = t % NB3
                    sc.op("act", lambda e: e.activation(out=g1[b].ap(), in_=g1[b].ap(), func=AF.Copy, scale=WT.ap()[:, t, 0:1]), R=[f"g1_{b}", "WT"], W=[f"g1_{b}"])

                def m3_produce(t):
                    b = t % NB3
                    sc.op("dve", lambda e: e.scalar_tensor_tensor(out=g2[b].ap(), in0=g2[b].ap(), scalar=WT.ap()[:, t, 1:2], in1=g1[b].ap(), op0=ALU.mult, op1=ALU.add),
                          R=[f"g2_{b}", f"g1_{b}", "WT"], W=[f"g2_{b}"])
                    sc.op("dve", lambda e: e.scalar_tensor_tensor(out=yb[b].ap(), in0=xres[b].ap(), scalar=float(ALPHA), in1=g2[b].ap(), op0=ALU.mult, op1=ALU.add),
                          R=[f"xres{b}", f"g2_{b}"], W=[f"yb{b}"])

                ln_pipeline(m3_fetch, m3_produce_a, m3_produce, yb, (lambda b: f"yb{b}"), stat, gb, dst, None if last else xbf)
                sc.barrier()

    stages = []
    for layer in range(DEPTH):
        stages.append(("att", layer))
        stages.append(("moe", layer))
    for i, (kind, layer) in enumerate(stages):
        if stop_after is not None and i > stop_after:
            break
        if kind == "att":
            attention_layer(layer)
        elif SPARSE:
            moe_layer_sparse(layer)
        else:
            moe_layer(layer)

    sc.final_wait("sp")
    return nc


_CACHE = {}


def _prep_common(inputs):
    f = lambda a: np.ascontiguousarray(np.asarray(a), dtype=np.float32)
    m = {
        "rel_table": f(inputs["rel_table"]),
        "da_wq": f(inputs["da_wq"][0]), "da_wk": f(inputs["da_wk"][0]), "da_wv": f(inputs["da_wv"][0]), "da_wo": f(inputs["da_wo"][0]),
        "sb_wq": f(inputs["sb_wq"][0]), "sb_wk": f(inputs["sb_wk"][0]), "sb_wv": f(inputs["sb_wv"][0]), "sb_wo": f(inputs["sb_wo"][0]),
        "da_l": f(np.stack([inputs["da_lq1"][0], inputs["da_lk1"][0], inputs["da_lq2"][0], inputs["da_lk2"][0]], axis=0)),
        "da_subln_g": f(inputs["da_subln_g"]),
        "ln_mix_g": f(inputs["ln_mix_g"]), "ln_mix_b": f(inputs["ln_mix_b"]),
        "ln_ffn_g": f(inputs["ln_ffn_g"]), "ln_ffn_b": f(inputs["ln_ffn_b"]),
        "moe_w_group": f(inputs["moe_w_group"]), "moe_b_group": f(inputs["moe_b_group"]),
        "moe_w_expert": f(inputs["moe_w_expert"]), "moe_b_expert": f(inputs["moe_b_expert"]),
        "moe_w1": f(inputs["moe_w1"]), "moe_w3": f(inputs["moe_w3"]), "moe_w2": f(inputs["moe_w2"]),
    }
    m.update(_consts_host())
    return m


def kernel(**inputs):
    if "nc" not in _CACHE:
        _CACHE["nc"] = build_program()
    nc = _CACHE["nc"]
    common = _prep_common(inputs)
    x = np.asarray(inputs["x"], dtype=np.float32)
    in_maps = []
    for b in range(8):
        m = dict(common)
        m["x"] = np.ascontiguousarray(x[b])
        in_maps.append(m)
    res = run_bass_kernel_spmd(nc, in_maps, core_ids=list(range(8)))
    return np.stack([np.asarray(r["out"], dtype=np.float32) for r in res.results], axis=0)
```
